# Optimizing a Trainium2 kernel written in Bass

```python
import math
import jax
import jax.numpy as jnp
from jax import lax
import numpy as np

D_MODEL = 2048
BATCH = 4
SEQ = 2048
DEPTH = 2

A_HEADS = 4
A_DH = 64
B_HEADS = 4
B_DH = 128
C_HEADS = 4
C_DH = 128
D_HEADS = 4
D_DH = 128
IDX_HEADS = 8
IDX_DH = 64
N_BRANCH = 4
BRANCH_W = D_MODEL // 4
Q_BLOCK = 128
MOBA_BLOCK = 256
MOBA_TOPK = 3
MOBA_QCHUNK = 32
DSA_TOPK = 256
N_EXPERTS = 32
TOP_K = 4
D_FF = D_MODEL
SWIGLU_ALPHA = 1.702
SWIGLU_LIMIT = 7.0
MOE_BLOCK = 256
N_ALIBI = A_HEADS + C_HEADS + D_HEADS
RMS_EPS = 1e-6
IN_SIZES = (
    A_HEADS * 2 * A_DH, A_HEADS * 2 * A_DH, A_HEADS * 2 * A_DH,
    B_HEADS * B_DH, B_HEADS * B_DH, B_HEADS * B_DH,
    C_HEADS * C_DH, C_HEADS * C_DH, C_HEADS * C_DH,
    D_HEADS * D_DH, D_DH, D_DH, IDX_HEADS * IDX_DH, IDX_DH, IDX_HEADS,
    N_BRANCH * D_MODEL,
)
N_IN = sum(IN_SIZES)

kernel_name = 'hybrid_gated_mixers_adaln_moe'


def rms_norm(x, g):
    xf = x.astype(jnp.float32)
    y = xf * lax.rsqrt(jnp.mean(xf * xf, axis=-1, keepdims=True) + RMS_EPS)
    return (y * g.astype(jnp.float32)).astype(x.dtype)


def alibi_slopes():
    return jnp.exp2(-8.0 * (jnp.arange(N_ALIBI, dtype=jnp.float32) + 1.0) / N_ALIBI)


def diff_attention(q, k, v, slopes, lam, lambda_init, subln_g):
    bsz, seq = q.shape[:2]
    scale = A_DH ** -0.5
    qh = q.transpose(0, 2, 3, 1, 4)
    kh = k.transpose(0, 2, 3, 1, 4)
    vh = v.transpose(0, 2, 1, 3)
    outs = []
    for i in range(seq // Q_BLOCK):
        lo, hi = i * Q_BLOCK, (i + 1) * Q_BLOCK
        dist = (jnp.arange(lo, hi)[:, None] - jnp.arange(hi)[None, :]).astype(jnp.float32)
        bias = -slopes[:, None, None] * dist
        s = jnp.einsum('bhcqd,bhckd->bhcqk', qh[:, :, :, lo:hi], kh[:, :, :, :hi]).astype(jnp.float32) * scale
        s = jnp.where(dist >= 0, s + bias[None, :, None], -jnp.inf)
        p = jax.nn.softmax(s, axis=-1)
        w = p[:, :, 0] - lam * p[:, :, 1]
        outs.append(jnp.einsum('bhqk,bhkd->bhqd', w.astype(vh.dtype), vh[:, :, :hi]))
    o = jnp.concatenate(outs, axis=2)
    o = rms_norm(o, subln_g) * (1.0 - lambda_init)
    return o.transpose(0, 2, 1, 3).reshape(bsz, seq, -1)


def stick_breaking_attention(q, k, v):
    bsz, seq, _, dh = q.shape
    scale = dh ** -0.5
    qh, kh, vh = (t.transpose(0, 2, 1, 3) for t in (q, k, v))
    outs = []
    for i in range(seq // Q_BLOCK):
        lo, hi = i * Q_BLOCK, (i + 1) * Q_BLOCK
        mask = jnp.arange(hi)[None, :] < jnp.arange(lo, hi)[:, None]
        z = jnp.einsum('bhqd,bhkd->bhqk', qh[:, :, lo:hi], kh[:, :, :hi]).astype(jnp.float32) * scale
        log_1mb = jnp.where(mask, jax.nn.log_sigmoid(-z), 0.0)
        after = lax.cumsum(log_1mb, axis=3, reverse=True) - log_1mb
        w = jnp.where(mask, jnp.exp(jax.nn.log_sigmoid(z) + after), 0.0)
        outs.append(jnp.einsum('bhqk,bhkd->bhqd', w.astype(vh.dtype), vh[:, :, :hi]))
    o = jnp.concatenate(outs, axis=2)
    return o.transpose(0, 2, 1, 3).reshape(bsz, seq, -1)


def moba_attention(q, k, v, slopes):
    bsz, seq, nh, dh = q.shape
    scale = dh ** -0.5
    n_kb = -(-seq // MOBA_BLOCK)
    pad = n_kb * MOBA_BLOCK - seq
    n_sel = min(MOBA_TOPK, n_kb - 1)
    qh, kh, vh = (t.transpose(0, 2, 1, 3) for t in (q, k, v))
    kp = jnp.pad(kh, ((0, 0), (0, 0), (0, pad), (0, 0)))
    vp = jnp.pad(vh, ((0, 0), (0, 0), (0, pad), (0, 0)))
    kb = kp.reshape(bsz, nh, n_kb, MOBA_BLOCK, dh)
    vb = vp.reshape(bsz, nh, n_kb, MOBA_BLOCK, dh)
    k_mean = jnp.mean(kb.astype(jnp.float32), axis=3).astype(q.dtype)
    bi = jnp.arange(bsz)[:, None, None, None]
    hi = jnp.arange(nh)[None, :, None, None]
    sl4 = slopes[None, :, None, None]

    def chunk(j):
        t0 = j * MOBA_QCHUNK
        blk = t0 // MOBA_BLOCK
        tq = t0 + jnp.arange(MOBA_QCHUNK)
        qc = lax.dynamic_slice_in_dim(qh, t0, MOBA_QCHUNK, axis=2)
        b0 = blk * MOBA_BLOCK
        k_own = lax.dynamic_slice_in_dim(kp, b0, MOBA_BLOCK, axis=2)
        v_own = lax.dynamic_slice_in_dim(vp, b0, MOBA_BLOCK, axis=2)
        dist_own = (tq[:, None] - (b0 + jnp.arange(MOBA_BLOCK))[None, :]).astype(jnp.float32)
        s_own = jnp.einsum('bhqd,bhkd->bhqk', qc, k_own).astype(jnp.float32) * scale - sl4 * dist_own
        s_own = jnp.where(dist_own >= 0, s_own, -jnp.inf)
        if n_sel == 0:
            p = jax.nn.softmax(s_own, axis=-1)
            return jnp.einsum('bhqk,bhkd->bhqd', p.astype(v.dtype), v_own)
        gate = jnp.einsum('bhqd,bhnd->bhqn', qc, k_mean).astype(jnp.float32)
        gate = jnp.where(jnp.arange(n_kb) < blk, gate, -jnp.inf)
        _, sel = lax.top_k(gate, n_sel)
        sel_ok = jnp.arange(n_sel) < blk
        k_sel = kb[bi, hi, sel]
        v_sel = vb[bi, hi, sel]
        pos_sel = sel[..., None] * MOBA_BLOCK + jnp.arange(MOBA_BLOCK)
        dist_sel = (tq[None, None, :, None, None] - pos_sel).astype(jnp.float32)
        s_sel = jnp.einsum('bhqd,bhqnkd->bhqnk', qc, k_sel).astype(jnp.float32) * scale - slopes[None, :, None, None, None] * dist_sel
        s_sel = jnp.where(sel_ok[:, None], s_sel, -jnp.inf).reshape(bsz, nh, MOBA_QCHUNK, n_sel * MOBA_BLOCK)
        p = jax.nn.softmax(jnp.concatenate([s_sel, s_own], axis=-1), axis=-1)
        p_sel = p[..., :n_sel * MOBA_BLOCK].reshape(bsz, nh, MOBA_QCHUNK, n_sel, MOBA_BLOCK)
        p_own = p[..., n_sel * MOBA_BLOCK:]
        return (jnp.einsum('bhqnk,bhqnkd->bhqd', p_sel.astype(v.dtype), v_sel)
                + jnp.einsum('bhqk,bhkd->bhqd', p_own.astype(v.dtype), v_own))

    outs = lax.map(chunk, jnp.arange(seq // MOBA_QCHUNK))
    o = outs.transpose(1, 2, 0, 3, 4).reshape(bsz, nh, seq, dh)
    return o.transpose(0, 2, 1, 3).reshape(bsz, seq, nh * dh)


def dsa_attention(q, k, v, q_idx, k_idx, w_idx, slopes):
    bsz, seq, nh, dh = q.shape
    scale = dh ** -0.5
    n_keep = min(DSA_TOPK, seq // 4)
    bi = jnp.arange(bsz)[:, None, None]

    def block(i):
        t0 = i * Q_BLOCK
        tq = t0 + jnp.arange(Q_BLOCK)
        qc = lax.dynamic_slice_in_dim(q, t0, Q_BLOCK, axis=1)
        qic = lax.dynamic_slice_in_dim(q_idx, t0, Q_BLOCK, axis=1)
        wc = lax.dynamic_slice_in_dim(w_idx, t0, Q_BLOCK, axis=1)
        logits = jnp.einsum('bqhd,bsd->bqhs', qic, k_idx).astype(jnp.float32) * IDX_DH ** -0.5
        score = jnp.einsum('bqhs,bqh->bqs', jax.nn.relu(logits), wc.astype(jnp.float32) * IDX_HEADS ** -0.5)
        score = jnp.where(jnp.arange(seq)[None, :] <= tq[:, None], score, -jnp.inf)
        _, idx = lax.top_k(score, n_keep)
        k_g = k[bi, idx]
        v_g = v[bi, idx]
        dist = (tq[None, :, None] - idx).astype(jnp.float32)[:, None]
        s = jnp.einsum('bqhd,bqkd->bhqk', qc, k_g).astype(jnp.float32) * scale - slopes[None, :, None, None] * dist
        s = jnp.where(dist >= 0, s, -jnp.inf)
        p = jax.nn.softmax(s, axis=-1)
        return jnp.einsum('bhqk,bqkd->bqhd', p.astype(v.dtype), v_g)

    outs = lax.map(block, jnp.arange(seq // Q_BLOCK))
    return outs.transpose(1, 0, 2, 3, 4).reshape(bsz, seq, nh * dh)


def token_mixer(h, l, w_in, gate_b, a_qn_g, a_kn_g, a_lam_q1, a_lam_k1, a_lam_q2, a_lam_k2,
                a_subln_g, c_qn_g, c_kn_g, d_qn_g, d_kn_g, w_branch, w_out, slopes):
    bsz, seq, _ = h.shape
    f32 = jnp.float32
    proj = h @ w_in[l]
    splits = [int(s) for s in np.cumsum(IN_SIZES)[:-1]]
    (aq, ak, av, bq, bk, bv, cq, ck, cv, dq, dk, dv, iq, ik, iw, gl) = jnp.split(proj, splits, axis=-1)
    s_a, s_c, s_d = slopes[0::3], slopes[1::3], slopes[2::3]
    lambda_init = 0.8 - 0.6 * math.exp(-0.3 * l)
    lam = (jnp.exp(jnp.sum(a_lam_q1[l].astype(f32) * a_lam_k1[l].astype(f32)))
           - jnp.exp(jnp.sum(a_lam_q2[l].astype(f32) * a_lam_k2[l].astype(f32))) + lambda_init)
    aq = rms_norm(aq.reshape(bsz, seq, A_HEADS, 2, A_DH), a_qn_g[l])
    ak = rms_norm(ak.reshape(bsz, seq, A_HEADS, 2, A_DH), a_kn_g[l])
    oa = diff_attention(aq, ak, av.reshape(bsz, seq, A_HEADS, 2 * A_DH), s_a, lam, lambda_init, a_subln_g[l])
    ob = stick_breaking_attention(bq.reshape(bsz, seq, B_HEADS, B_DH), bk.reshape(bsz, seq, B_HEADS, B_DH),
                                  bv.reshape(bsz, seq, B_HEADS, B_DH))
    cq = rms_norm(cq.reshape(bsz, seq, C_HEADS, C_DH), c_qn_g[l])
    ck = rms_norm(ck.reshape(bsz, seq, C_HEADS, C_DH), c_kn_g[l])
    oc = moba_attention(cq, ck, cv.reshape(bsz, seq, C_HEADS, C_DH), s_c)
    dq = rms_norm(dq.reshape(bsz, seq, D_HEADS, D_DH), d_qn_g[l])
    dk = rms_norm(dk, d_kn_g[l])
    od = dsa_attention(dq, dk, dv, iq.reshape(bsz, seq, IDX_HEADS, IDX_DH), ik, iw, s_d)
    branches = jnp.stack([oa, ob, oc, od], axis=2)
    up = jnp.einsum('bsnw,nwd->bsnd', branches, w_branch[l])
    gates = jax.nn.sigmoid(gl + gate_b[l]).reshape(bsz, seq, N_BRANCH, D_MODEL)
    y = jnp.sum(gates * up, axis=2)
    return y @ w_out[l]


def moe_ffn(h, l, router_w, router_b, w1, b1, w2, b2):
    bsz, seq, dm = h.shape
    xt = h.reshape(-1, dm)
    n_tok = xt.shape[0]
    logits = (xt @ router_w[l]).astype(jnp.float32) + router_b[l].astype(jnp.float32)
    top_val, top_idx = lax.top_k(logits, TOP_K)
    gate = jax.nn.softmax(top_val, axis=-1).astype(xt.dtype)
    n_assign = n_tok * TOP_K
    e_flat = top_idx.reshape(-1)
    tok_flat = jnp.arange(n_assign) // TOP_K
    g_flat = gate.reshape(-1)
    order = jnp.argsort(e_flat)
    e_sorted = e_flat[order]
    counts = jnp.zeros((N_EXPERTS,), jnp.int32).at[e_flat].add(1)
    padded = (counts + MOE_BLOCK - 1) // MOE_BLOCK * MOE_BLOCK
    start = jnp.cumsum(counts) - counts
    pend = jnp.cumsum(padded)
    pstart = pend - padded
    dest = pstart[e_sorted] + (jnp.arange(n_assign) - start[e_sorted])
    n_blocks = -(-(n_assign + N_EXPERTS * (MOE_BLOCK - 1)) // MOE_BLOCK)
    n_rows = n_blocks * MOE_BLOCK
    row_tok = jnp.zeros((n_rows,), jnp.int32).at[dest].set(tok_flat[order])
    row_gate = jnp.zeros((n_rows,), xt.dtype).at[dest].set(g_flat[order])
    block_start = jnp.arange(n_blocks) * MOE_BLOCK
    block_exp = jnp.minimum(jnp.sum(pend[None, :] <= block_start[:, None], axis=1), N_EXPERTS - 1)

    def expert_block(args):
        e, toks, g = args
        hb = xt[toks] @ w1[l, e] + b1[l, e]
        x_glu = jnp.minimum(hb[:, :D_FF], SWIGLU_LIMIT)
        x_lin = jnp.clip(hb[:, D_FF:], -SWIGLU_LIMIT, SWIGLU_LIMIT)
        act = x_glu * jax.nn.sigmoid(SWIGLU_ALPHA * x_glu) * (x_lin + 1.0)
        return (act @ w2[l, e] + b2[l, e]) * g[:, None]

    y_rows = lax.map(expert_block, (block_exp, row_tok.reshape(n_blocks, MOE_BLOCK),
                                    row_gate.reshape(n_blocks, MOE_BLOCK)))
    out = jnp.zeros_like(xt).at[row_tok].add(y_rows.reshape(n_rows, dm))
    return out.reshape(bsz, seq, dm)


def setup_inputs(seed: int = 0) -> dict:
    key = jax.random.key(seed)
    ks = jax.random.split(key, 32)
    L, D = DEPTH, D_MODEL

    def nrm(k, shape, scale):
        return jax.random.normal(k, shape, jnp.float32) * scale

    def gain(k, shape):
        return 1.0 + 0.1 * jax.random.normal(k, shape, jnp.float32)

    return {
        'x': nrm(ks[0], (BATCH, SEQ, D), 1.0),
        'c': nrm(ks[1], (BATCH, D), 1.0),
        'ada_w': nrm(ks[2], (L, D, 6 * D), 0.5 * D ** -0.5),
        'ada_b': nrm(ks[3], (L, 6 * D), 0.02),
        'norm1_g': gain(ks[4], (L, D)),
        'norm2_g': gain(ks[5], (L, D)),
        'w_in': nrm(ks[6], (L, D, N_IN), D ** -0.5),
        'gate_b': nrm(ks[7], (L, N_BRANCH * D), 0.1),
        'a_qn_g': gain(ks[8], (L, A_DH)),
        'a_kn_g': gain(ks[9], (L, A_DH)),
        'a_lam_q1': nrm(ks[10], (L, A_DH), 0.1),
        'a_lam_k1': nrm(ks[11], (L, A_DH), 0.1),
        'a_lam_q2': nrm(ks[12], (L, A_DH), 0.1),
        'a_lam_k2': nrm(ks[13], (L, A_DH), 0.1),
        'a_subln_g': gain(ks[14], (L, 2 * A_DH)),
        'c_qn_g': gain(ks[15], (L, C_DH)),
        'c_kn_g': gain(ks[16], (L, C_DH)),
        'd_qn_g': gain(ks[17], (L, D_DH)),
        'd_kn_g': gain(ks[18], (L, D_DH)),
        'w_branch': nrm(ks[19], (L, N_BRANCH, BRANCH_W, D), BRANCH_W ** -0.5),
        'w_out': nrm(ks[20], (L, D, D), D ** -0.5),
        'router_w': nrm(ks[21], (L, D, N_EXPERTS), D ** -0.5),
        'router_b': nrm(ks[22], (L, N_EXPERTS), 0.01),
        'w1': nrm(ks[23], (L, N_EXPERTS, D, 2 * D_FF), D ** -0.5),
        'b1': nrm(ks[24], (L, N_EXPERTS, 2 * D_FF), 0.02),
        'w2': nrm(ks[25], (L, N_EXPERTS, D_FF, D), D_FF ** -0.5),
        'b2': nrm(ks[26], (L, N_EXPERTS, D), 0.02),
    }


def reference(x, c, ada_w, ada_b, norm1_g, norm2_g, w_in, gate_b, a_qn_g, a_kn_g, a_lam_q1, a_lam_k1,
              a_lam_q2, a_lam_k2, a_subln_g, c_qn_g, c_kn_g, d_qn_g, d_kn_g, w_branch, w_out,
              router_w, router_b, w1, b1, w2, b2):
    slopes = alibi_slopes()
    cond = jax.nn.silu(c)
    for l in range(DEPTH):
        mod = cond @ ada_w[l] + ada_b[l]
        sh1, sc1, g1, sh2, sc2, g2 = jnp.split(mod[:, None, :], 6, axis=-1)
        h = rms_norm(x, norm1_g[l]) * (1.0 + sc1) + sh1
        x = x + g1 * token_mixer(h, l, w_in, gate_b, a_qn_g, a_kn_g, a_lam_q1, a_lam_k1, a_lam_q2, a_lam_k2,
                                 a_subln_g, c_qn_g, c_kn_g, d_qn_g, d_kn_g, w_branch, w_out, slopes)
        h = rms_norm(x, norm2_g[l]) * (1.0 + sc2) + sh2
        x = x + g2 * moe_ffn(h, l, router_w, router_b, w1, b1, w2, b2)
    return x
```

```python
import math
import numpy as np
import ml_dtypes
from contextlib import ExitStack
import concourse.bass as bass
import concourse.mybir as mybir
from concourse.bass_utils import run_bass_kernel_spmd


ENGS = ("pe", "act", "dve", "pool", "sp")


class _Rec:
    def __init__(self):
        self.call = None

    def __getattr__(self, name):
        def f(*a, **k):
            self.call = (name, a, k)
            return self
        return f


class Sched:
    def __init__(self, nc, n_dma_sems=16):
        self.nc = nc
        self.streams = {e: [] for e in ENGS}
        self.cnt = {e: 0 for e in ENGS}
        self.last_w = {}
        self.readers = {}
        self.waited = {e: {} for e in ENGS}
        self.n_dma = n_dma_sems
        self.dma_next = 0
        self.dma_val = [0] * n_dma_sems
        self.n_ops = 0
        self.n_waits = 0

    def _need(self, eng, ev):
        key, val = ev
        if self.waited[eng].get(key, 0) >= val:
            return
        self.waited[eng][key] = val
        self.streams[eng].append(("wait", key, val))
        self.n_waits += 1

    def _deps(self, eng, reads, writes):
        for r in reads:
            ev = self.last_w.get(r)
            if ev is not None:
                self._dep1(eng, ev)
        for w in writes:
            ev = self.last_w.get(w)
            if ev is not None:
                self._dep1(eng, ev)
            for k, v in self.readers.get(w, {}).items():
                self._dep1(eng, (k, v))

    def _dep1(self, eng, ev):
        if eng == "pe" and ev[0] == "pe":
            return
        self._need(eng, ev)

    def _commit(self, ev, reads, writes):
        for r in reads:
            d = self.readers.setdefault(r, {})
            if d.get(ev[0], 0) < ev[1]:
                d[ev[0]] = ev[1]
        for w in writes:
            self.last_w[w] = ev
            self.readers[w] = {}

    def op(self, eng, fn, reads=(), writes=()):
        self._deps(eng, reads, writes)
        self.cnt[eng] += 1
        ev = (eng, self.cnt[eng])
        rec = _Rec(); fn(rec)
        self.streams[eng].append(("op", rec.call, eng, 1))
        self._commit(ev, reads, writes)
        self.n_ops += 1
        return ev

    def dma(self, q, fn, reads=(), writes=()):
        self._deps(q, reads, writes)
        j = self.dma_next
        self.dma_next = (j + 1) % self.n_dma
        key = ("dma", j)
        if self.dma_val[j] > 0:
            self._need(q, (key, self.dma_val[j]))
        self.dma_val[j] += 16
        ev = (key, self.dma_val[j])
        rec = _Rec(); fn(rec)
        self.streams[q].append(("op", rec.call, key, 16))
        self._commit(ev, reads, writes)
        self.n_ops += 1
        return ev

    def finish(self, out_res):
        for r in out_res:
            ev = self.last_w.get(r)
            if ev is not None:
                self._need("sp", ev)

    def emit(self):
        nc = self.nc
        with ExitStack() as st:
            sems = {}
            for e in ENGS:
                sems[e] = st.enter_context(nc.semaphore("s_" + e))
            for j in range(self.n_dma):
                sems[("dma", j)] = st.enter_context(nc.semaphore("s_dma%d" % j))
            block = st.enter_context(nc.Block())

            regcache = {}

            def run(eng_obj, stream):
                for it in stream:
                    if it[0] == "wait":
                        eng_obj.wait_ge(sems[it[1]], it[2])
                    else:
                        name, a, k = it[1]
                        if name == "indirect_dma_start" and isinstance(k.get("bounds_check"), int):
                            v = k["bounds_check"]
                            if v not in regcache:
                                regcache[v] = eng_obj.to_reg(v)
                            k = dict(k); k["bounds_check"] = regcache[v]
                        try:
                            ins = getattr(eng_obj, name)(*a, **k)
                        except Exception as ex_:
                            import traceback
                            traceback.print_exception(ex_)
                            print("CAUSE", repr(ex_.__cause__), repr(ex_.__context__))
                            print("FAILED OP:", name, [str(x)[:200] for x in a], {kk: str(v)[:200] for kk, v in k.items()})
                            raise
                        ins.then_inc(sems[it[2]], it[3])

            @block.tensor
            def _(e):
                run(e, self.streams["pe"])

            @block.scalar
            def _(e):
                run(e, self.streams["act"])

            @block.vector
            def _(e):
                run(e, self.streams["dve"])

            @block.gpsimd
            def _(e):
                run(e, self.streams["pool"])

            @block.sync
            def _(e):
                run(e, self.streams["sp"])


F32 = mybir.dt.float32; BF16 = mybir.dt.bfloat16
AF = mybir.ActivationFunctionType; ALU = mybir.AluOpType
D = 2048


def build_l0(L=2, NCOL=1536):
    nc = bass.Bass("TRN2", target_bir_lowering=False)
    cT = nc.dram_tensor("cT", [D, 4], F32, kind="ExternalInput").ap()
    w = nc.dram_tensor("w", [L, D, NCOL], F32, kind="ExternalInput").ap()
    bia = nc.dram_tensor("b", [L, NCOL], F32, kind="ExternalInput").ap()
    out = nc.dram_tensor("mod", [L, 4, NCOL], F32, kind="ExternalOutput").ap()
    S = Sched(nc)
    with ExitStack() as st:
        def sb(name, shape, dt): return st.enter_context(nc.sbuf_tensor(name, shape, dt))
        def ps(name, shape, dt): return st.enter_context(nc.psum_tensor(name, shape, dt))
        cond = sb("cond", [128, 16, 4], F32)
        ones = sb("ones", [1, 4], F32)
        wt = [sb("wt%d" % i, [128, 16, 512], F32) for i in range(2)]
        bt = [sb("bt%d" % i, [1, 512], F32) for i in range(2)]
        ot = [sb("ot%d" % i, [4, 512], F32) for i in range(2)]
        pt = [ps("pt%d" % i, [4, 512], F32) for i in range(2)]
        S.dma("sp", lambda e: e.dma_start(out=cond[:], in_=cT.rearrange("(k p) b -> p k b", p=128)), writes=["cond"])
        S.op("act", lambda e: e.activation(out=cond[:], in_=cond[:], func=AF.Silu), reads=["cond"], writes=["cond"])
        S.op("dve", lambda e: e.memset(ones[:], 1.0), writes=["ones"])
        it = 0
        for l in range(L):
            for cch in range(NCOL // 512):
                s = it % 2; it += 1
                S.dma("sp", lambda e, s=s, l=l, cch=cch: e.dma_start(out=wt[s][:], in_=w[l, :, cch * 512:(cch + 1) * 512].rearrange("(k p) n -> p k n", p=128)), writes=["wt%d" % s])
                S.dma("sp", lambda e, s=s, l=l, cch=cch: e.dma_start(out=bt[s][:], in_=bia[l:l + 1, cch * 512:(cch + 1) * 512]), writes=["bt%d" % s])
                for k in range(16):
                    S.op("pe", lambda e, s=s, k=k: e.matmul(pt[s][:], cond[:, k, :], wt[s][:, k, :], start=(k == 0), stop=False),
                         reads=["cond", "wt%d" % s], writes=["pt%d" % s])
                S.op("pe", lambda e, s=s: e.matmul(pt[s][:], ones[:], bt[s][:], start=False, stop=True),
                     reads=["ones", "bt%d" % s], writes=["pt%d" % s])
                S.op("dve", lambda e, s=s: e.tensor_copy(ot[s][:], pt[s][:]), reads=["pt%d" % s], writes=["ot%d" % s])
                S.dma("sp", lambda e, s=s, l=l, cch=cch: e.dma_start(out=out[l, :, cch * 512:(cch + 1) * 512], in_=ot[s][:]), reads=["ot%d" % s], writes=["OUT"])
        S.finish(["OUT"])
        S.emit()
    return nc


F32 = mybir.dt.float32; BF16 = mybir.dt.bfloat16; I32 = mybir.dt.int32
AF = mybir.ActivationFunctionType; ALU = mybir.AluOpType
D = 2048; FF = 2048


def e2_consts():
    c = np.zeros((128, 384), np.float32)
    pp, p = np.arange(128)[:, None], np.arange(128)[None, :]
    c[:, 0:128] = (pp < p)
    c[:, 128:256] = 1.0
    c[:, 256:384] = np.eye(128)
    return c.astype(ml_dtypes.bfloat16)


def build_e2(ntok=8192, nexp=4, CAP=3072):
    nc = bass.Bass("TRN2", target_bir_lowering=False)
    NT = ntok // 128; NS = CAP // 1024
    h2 = nc.dram_tensor("h2", [ntok, D], BF16, kind="ExternalInput").ap()
    Gtok = nc.dram_tensor("Gtok", [ntok, nexp], F32, kind="ExternalInput").ap()
    w1 = nc.dram_tensor("w1", [nexp, D, 2 * FF], F32, kind="ExternalInput").ap()
    b1 = nc.dram_tensor("b1t_in", [128, nexp, 32], F32, kind="ExternalInput").ap()
    w2 = nc.dram_tensor("w2", [nexp, FF, D], F32, kind="ExternalInput").ap()
    b2 = nc.dram_tensor("b2", [nexp, D], F32, kind="ExternalInput").ap()
    cst = nc.dram_tensor("cst", [128, 384], BF16, kind="ExternalInput").ap()
    y = nc.dram_tensor("y", [ntok, D], BF16, kind="ExternalOutput").ap()
    Xc = [nc.dram_tensor("Xc%d" % i, [CAP, D], BF16, kind="Internal").ap() for i in range(nexp)]
    Yc = [nc.dram_tensor("Yc%d" % i, [CAP, D], BF16, kind="Internal").ap() for i in range(nexp)]
    S = Sched(nc)
    uid = [0]; RN = {}; KEEP = []

    def barrier():
        for e in ("pe", "act", "dve", "pool", "sp"):
            for e2 in ("pe", "act", "dve", "pool", "sp"):
                if e != e2 and S.cnt[e2] > 0:
                    S._need(e, (e2, S.cnt[e2]))
            for j in range(S.n_dma):
                if S.dma_val[j] > 0:
                    S._need(e, (("dma", j), S.dma_val[j]))

    class Scope:
        def __init__(self): self.st = ExitStack()
        def __enter__(self): self.st.__enter__(); return self
        def __exit__(self, *a):
            barrier(); return self.st.__exit__(*a)
        def sb(self, name, shape, dt):
            uid[0] += 1
            t = self.st.enter_context(nc.sbuf_tensor("%s_%d" % (name, uid[0]), shape, dt)); RN[id(t)] = "%s_%d" % (name, uid[0]); KEEP.append(t); return t
        def ps(self, name, shape, dt=F32):
            uid[0] += 1
            t = self.st.enter_context(nc.psum_tensor("%s_%d" % (name, uid[0]), shape, dt)); RN[id(t)] = "%s_%d" % (name, uid[0]); KEEP.append(t); return t

    def R(*ts): return [t if isinstance(t, str) else RN[id(t)] for t in ts]
    NC4 = NT * nexp
    with Scope() as G0:
        cb = G0.sb("cb", [128, 384], BF16)
        Gt = G0.sb("Gt", [128, NT, nexp], F32)
        slot = G0.sb("slot", [128, NC4], I32)
        b1t = G0.sb("b1t", [128, nexp, 32], F32); b1p = G0.sb("b1p", [128, nexp, 32], F32)
        S.dma("sp", lambda e: e.dma_start(out=cb[:], in_=cst), writes=R(cb))
        S.dma("sp", lambda e: e.dma_start(out=Gt[:], in_=Gtok.rearrange("(t p) e -> p t e", p=128)), writes=R(Gt))
        S.dma("sp", lambda e: e.dma_start(out=b1t[:], in_=b1), writes=R(b1t))
        S.op("dve", lambda e: e.tensor_scalar(b1p[:], b1t[:], 1.0, None, op0=ALU.add), reads=R(b1t), writes=R(b1p))
        triu = cb[:, 0:128]; onesb = cb[:, 128:256]; idb = cb[:, 256:384]
        Gf = Gt[:].rearrange("p t e -> p (t e)")
        with Scope() as PA:
            mask = PA.sb("mask", [128, NC4], BF16); maskf = PA.sb("maskf", [128, NC4], F32)
            sa = PA.sb("sa", [128, NC4], F32); sb_ = PA.sb("sb", [128, NC4], F32); posf = PA.sb("posf", [128, NC4], F32)
            pp1 = PA.ps("pp1", [128, NC4]); ptot = PA.ps("ptot", [128, NC4])
            S.op("dve", lambda e: e.tensor_scalar(maskf[:], Gf, 0.0, None, op0=ALU.is_gt), reads=R(Gt), writes=R(maskf))
            S.op("dve", lambda e: e.tensor_copy(mask[:], maskf[:]), reads=R(maskf), writes=R(mask))
            S.op("pe", lambda e: e.matmul(pp1[:], triu, mask[:], start=True, stop=True), reads=R(cb, mask), writes=R(pp1))
            S.op("pe", lambda e: e.matmul(ptot[:], onesb, mask[:], start=True, stop=True), reads=R(cb, mask), writes=R(ptot))
            S.op("dve", lambda e: e.tensor_copy(sa[:], ptot[:]), reads=R(ptot), writes=R(sa))
            cur, oth = sa, sb_
            s = 1
            while s < NT:
                w = s * nexp
                S.op("dve", lambda e, cur=cur, oth=oth, w=w: e.tensor_tensor(oth[:, w:NC4], cur[:, w:NC4], cur[:, 0:NC4 - w], op=ALU.add), reads=R(cur), writes=R(oth))
                S.op("dve", lambda e, cur=cur, oth=oth, w=w: e.tensor_copy(oth[:, 0:w], cur[:, 0:w]), reads=R(cur), writes=R(oth))
                cur, oth = oth, cur
                s *= 2
            S.op("dve", lambda e, cur=cur: e.tensor_tensor(posf[:], cur[:], ptot[:], op=ALU.subtract), reads=R(cur, ptot), writes=R(posf))
            S.op("dve", lambda e: e.tensor_tensor(posf[:], posf[:], pp1[:], op=ALU.add), reads=R(posf, pp1), writes=R(posf))
            S.op("dve", lambda e: e.tensor_scalar(maskf[:], maskf[:], -1.0e6, 1.0e6, op0=ALU.mult, op1=ALU.add), reads=R(maskf), writes=R(maskf))
            S.op("dve", lambda e: e.tensor_tensor(posf[:], posf[:], maskf[:], op=ALU.add), reads=R(posf, maskf), writes=R(posf))
            S.op("dve", lambda e: e.tensor_copy(slot[:], posf[:]), reads=R(posf), writes=R(slot))
        with Scope() as PB:
            xt = [PB.sb("xt", [128, D], BF16) for _ in range(4)]
            for t in range(NT):
                x_ = xt[t % 4]
                S.dma("sp", lambda e, x_=x_, t=t: e.dma_start(out=x_[:], in_=h2[t * 128:(t + 1) * 128, :]), writes=R(x_))
                for ex in range(nexp):
                    S.dma("pool", lambda e, x_=x_, t=t, ex=ex: e.indirect_dma_start(out=Xc[ex], out_offset=bass.IndirectOffsetOnAxis(ap=slot[:, t * nexp + ex:t * nexp + ex + 1], axis=0),
                                                                                   in_=x_[:], in_offset=None, bounds_check=CAP - 1, oob_is_err=False),
                          reads=R(x_, slot), writes=["Xc%d" % ex])
        with Scope() as PD:
            Xrs = [PD.sb("Xr", [128, 8, D], BF16) for _ in range(2)]
            xri = [0]
            XT = PD.sb("XT", [128, 16, 1024], BF16)
            actT = PD.sb("actT", [128, 16, 1024], BF16)
            Yrs = [PD.sb("Yr", [128, 8, 512], BF16) for _ in range(2)]
            wsl = [PD.sb("wsl", [128, 16, 512], BF16) for _ in range(2)]
            b2bc = PD.sb("b2bc", [128, D], F32)
            tmp = {n: [PD.sb(n, [128, 512], F32) for _ in range(2)] for n in ("xg", "sg", "xl")}
            pg = [PD.ps("pg", [128, 512]) for _ in range(2)]
            pl = [PD.ps("pl", [128, 512]) for _ in range(2)]
            py = [PD.ps("py", [128, 512]) for _ in range(2)]
            ptr = [PD.ps("ptr", [128, 512], BF16) for _ in range(2)]
            wi = 0; ti = 0; yi = 0; tri = 0
            units = [(ex, s_) for ex in range(nexp) for s_ in range(NS)]

            def load_x(u):
                ex, s_ = units[u]
                r0 = s_ * 1024
                Xr = Xrs[u % 2]
                S.dma("sp", lambda e: e.dma_start(out=Xr[:], in_=Xc[ex][r0:r0 + 1024, :].rearrange("(r p) f -> p r f", p=128)), reads=["Xc%d" % ex], writes=R(Xr))

            def prep(u):
                Xr = Xrs[u % 2]
                for fc in range(16):
                    for half in range(2):
                        pt_ = ptr[(fc * 2 + half) % 2]
                        for r4 in range(4):
                            rt = half * 4 + r4
                            S.op("pe", lambda e: e.transpose(pt_[:, r4 * 128:(r4 + 1) * 128], Xr[:, rt, fc * 128:(fc + 1) * 128], idb), reads=R(Xr, cb), writes=R(pt_))
                        if (fc + half) % 2 == 0:
                            S.op("act", lambda e: e.copy(XT[:, fc, half * 512:(half + 1) * 512], pt_[:]), reads=R(pt_), writes=R(XT))
                        else:
                            S.op("dve", lambda e: e.tensor_copy(XT[:, fc, half * 512:(half + 1) * 512], pt_[:]), reads=R(pt_), writes=R(XT))
            load_x(0)
            prep(0)
            if len(units) > 1:
                load_x(1)
            for u, (ex, s_) in enumerate(units):
                r0 = s_ * 1024
                if s_ == 0:
                    S.dma("sp", lambda e, ex=ex: e.dma_start(out=b2bc[:], in_=b2[ex:ex + 1, :].partition_broadcast(128)), writes=R(b2bc))
                for sl in range(8):
                    ws = wsl[wi % 2]; wi += 1
                    c0 = sl * 256
                    S.dma("pool", lambda e, ws=ws, ex=ex, c0=c0: e.dma_start(out=ws[:, :, 0:256], in_=w1[ex, :, c0:c0 + 256].rearrange("(k p) n -> p k n", p=128)), writes=R(ws))
                    S.dma("pool", lambda e, ws=ws, ex=ex, c0=c0: e.dma_start(out=ws[:, :, 256:512], in_=w1[ex, :, FF + c0:FF + c0 + 256].rearrange("(k p) n -> p k n", p=128)), writes=R(ws))
                    for sub in range(2):
                        j = sl * 2 + sub
                        for tt in range(2):
                            b = ti % 2; ti += 1
                            for k in range(16):
                                S.op("pe", lambda e, b=b, ws=ws, k=k, sub=sub, tt=tt: e.matmul(pg[b][:], ws[:, k, sub * 128:(sub + 1) * 128], XT[:, k, tt * 512:(tt + 1) * 512], start=(k == 0), stop=(k == 15)),
                                     reads=R(ws, XT), writes=R(pg[b]))
                            for k in range(16):
                                S.op("pe", lambda e, b=b, ws=ws, k=k, sub=sub, tt=tt: e.matmul(pl[b][:], ws[:, k, 256 + sub * 128:256 + (sub + 1) * 128], XT[:, k, tt * 512:(tt + 1) * 512], start=(k == 0), stop=(k == 15)),
                                     reads=R(ws, XT), writes=R(pl[b]))
                            xg, sg, xl = tmp["xg"][b], tmp["sg"][b], tmp["xl"][b]
                            S.op("dve", lambda e, b=b, xg=xg, ex=ex, j=j: e.tensor_scalar(xg[:], pg[b][:], b1t[:, ex, j:j + 1], 7.0, op0=ALU.add, op1=ALU.min), reads=R(pg[b], b1t), writes=R(xg))
                            S.op("act", lambda e, xg=xg, sg=sg: e.activation(out=sg[:], in_=xg[:], func=AF.Sigmoid, scale=1.702), reads=R(xg), writes=R(sg))
                            S.op("act", lambda e, b=b, xl=xl, ex=ex, j=j: e.activation(out=xl[:], in_=pl[b][:], func=AF.Identity, bias=b1p[:, ex, 16 + j:17 + j]), reads=R(pl[b], b1p), writes=R(xl))
                            S.op("dve", lambda e, xl=xl: e.tensor_scalar(xl[:], xl[:], 8.0, -6.0, op0=ALU.min, op1=ALU.max), reads=R(xl), writes=R(xl))
                            S.op("dve", lambda e, xl=xl, xg=xg: e.tensor_tensor(xg[:], xg[:], xl[:], op=ALU.mult), reads=R(xl, xg), writes=R(xg))
                            S.op("dve", lambda e, sg=sg, xg=xg, j=j, tt=tt: e.tensor_tensor(actT[:, j, tt * 512:(tt + 1) * 512], sg[:], xg[:], op=ALU.mult), reads=R(sg, xg), writes=R(actT))
                if u + 1 < len(units):
                    prep(u + 1)
                    if u + 2 < len(units):
                        load_x(u + 2)
                for sl in range(4):
                    ws = wsl[wi % 2]; wi += 1
                    Yr = Yrs[sl % 2]
                    c0 = sl * 512
                    S.dma("pool", lambda e, ws=ws, ex=ex, c0=c0: e.dma_start(out=ws[:], in_=w2[ex, :, c0:c0 + 512].rearrange("(k p) n -> p k n", p=128)), writes=R(ws))
                    for jt in range(8):
                        b = yi % 2; yi += 1
                        for k in range(16):
                            S.op("pe", lambda e, b=b, ws=ws, k=k, jt=jt: e.matmul(py[b][:], actT[:, k, jt * 128:(jt + 1) * 128], ws[:, k, :], start=(k == 0), stop=(k == 15)),
                                 reads=R(ws, actT), writes=R(py[b]))
                        S.op("dve", lambda e, b=b, jt=jt, c0=c0, Yr=Yr: e.tensor_tensor(Yr[:, jt, :], py[b][:], b2bc[:, c0:c0 + 512], op=ALU.add), reads=R(py[b], b2bc), writes=R(Yr))
                    S.dma("sp", lambda e, ex=ex, r0=r0, c0=c0, Yr=Yr: e.dma_start(out=Yc[ex][r0:r0 + 1024, c0:c0 + 512].rearrange("(r p) f -> p r f", p=128), in_=Yr[:]), reads=R(Yr), writes=["Yc%d" % ex])
        with Scope() as PE_:
            ygt = PE_.sb("ygt", [128, 2 * nexp, D], BF16)
            RNX = {}
            class _V:
                def __init__(self, i, ex): self.i = i; self.ex = ex
                def __getitem__(self, k): return ygt[:, self.i * nexp + self.ex, :]
            yg = [[_V(i, ex) for ex in range(nexp)] for i in range(2)]
            for i in range(2):
                for ex in range(nexp):
                    RN[id(yg[i][ex])] = "yg_%d_%d" % (i, ex)
            acc = [PE_.sb("acc", [128, D], F32) for _ in range(2)]
            ob = [PE_.sb("ob", [128, D], BF16) for _ in range(2)]
            for i in range(2):
                for ex in range(nexp):
                    S.op("dve", lambda e, i=i, ex=ex: e.memset(yg[i][ex][:], 0.0), writes=R(yg[i][ex]))
            for t in range(NT):
                i = t % 2
                for ex in range(nexp):
                    S.dma("pool", lambda e, i=i, ex=ex, t=t: e.indirect_dma_start(out=yg[i][ex][:], out_offset=None, in_=Yc[ex],
                                                                                  in_offset=bass.IndirectOffsetOnAxis(ap=slot[:, t * nexp + ex:t * nexp + ex + 1], axis=0),
                                                                                  bounds_check=CAP - 1, oob_is_err=False),
                          reads=["Yc%d" % ex] + R(slot, yg[i][ex]), writes=R(yg[i][ex]))
                for ex in range(nexp):
                    if ex == 0:
                        S.op("act", lambda e, i=i, t=t: e.activation(out=acc[i][:], in_=yg[i][0][:], func=AF.Identity, scale=Gt[:, t, 0:1]), reads=R(yg[i][0], Gt), writes=R(acc[i]))
                    else:
                        last = (ex == nexp - 1)
                        dst = ob[i] if last else acc[i]
                        S.op("dve", lambda e, i=i, t=t, ex=ex, dst=dst: e.scalar_tensor_tensor(out=dst[:], in0=yg[i][ex][:], scalar=Gt[:, t, ex:ex + 1], in1=acc[i][:], op0=ALU.mult, op1=ALU.add),
                             reads=R(yg[i][ex], Gt, acc[i]), writes=R(dst))
                S.dma("sp", lambda e, i=i, t=t: e.dma_start(out=y[t * 128:(t + 1) * 128, :], in_=ob[i][:]), reads=R(ob[i]), writes=["OUT"])
        S.finish(["OUT"])
        S.emit()
    print("E2 ops", S.n_ops, "waits", S.n_waits)
    return nc


F32 = mybir.dt.float32; BF16 = mybir.dt.bfloat16
AF = mybir.ActivationFunctionType; ALU = mybir.AluOpType
D = 2048


def build_c2():
    nc = bass.Bass("TRN2", target_bir_lowering=False)
    P8 = nc.dram_tensor("P8", [8, 1024, D], BF16, kind="ExternalInput").ap()
    xm = nc.dram_tensor("xm_in", [1024, D], F32, kind="ExternalInput").ap()
    g2 = nc.dram_tensor("g2row", [1, D], F32, kind="ExternalInput").ap()
    xn = nc.dram_tensor("xn", [1024, D], F32, kind="ExternalOutput").ap()
    S = Sched(nc)
    with ExitStack() as st:
        def sb(name, shape, dt): return st.enter_context(nc.sbuf_tensor(name, shape, dt))
        g2t = sb("g2t", [128, D], F32)
        pt = [sb("pt%d" % i, [128, 8, D], BF16) for i in range(2)]
        xt = [sb("xt%d" % i, [128, D], F32) for i in range(2)]
        acc = [sb("acc%d" % i, [128, D], F32) for i in range(2)]
        acb = [sb("acb%d" % i, [128, D], F32) for i in range(2)]
        S.dma("sp", lambda e: e.dma_start(out=g2t[:], in_=g2.partition_broadcast(128)), writes=["g2t"])
        for k in range(8):
            s = k % 2
            S.dma("sp", lambda e, s=s, k=k: e.dma_start(out=pt[s][:], in_=P8[:, k * 128:(k + 1) * 128, :].rearrange("c p n -> p c n")), writes=["pt%d" % s])
            S.dma("sp", lambda e, s=s, k=k: e.dma_start(out=xt[s][:], in_=xm[k * 128:(k + 1) * 128, :]), writes=["xt%d" % s])
            S.op("dve", lambda e, s=s: e.tensor_tensor(acc[s][:], pt[s][:, 0, :], pt[s][:, 1, :], op=ALU.add), reads=["pt%d" % s], writes=["acc%d" % s])
            S.op("pool", lambda e, s=s: e.tensor_tensor(acb[s][:], pt[s][:, 4, :], pt[s][:, 5, :], op=ALU.add), reads=["pt%d" % s], writes=["acb%d" % s])
            for c in (2, 3):
                S.op("dve", lambda e, s=s, c=c: e.tensor_tensor(acc[s][:], acc[s][:], pt[s][:, c, :], op=ALU.add), reads=["pt%d" % s, "acc%d" % s], writes=["acc%d" % s])
            for c in (6, 7):
                S.op("pool", lambda e, s=s, c=c: e.tensor_tensor(acb[s][:], acb[s][:], pt[s][:, c, :], op=ALU.add), reads=["pt%d" % s, "acb%d" % s], writes=["acb%d" % s])
            S.op("dve", lambda e, s=s: e.tensor_tensor(acc[s][:], acc[s][:], acb[s][:], op=ALU.add), reads=["acb%d" % s, "acc%d" % s], writes=["acc%d" % s])
            S.op("dve", lambda e, s=s: e.tensor_tensor(acc[s][:], acc[s][:], g2t[:], op=ALU.mult), reads=["acc%d" % s, "g2t"], writes=["acc%d" % s])
            S.op("dve", lambda e, s=s: e.tensor_tensor(xt[s][:], xt[s][:], acc[s][:], op=ALU.add), reads=["acc%d" % s, "xt%d" % s], writes=["xt%d" % s])
            S.dma("sp", lambda e, s=s, k=k: e.dma_start(out=xn[k * 128:(k + 1) * 128, :], in_=xt[s][:]), reads=["xt%d" % s], writes=["OUT"])
        S.finish(["OUT"])
        S.emit()
    return nc


F32 = mybir.dt.float32; BF16 = mybir.dt.bfloat16
AF = mybir.ActivationFunctionType; ALU = mybir.AluOpType; AX = mybir.AxisListType
D = 2048
NIN = 14152
EPS = 1e-6
TW = 256
SLOPES = [2.0 ** (-8.0 * (i + 1) / 12) for i in range(12)]
S_A, S_C, S_D = SLOPES[0::3], SLOPES[1::3], SLOPES[2::3]
C_AQ, C_AK, C_AV, C_BQ, C_BK, C_BV, C_CQ, C_CK, C_CV = 0, 512, 1024, 1536, 2048, 2560, 3072, 3584, 4096
C_DQ, C_DK, C_DV, C_IQ, C_IK, C_IW, C_GL = 4608, 5120, 5248, 5376, 5888, 5952, 5960
O_BF, O_BD, O_BM, O_TMA, O_MG, O_OWN, O_IDF, O_UNEG, O_ONEG, NCF = 0, 512, 4608, 8704, 8960, 9024, 9088, 9216, 9344, 9472
O_IDB, O_ONESB, O_BLK64, O_EALL, NCB = 0, 128, 256, 384, 1408
V_MOD, V_N1G, V_N2G, V_GB, V_AQG, V_AKG, V_CQG, V_CKG, V_DQG, V_DKG, V_SUBLN, V_LAM, V_LI, V_OML, NV = 0, 96, 112, 128, 192, 193, 194, 195, 196, 197, 198, 199, 203, 204, 208
NEG = -1.0e9


def m_consts(p):
    k = np.arange(128)[:, None].astype(np.float32); q = np.arange(128)[None, :].astype(np.float32)
    kq = k - q
    one = np.ones((128, 128), np.float32)
    cf = np.zeros((128, NCF), np.float32)
    cf[:, O_BF:O_BF + 512] = np.concatenate([kq - 128 * p - 256 * jj for jj in range(4)], axis=1)
    for r in range(8):
        for jj in range(4):
            d = r - 2 * jj - p
            if d < 0:
                bd = kq + 128 * d; bm = one
            elif d == 0:
                bd = np.where(k <= q, kq, NEG); bm = (k < q).astype(np.float32)
            else:
                bd = NEG * one; bm = 0 * one
            cf[:, O_BD + r * 512 + jj * 128:O_BD + r * 512 + (jj + 1) * 128] = bd
            cf[:, O_BM + r * 512 + jj * 128:O_BM + r * 512 + (jj + 1) * 128] = bm
    qq = np.arange(128)[:, None]; ss = np.arange(128)[None, :]
    tri = np.where(ss <= qq, 0.0, -1e30).astype(np.float32)
    blkA = np.zeros((128, 128), np.float32) if p == 1 else tri
    blkB = tri if p == 1 else np.full((128, 128), -1e30, np.float32)
    cf[:, O_TMA:O_TMA + 128] = blkA; cf[:, O_TMA + 128:O_TMA + 256] = blkB
    for j in range(8):
        for n in range(8):
            cf[:, O_MG + j * 8 + n] = 0.0 if n < j else -1e30
            cf[:, O_OWN + j * 8 + n] = 1.0 if n == j else 0.0
    cf[:, O_IDF:O_IDF + 128] = np.eye(128)
    jj_, s_ = np.arange(128)[:, None], np.arange(128)[None, :]
    cf[:, O_UNEG:O_UNEG + 128] = np.where(jj_ >= s_, -1.0, 0.0)
    cf[:, O_ONEG:O_ONEG + 128] = -1.0
    cb = np.zeros((128, NCB), np.float32)
    cb[:, O_IDB:O_IDB + 128] = np.eye(128)
    cb[:, O_ONESB:O_ONESB + 128] = 1.0
    cb[0:64, O_BLK64:O_BLK64 + 64] = 1.0; cb[64:128, O_BLK64 + 64:O_BLK64 + 128] = 1.0
    for n in range(8):
        cb[n, O_EALL + n * 128:O_EALL + (n + 1) * 128] = 1.0
    return cf, cb.astype(ml_dtypes.bfloat16)


def build_m(dbg=False, phases=(1, 2, 3, 4), mixers="ABCD"):
    nc = bass.Bass("TRN2", target_bir_lowering=False)
    def din(name, shape, dt=F32): return nc.dram_tensor(name, shape, dt, kind="ExternalInput").ap()
    def dout(name, shape, dt=F32): return nc.dram_tensor(name, shape, dt, kind="ExternalOutput").ap()
    def dscr(name, shape, dt=BF16): return nc.dram_tensor(name, shape, dt, kind="Internal").ap()
    xTa = din("xTa", [D, 2048]); xTo = din("xTo", [D, 1024])
    vecs_d = din("vecs", [128, NV]); cf_d = din("cf", [128, NCF]); cb_d = din("cb", [128, NCB], BF16)
    w_in = din("w_in", [D, NIN]); w_br = din("w_br", [4, 512, D]); w_out = din("w_out", [D, D])
    rw_d = din("rw", [128, 16, 32]); rb_d = din("rb", [1, 32])
    xmT = dout("xmT", [D, 1024]); h2T = dout("h2T", [D, 1024], BF16); G_o = dout("G", [1024, 32])
    QT = dscr("QT", [5, 512, 1024]); KT = dscr("KT", [4, 512, 2048]); Vd = dscr("Vd", [2048, 1664])
    IW = dscr("IW", [1024, 8], F32)
    hTo_d = dscr("hTo_d", [D, 1024])
    if dbg:
        BRT = dout("BRT", [D, 1024], BF16)
    else:
        BRT = dscr("BRT", [D, 1024])
    S = Sched(nc)
    uid = [0]; RN = {}; KEEP = []

    def barrier():
        for e in ("pe", "act", "dve", "pool", "sp"):
            for e2 in ("pe", "act", "dve", "pool", "sp"):
                if e != e2 and S.cnt[e2] > 0:
                    S._need(e, (e2, S.cnt[e2]))
            for j in range(S.n_dma):
                if S.dma_val[j] > 0:
                    S._need(e, (("dma", j), S.dma_val[j]))

    class Scope:
        def __init__(self): self.st = ExitStack()
        def __enter__(self): self.st.__enter__(); return self
        def __exit__(self, *a):
            barrier(); return self.st.__exit__(*a)
        def sb(self, name, shape, dt):
            uid[0] += 1
            t = self.st.enter_context(nc.sbuf_tensor("%s_%d" % (name, uid[0]), shape, dt)); RN[id(t)] = "%s_%d" % (name, uid[0]); KEEP.append(t); return t
        def ps(self, name, shape, dt=F32):
            uid[0] += 1
            t = self.st.enter_context(nc.psum_tensor("%s_%d" % (name, uid[0]), shape, dt)); RN[id(t)] = "%s_%d" % (name, uid[0]); KEEP.append(t); return t

    def R(*ts): return [t if isinstance(t, str) else RN[id(t)] for t in ts]

    with Scope() as G0:
        vecs = G0.sb("vecs", [128, NV], F32); cf = G0.sb("cf", [128, NCF], F32); cb = G0.sb("cb", [128, NCB], BF16)
        AB = G0.sb("AB", [128, 4, 16], F32)
        epsc = G0.sb("epsc", [128, 1], F32)
        S.op("dve", lambda e: e.memset(epsc[:], EPS), writes=R(epsc))
        S.dma("sp", lambda e: e.dma_start(out=vecs[:], in_=vecs_d), writes=R(vecs))
        S.dma("sp", lambda e: e.dma_start(out=cf[:], in_=cf_d), writes=R(cf))
        S.dma("sp", lambda e: e.dma_start(out=cb[:], in_=cb_d), writes=R(cb))
        mod = lambda i: vecs[:, V_MOD + 16 * i:V_MOD + 16 * (i + 1)]
        for (ai, sci, gi) in ((0, 1, V_N1G), (2, 4, V_N2G)):
            S.op("dve", lambda e, ai=ai, sci=sci: e.tensor_scalar(AB[:, ai, :], mod(sci), 1.0, None, op0=ALU.add), reads=R(vecs), writes=R(AB))
            S.op("dve", lambda e, ai=ai, gi=gi: e.tensor_tensor(AB[:, ai, :], AB[:, ai, :], vecs[:, gi:gi + 16], op=ALU.mult), reads=R(vecs, AB), writes=R(AB))
        onesb = cb[:, O_ONESB:O_ONESB + 128]; idb = cb[:, O_IDB:O_IDB + 128]; blk64 = cb[:, O_BLK64:O_BLK64 + 128]
        idf = cf[:, O_IDF:O_IDF + 128]

        def norm_tiles(sc, src, ntok, Acol, Bcol, consume):
            xs = [sc.sb("xs", [128, 16, TW], F32) for _ in range(2)]
            sq = sc.sb("sq", [128, 16, TW], BF16)
            rstd = sc.sb("rstd", [128, TW], F32)
            pss = sc.ps("pss", [128, TW])
            for tt in range(ntok // TW):
                x = xs[tt % 2]
                S.dma("sp", lambda e, x=x, tt=tt: e.dma_start(out=x[:], in_=src[:, tt * TW:(tt + 1) * TW].rearrange("(k p) n -> p k n", p=128)), writes=R(x))
                S.op("act", lambda e, x=x: e.activation(out=sq[:], in_=x[:], func=AF.Square), reads=R(x), writes=R(sq))
                for k in range(16):
                    S.op("pe", lambda e, k=k: e.matmul(pss[:], onesb, sq[:, k, :], start=(k == 0), stop=(k == 15)), reads=R(sq, cb), writes=R(pss))
                S.op("act", lambda e: e.activation(out=rstd[:], in_=pss[:], func=AF.Sqrt, bias=epsc[:, 0:1], scale=1.0 / D), reads=R(pss, epsc), writes=R(rstd))
                S.op("dve", lambda e: e.reciprocal(rstd[:], rstd[:]), reads=R(rstd), writes=R(rstd))
                for k in range(16):
                    S.op("dve", lambda e, x=x, k=k: e.tensor_tensor(x[:, k, :], x[:, k, :], rstd[:], op=ALU.mult), reads=R(x, rstd), writes=R(x))
                    S.op("act", lambda e, x=x, k=k: e.activation(out=x[:, k, :], in_=x[:, k, :], func=AF.Identity, bias=Bcol[:, k:k + 1], scale=Acol[:, k:k + 1]),
                         reads=R(x, AB, vecs), writes=R(x))
                consume(tt, x)

        if 1 in phases:
          with Scope() as P1:
            hTa = P1.sb("hTa", [128, 16, 2048], BF16)
            hTo = P1.sb("hTo", [128, 16, 1024], BF16)
            gq = P1.sb("gq", [128, 8], F32)
            S.op("dve", lambda e: e.tensor_scalar(gq[:, 0:1], vecs[:, V_AQG:V_AQG + 1], 64 ** -0.5, None, op0=ALU.mult), reads=R(vecs), writes=R(gq))
            S.op("dve", lambda e: e.tensor_copy(gq[:, 1:2], vecs[:, V_AKG:V_AKG + 1]), reads=R(vecs), writes=R(gq))
            S.op("dve", lambda e: e.tensor_scalar(gq[:, 2:3], vecs[:, V_CQG:V_CQG + 1], 128 ** -0.5, None, op0=ALU.mult), reads=R(vecs), writes=R(gq))
            S.op("dve", lambda e: e.tensor_copy(gq[:, 3:4], vecs[:, V_CKG:V_CKG + 1]), reads=R(vecs), writes=R(gq))
            S.op("dve", lambda e: e.tensor_scalar(gq[:, 4:5], vecs[:, V_DQG:V_DQG + 1], 128 ** -0.5, None, op0=ALU.mult), reads=R(vecs), writes=R(gq))
            S.op("dve", lambda e: e.tensor_copy(gq[:, 5:6], vecs[:, V_DKG:V_DKG + 1]), reads=R(vecs), writes=R(gq))
            with Scope() as N1:
                def c_all(tt, x):
                    S.op("dve", lambda e, tt=tt, x=x: e.tensor_copy(hTa[:, :, tt * TW:(tt + 1) * TW], x[:]), reads=R(x), writes=R(hTa))
                norm_tiles(N1, xTa, 2048, AB[:, 0, :], mod(0), c_all)
            with Scope() as N1:
                def c_own(tt, x):
                    S.op("dve", lambda e, tt=tt, x=x: e.tensor_copy(hTo[:, :, tt * TW:(tt + 1) * TW], x[:]), reads=R(x), writes=R(hTo))
                norm_tiles(N1, xTo, 1024, AB[:, 0, :], mod(0), c_own)
            S.dma("sp", lambda e: e.dma_start(out=hTo_d.rearrange("(k p) n -> p k n", p=128), in_=hTo[:]), reads=R(hTo), writes=["hTo_d"])
            with Scope() as PJ:
                wsl = [PJ.sb("wsl", [128, 16, 512], BF16) for _ in range(2)]
                stg = [PJ.sb("stg", [128, 512], BF16) for _ in range(3)]
                stgf = PJ.sb("stgf", [128, 8], F32)
                sqn = [PJ.sb("sqn", [128, 512], BF16) for _ in range(2)]
                rs = [PJ.sb("rs", [128, 512], F32) for _ in range(2)]
                tq = [PJ.sb("tq", [128, 512], F32) for _ in range(2)]
                pp = [PJ.ps("pp", [128, 512]) for _ in range(3)]
                pn = [PJ.ps("pn", [128, 512]) for _ in range(2)]
                cnt = {"w": 0, "p": 0, "s": 0, "n": 0}

                def load_slab(c0, ncols):
                    ws = wsl[cnt["w"] % 2]; cnt["w"] += 1
                    S.dma("pool", lambda e, ws=ws: e.dma_start(out=ws[:, :, 0:ncols], in_=w_in[:, c0:c0 + ncols].rearrange("(k p) n -> p k n", p=128)), writes=R(ws))
                    return ws

                def proj_fm(c0, ncols, src, ntok, dst_fn, normed, gcol, blkmat, dh, cscale=1.0, rows=128):
                    ws = load_slab(c0, ncols)
                    for sub in range(max(1, ncols // 128)):
                        for tt in range(ntok // 512):
                            p_ = pp[cnt["p"] % 3]; cnt["p"] += 1
                            sg_ = stg[cnt["s"] % 3]; cnt["s"] += 1
                            for k in range(16):
                                S.op("pe", lambda e, p_=p_, ws=ws, k=k, sub=sub, tt=tt: e.matmul(p_[0:rows, :], ws[:, k, sub * 128:sub * 128 + rows], src[:, k, tt * 512:(tt + 1) * 512], start=(k == 0), stop=(k == 15)),
                                     reads=R(ws, src), writes=R(p_))
                            if normed:
                                i = cnt["n"] % 2; cnt["n"] += 1
                                S.op("act", lambda e, p_=p_, i=i: e.activation(out=sqn[i][:], in_=p_[:], func=AF.Square), reads=R(p_), writes=R(sqn[i]))
                                S.op("pe", lambda e, i=i: e.matmul(pn[i][:], blkmat, sqn[i][:], start=True, stop=True), reads=R(sqn[i], cb), writes=R(pn[i]))
                                S.op("act", lambda e, i=i: e.activation(out=rs[i][:], in_=pn[i][:], func=AF.Sqrt, bias=epsc[:, 0:1], scale=1.0 / dh), reads=R(pn[i], epsc), writes=R(rs[i]))
                                S.op("dve", lambda e, i=i: e.reciprocal(rs[i][:], rs[i][:]), reads=R(rs[i]), writes=R(rs[i]))
                                S.op("dve", lambda e, i=i, p_=p_: e.tensor_tensor(tq[i][:], p_[:], rs[i][:], op=ALU.mult), reads=R(p_, rs[i]), writes=R(tq[i]))
                                S.op("act", lambda e, i=i, sg_=sg_: e.activation(out=sg_[:], in_=tq[i][:], func=AF.Identity, scale=gcol), reads=R(tq[i], gq), writes=R(sg_))
                            else:
                                S.op("act", lambda e, p_=p_, sg_=sg_: e.activation(out=sg_[0:rows, :], in_=p_[0:rows, :], func=AF.Identity, scale=cscale), reads=R(p_), writes=R(sg_))
                            dst, dres = dst_fn(sub, tt)
                            S.dma("sp", lambda e, dst=dst, sg_=sg_: e.dma_start(out=dst, in_=sg_[0:rows, :]), reads=R(sg_), writes=[dres])

                def qdst(m):
                    return lambda sub, tt: (QT[m, sub * 128:(sub + 1) * 128, tt * 512:(tt + 1) * 512], "QT%d" % m)

                def kdst(m, r0=0, rows=128):
                    return lambda sub, tt: (KT[m, r0 + sub * 128:r0 + sub * 128 + rows, tt * 512:(tt + 1) * 512], "KT%d" % m)
                proj_fm(C_AQ, 512, hTo, 1024, qdst(0), True, gq[:, 0:1], blk64, 64)
                proj_fm(C_BQ, 512, hTo, 1024, qdst(1), False, None, None, 0, cscale=128 ** -0.5)
                proj_fm(C_CQ, 512, hTo, 1024, qdst(2), True, gq[:, 2:3], onesb, 128)
                proj_fm(C_DQ, 512, hTo, 1024, qdst(3), True, gq[:, 4:5], onesb, 128)
                proj_fm(C_IQ, 512, hTo, 1024, qdst(4), False, None, None, 0)
                proj_fm(C_AK, 512, hTa, 2048, kdst(0), True, gq[:, 1:2], blk64, 64)
                proj_fm(C_BK, 512, hTa, 2048, kdst(1), False, None, None, 0)
                proj_fm(C_CK, 512, hTa, 2048, kdst(2), True, gq[:, 3:4], onesb, 128)
                proj_fm(C_DK, 128, hTa, 2048, kdst(3), True, gq[:, 5:6], onesb, 128)
                proj_fm(C_IK, 64, hTa, 2048, kdst(3, r0=128, rows=64), False, None, None, 0, rows=64)
                for (c0, ncols, v0) in ((C_AV, 512, 0), (C_BV, 512, 512), (C_CV, 512, 1024), (C_DV, 128, 1536)):
                    ws = load_slab(c0, ncols)
                    for t in range(16):
                        p_ = pp[cnt["p"] % 3]; cnt["p"] += 1
                        sg_ = stg[cnt["s"] % 3]; cnt["s"] += 1
                        for k in range(16):
                            S.op("pe", lambda e, p_=p_, ws=ws, k=k, t=t, ncols=ncols: e.matmul(p_[:, 0:ncols], hTa[:, k, t * 128:(t + 1) * 128], ws[:, k, 0:ncols], start=(k == 0), stop=(k == 15)),
                                 reads=R(ws, hTa), writes=R(p_))
                        S.op("act", lambda e, p_=p_, sg_=sg_, ncols=ncols: e.copy(sg_[:, 0:ncols], p_[:, 0:ncols]), reads=R(p_), writes=R(sg_))
                        S.dma("sp", lambda e, sg_=sg_, t=t, v0=v0, ncols=ncols: e.dma_start(out=Vd[t * 128:(t + 1) * 128, v0:v0 + ncols], in_=sg_[:, 0:ncols]), reads=R(sg_), writes=["Vd"])
                ws = load_slab(C_IW, 8)
                for t in range(8):
                    p_ = pp[cnt["p"] % 3]; cnt["p"] += 1
                    for k in range(16):
                        S.op("pe", lambda e, p_=p_, ws=ws, k=k, t=t: e.matmul(p_[:, 0:8], hTo[:, k, t * 128:(t + 1) * 128], ws[:, k, 0:8], start=(k == 0), stop=(k == 15)),
                             reads=R(ws, hTo), writes=R(p_))
                    S.op("act", lambda e, p_=p_: e.copy(stgf[:], p_[:, 0:8]), reads=R(p_), writes=R(stgf))
                    S.dma("sp", lambda e, t=t: e.dma_start(out=IW[t * 128:(t + 1) * 128, :], in_=stgf[:]), reads=R(stgf), writes=["IW"])

        if 2 in phases:
          with Scope() as P2:
            QTs = P2.sb("QTs", [128, 4, 1024], BF16)
            KTs = P2.sb("KTs", [128, 4, 2048], BF16)
            Vs = P2.sb("Vs", [128, 16, 512], BF16)
            tS = [P2.sb("tS", [128, 512], F32) for _ in range(2)]
            pT = [P2.sb("pT", [128, 512], BF16) for _ in range(2)]
            ostg = [P2.sb("ostg", [128, 512], BF16) for _ in range(2)]
            rden = P2.sb("rden", [128, 512], F32)
            pS3 = [P2.ps("pS", [128, 512]) for _ in range(3)]
            pS = pS3[0:2]
            pI = P2.ps("pI", [128, 512])
            dgen = [None]

            def tick(n=1):
                if dgen[0] is not None:
                    for _ in range(n):
                        try:
                            next(dgen[0])
                        except StopIteration:
                            dgen[0] = None
                            break
            pO = P2.ps("pO", [128, 512]); pD = P2.ps("pD", [128, 512])
            pX = P2.ps("pX", [128, 512])
            pXb = P2.ps("pXb", [128, 128], BF16)
            cn = {"s": 0, "o": 0}

            def load_qkv(m, kheads=4, vcols=512, v0=0):
                S.dma("sp", lambda e: e.dma_start(out=QTs[:], in_=QT[m].rearrange("(h p) n -> p h n", p=128)), reads=["QT%d" % m], writes=R(QTs))
                if kheads == 4:
                    S.dma("sp", lambda e: e.dma_start(out=KTs[:], in_=KT[m].rearrange("(h p) n -> p h n", p=128)), reads=["KT%d" % m], writes=R(KTs))
                else:
                    S.dma("sp", lambda e: e.dma_start(out=KTs[:, 0, :], in_=KT[m, 0:128, :]), reads=["KT%d" % m], writes=R(KTs))
                S.dma("sp", lambda e: e.dma_start(out=Vs[:, :, 0:vcols], in_=Vd[:, v0:v0 + vcols].rearrange("(t p) c -> p t c", p=128)), reads=["Vd"], writes=R(Vs))

            def softmax_attn(J, qap, kap, vsl, slope, extra=None):
                na = 8 * J + 8

                def emit_qk(a):
                    i3 = a % 3
                    ex = extra(a) if extra is not None else None
                    S.op("pe", lambda e: e.matmul(pS3[i3][:], kap(a), qap(J), start=True, stop=(ex is None)), reads=R(KTs, QTs), writes=R(pS3[i3]))
                    if ex is not None:
                        S.op("pe", lambda e: e.matmul(pS3[i3][:], ex[0], ex[1], start=False, stop=True), reads=ex[2], writes=R(pS3[i3]))
                emit_qk(0)
                if na > 1:
                    emit_qk(1)
                for a in range(na):
                    i = a % 2; i3 = a % 3
                    if a < 8 * J:
                        tab = cf[:, O_BF:O_BF + 512]; cbias = slope * 128.0 * (a - 8 * J)
                    else:
                        r = a - 8 * J
                        tab = cf[:, O_BD + r * 512:O_BD + (r + 1) * 512]; cbias = 0.0
                    S.op("dve", lambda e: e.scalar_tensor_tensor(out=tS[i][:], in0=tab, scalar=float(slope), in1=pS3[i3][:], op0=ALU.mult, op1=ALU.add),
                         reads=R(pS3[i3], cf), writes=R(tS[i]))
                    S.op("act", lambda e: e.activation(out=pT[i][:], in_=tS[i][:], func=AF.Exp, bias=float(cbias)), reads=R(tS[i]), writes=R(pT[i]))
                    if a + 2 < na:
                        emit_qk(a + 2)
                    S.op("pe", lambda e: e.matmul(pO[:], vsl(a), pT[i][:], start=(a == 0), stop=(a == na - 1)), reads=R(Vs, pT[i]), writes=R(pO))
                    S.op("pe", lambda e: e.matmul(pD[:], onesb, pT[i][:], start=(a == 0), stop=(a == na - 1)), reads=R(cb, pT[i]), writes=R(pD))
                    tick()

            qap = None
            QTs_cur = [QTs]

            def out_branch(n, h, J, src_fn):
                o = ostg[cn["o"] % 2]; cn["o"] += 1
                src_fn(o)
                S.dma("sp", lambda e, o=o: e.dma_start(out=BRT[(n * 4 + h) * 128:(n * 4 + h + 1) * 128, J * 512:(J + 1) * 512], in_=o[:]), reads=R(o), writes=["BRT"])

            if "D" in mixers:
                iqT = P2.sb("iqT", [128, 4, 1024], BF16); ikT = P2.sb("ikT", [128, 2048], BF16)
                iw = P2.sb("iw", [128, 8, 8], F32); absw = P2.sb("absw", [128, 8, 8], F32); sgn = P2.sb("sgn", [128, 8, 8], F32)
                acc = P2.sb("acc", [128, 2048], F32); work = P2.sb("work", [128, 2048], F32)
                rl = [P2.sb("rl", [128, 512], F32) for _ in range(2)]
                nm = P2.sb("nm", [128, 2048], BF16)
                nmT = P2.sb("nmT", [128, 16, 1024], BF16)
                m8 = P2.sb("m8", [128, 8], F32)
                blo = P2.sb("blo", [128, 1], F32); bmid = P2.sb("bmid", [128, 1], F32); bcnt = P2.sb("bcnt", [128, 1], F32)
                BW = 65536.0; NIT = 26
                S.dma("sp", lambda e: e.dma_start(out=iqT[:], in_=QT[4].rearrange("(h p) n -> p h n", p=128)), reads=["QT4"], writes=R(iqT))
                S.dma("sp", lambda e: e.dma_start(out=ikT[0:64, :], in_=KT[3, 128:192, :]), reads=["KT3"], writes=R(ikT))
                S.dma("sp", lambda e: e.dma_start(out=ikT[64:128, :], in_=KT[3, 128:192, :]), reads=["KT3"], writes=R(ikT))
                S.dma("sp", lambda e: e.dma_start(out=iw[:], in_=IW.rearrange("(t p) c -> p t c", p=128)), reads=["IW"], writes=R(iw))
                S.op("act", lambda e: e.activation(out=absw[:], in_=iw[:], func=AF.Abs), reads=R(iw), writes=R(absw))
                S.op("dve", lambda e: e.tensor_scalar(sgn[:], iw[:], 0.0, 2.0, op0=ALU.is_ge, op1=ALU.mult), reads=R(iw), writes=R(sgn))
                S.op("dve", lambda e: e.tensor_scalar(sgn[:], sgn[:], -1.0, None, op0=ALU.add), reads=R(sgn), writes=R(sgn))
                S.op("pool", lambda e: e.memset(nmT[:], 0.0), writes=R(nmT))

                def d_indexer():
                    ri = 0
                    for j in range(1, 8):
                        Lk = 256 * (j + 1)
                        for ih in range(8):
                            lo = (ih % 2) * 64
                            for c in range((Lk + 511) // 512):
                                w_ = min(512, Lk - c * 512)
                                i = ri % 2; ri += 1
                                S.op("pe", lambda e: e.matmul(pI[:, 0:w_], iqT[lo:lo + 64, ih // 2, j * 128:(j + 1) * 128], ikT[lo:lo + 64, c * 512:c * 512 + w_], start=True, stop=True),
                                     reads=R(iqT, ikT), writes=R(pI))
                                S.op("act", lambda e: e.activation(out=rl[i][:, 0:w_], in_=pI[:, 0:w_], func=AF.Relu, scale=absw[:, j, ih:ih + 1]), reads=R(pI, absw), writes=R(rl[i]))
                                if ih == 0:
                                    S.op("dve", lambda e: e.tensor_scalar(acc[:, c * 512:c * 512 + w_], rl[i][:, 0:w_], sgn[:, j, 0:1], None, op0=ALU.mult), reads=R(rl[i], sgn), writes=R(acc))
                                else:
                                    S.op("dve", lambda e: e.scalar_tensor_tensor(out=acc[:, c * 512:c * 512 + w_], in0=rl[i][:, 0:w_], scalar=sgn[:, j, ih:ih + 1], in1=acc[:, c * 512:c * 512 + w_], op0=ALU.mult, op1=ALU.add),
                                         reads=R(rl[i], sgn, acc), writes=R(acc))
                                yield
                        S.op("pool", lambda e: e.tensor_tensor(acc[:, Lk - 256:Lk], acc[:, Lk - 256:Lk], cf[:, O_TMA:O_TMA + 256], op=ALU.add), reads=R(acc, cf), writes=R(acc))
                        S.op("dve", lambda e: e.reduce_max(out=blo[:], in_=acc[:, 0:Lk], axis=AX.X), reads=R(acc), writes=R(blo))
                        S.op("dve", lambda e: e.tensor_scalar(blo[:], blo[:], -BW, None, op0=ALU.add), reads=R(blo), writes=R(blo))
                        yield
                        for it in range(NIT):
                            half = BW / (2.0 ** (it + 1))
                            S.op("dve", lambda e: e.tensor_scalar(bmid[:], blo[:], half, None, op0=ALU.add), reads=R(blo), writes=R(bmid))
                            S.op("dve", lambda e: e.tensor_scalar(work[:, 0:Lk], acc[:, 0:Lk], bmid[:, 0:1], 0.0, op0=ALU.is_ge, op1=ALU.add, accum_out=bcnt[:]), reads=R(acc, bmid), writes=R(work, bcnt))
                            S.op("dve", lambda e: e.tensor_scalar(bcnt[:], bcnt[:], 255.5, half, op0=ALU.is_ge, op1=ALU.mult), reads=R(bcnt), writes=R(bcnt))
                            S.op("dve", lambda e: e.tensor_tensor(blo[:], blo[:], bcnt[:], op=ALU.add), reads=R(blo, bcnt), writes=R(blo))
                            yield
                        S.op("dve", lambda e: e.tensor_scalar(nm[:, 0:Lk], acc[:, 0:Lk], blo[:, 0:1], -30000.0, op0=ALU.is_lt, op1=ALU.mult), reads=R(acc, blo), writes=R(nm))
                        for a in range(2 * j + 2):
                            S.op("pe", lambda e: e.transpose(pXb[:], nm[:, a * 128:(a + 1) * 128], idb), reads=R(nm, cb), writes=R(pXb))
                            S.op("act", lambda e: e.copy(nmT[:, a, j * 128:(j + 1) * 128], pXb[:]), reads=R(pXb), writes=R(nmT))
                            yield
                dgen[0] = d_indexer()

            if "A" in mixers:
                load_qkv(0, vcols=512, v0=0)
                lam = P2.sb("lam", [128, 4], F32)
                o0 = P2.sb("o0", [128, 512], F32); o1 = P2.sb("o1", [128, 512], F32); osq = P2.sb("osq", [128, 512], BF16)
                gsub = P2.sb("gsub", [128, 1], F32)
                S.op("dve", lambda e: e.tensor_tensor(lam[:, 0:1], vecs[:, V_LAM:V_LAM + 1], vecs[:, V_LAM + 1:V_LAM + 2], op=ALU.mult), reads=R(vecs), writes=R(lam))
                S.op("dve", lambda e: e.tensor_tensor(lam[:, 1:2], vecs[:, V_LAM + 2:V_LAM + 3], vecs[:, V_LAM + 3:V_LAM + 4], op=ALU.mult), reads=R(vecs), writes=R(lam))
                S.op("pe", lambda e: e.matmul(pX[:, 0:2], cf[:, O_ONEG:O_ONEG + 128], lam[:, 0:2], start=True, stop=True), reads=R(cf, lam), writes=R(pX))
                S.op("act", lambda e: e.activation(out=lam[:, 2:4], in_=pX[:, 0:2], func=AF.Exp, scale=-1.0), reads=R(pX), writes=R(lam))
                S.op("dve", lambda e: e.tensor_tensor(lam[:, 0:1], lam[:, 3:4], lam[:, 2:3], op=ALU.subtract), reads=R(lam), writes=R(lam))
                S.op("dve", lambda e: e.tensor_tensor(lam[:, 0:1], lam[:, 0:1], vecs[:, V_LI:V_LI + 1], op=ALU.subtract), reads=R(lam, vecs), writes=R(lam))
                S.op("dve", lambda e: e.tensor_tensor(gsub[:], vecs[:, V_SUBLN:V_SUBLN + 1], vecs[:, V_OML:V_OML + 1], op=ALU.mult), reads=R(vecs), writes=R(gsub))
                for h in range(4):
                    for J in range(2):
                        for comp in range(2):
                            lo, hi = comp * 64, comp * 64 + 64
                            qap = (lambda J, lo=lo, hi=hi, h=h: QTs[lo:hi, h, J * 512:(J + 1) * 512])
                            softmax_attn(J, qap, lambda a, lo=lo, hi=hi, h=h: KTs[lo:hi, h, a * 128:(a + 1) * 128],
                                         lambda a, h=h: Vs[:, a, h * 128:(h + 1) * 128], S_A[h])
                            dst = o0 if comp == 0 else o1
                            S.op("dve", lambda e: e.reciprocal(rden[:], pD[:]), reads=R(pD), writes=R(rden))
                            S.op("dve", lambda e, dst=dst: e.tensor_tensor(dst[:], pO[:], rden[:], op=ALU.mult), reads=R(pO, rden), writes=R(dst))
                        S.op("dve", lambda e: e.scalar_tensor_tensor(out=o0[:], in0=o1[:], scalar=lam[:, 0:1], in1=o0[:], op0=ALU.mult, op1=ALU.add), reads=R(o0, o1, lam), writes=R(o0))
                        S.op("act", lambda e: e.activation(out=osq[:], in_=o0[:], func=AF.Square), reads=R(o0), writes=R(osq))
                        S.op("pe", lambda e: e.matmul(pX[:], onesb, osq[:], start=True, stop=True), reads=R(osq, cb), writes=R(pX))
                        S.op("act", lambda e: e.activation(out=rden[:], in_=pX[:], func=AF.Sqrt, bias=epsc[:, 0:1], scale=1.0 / 128), reads=R(pX, epsc), writes=R(rden))
                        S.op("dve", lambda e: e.reciprocal(rden[:], rden[:]), reads=R(rden), writes=R(rden))
                        S.op("dve", lambda e: e.tensor_tensor(o0[:], o0[:], rden[:], op=ALU.mult), reads=R(o0, rden), writes=R(o0))
                        out_branch(0, h, J, lambda o: S.op("act", lambda e, o=o: e.activation(out=o[:], in_=o0[:], func=AF.Identity, scale=gsub[:, 0:1]), reads=R(o0, gsub), writes=R(o)))

            if "B" in mixers:
                load_qkv(1, vcols=512, v0=512)
                Ef = [P2.sb("Ef", [128, 512], F32) for _ in range(3)]
                SPf = [P2.sb("SPf", [128, 512], F32) for _ in range(3)]
                Racc = P2.sb("Racc", [128, 512], F32)
                pWs = [pD, pX]
                uneg = cf[:, O_UNEG:O_UNEG + 128]; oneg = cf[:, O_ONEG:O_ONEG + 128]
                for h in range(4):
                    for J in range(2):
                        na = 8 * J + 8
                        order = list(range(na - 1, -1, -1))

                        def ktqt(a):
                            return KTs[:, h, a * 128:(a + 1) * 128], QTs[:, h, J * 512:(J + 1) * 512]

                        def stage1(ai):
                            a = order[ai]; i3 = ai % 3
                            kt, qt = ktqt(a)
                            S.op("pe", lambda e: e.matmul(pS3[i3][:], kt, qt, start=True, stop=True), reads=R(KTs, QTs), writes=R(pS3[i3]))
                            S.op("act", lambda e: e.activation(out=Ef[i3][:], in_=pS3[i3][:], func=AF.Exp), reads=R(pS3[i3]), writes=R(Ef[i3]))
                            S.op("act", lambda e: e.activation(out=SPf[i3][:], in_=Ef[i3][:], func=AF.Ln, bias=1.0), reads=R(Ef[i3]), writes=R(SPf[i3]))
                            if a >= 8 * J:
                                bm = cf[:, O_BM + (a - 8 * J) * 512:O_BM + (a - 8 * J + 1) * 512]
                                S.op("dve", lambda e: e.tensor_tensor(SPf[i3][:], SPf[i3][:], bm, op=ALU.mult), reads=R(SPf[i3], cf), writes=R(SPf[i3]))
                        stage1(0)
                        if na > 1:
                            stage1(1)
                        for ai in range(na):
                            a = order[ai]; i = ai % 2; i3 = ai % 3
                            pW = pWs[ai % 2]
                            kt, qt = ktqt(a)
                            S.op("pe", lambda e: e.matmul(pW[:], kt, qt, start=True, stop=False), reads=R(KTs, QTs), writes=R(pW))
                            S.op("pe", lambda e: e.matmul(pW[:], uneg, SPf[i3][:], start=False, stop=(ai == 0)), reads=R(cf, SPf[i3]), writes=R(pW))
                            if ai > 0:
                                S.op("pe", lambda e: e.matmul(pW[:], oneg, Racc[:], start=False, stop=True), reads=R(cf, Racc), writes=R(pW))
                            S.op("act", lambda e: e.activation(out=pT[i][:], in_=pW[:], func=AF.Exp), reads=R(pW), writes=R(pT[i]))
                            if a >= 8 * J:
                                bm = cf[:, O_BM + (a - 8 * J) * 512:O_BM + (a - 8 * J + 1) * 512]
                                S.op("dve", lambda e: e.tensor_tensor(pT[i][:], pT[i][:], bm, op=ALU.mult), reads=R(pT[i], cf), writes=R(pT[i]))
                            if ai == 0:
                                S.op("dve", lambda e: e.tensor_copy(Racc[:], SPf[i3][:]), reads=R(SPf[i3]), writes=R(Racc))
                            else:
                                S.op("dve", lambda e: e.tensor_tensor(Racc[:], Racc[:], SPf[i3][:], op=ALU.add), reads=R(SPf[i3], Racc), writes=R(Racc))
                            if ai + 2 < na:
                                stage1(ai + 2)
                            S.op("pe", lambda e: e.matmul(pO[:], Vs[:, a, h * 128:(h + 1) * 128], pT[i][:], start=(ai == 0), stop=(ai == na - 1)), reads=R(Vs, pT[i]), writes=R(pO))
                            tick()
                        out_branch(1, h, J, lambda o: S.op("act", lambda e, o=o: e.copy(o[:], pO[:]), reads=R(pO), writes=R(o)))

            if "C" in mixers:
                load_qkv(2, vcols=512, v0=1024)
                kmf = P2.sb("kmf", [128, 4, 8], F32); kmb = P2.sb("kmb", [128, 4, 8], BF16)
                gsb = P2.sb("gsb", [128, 8], F32); top8 = P2.sb("top8", [128, 8], F32); thr = P2.sb("thr", [128, 1], F32)
                nsT = P2.sb("nsT", [8, 512], BF16)
                for h in range(4):
                    S.op("dve", lambda e, h=h: e.tensor_reduce(out=kmf[:, h, :], in_=KTs[:, h, :].rearrange("p (n k) -> p n k", k=256), axis=AX.X, op=ALU.add), reads=R(KTs), writes=R(kmf))
                S.op("dve", lambda e: e.tensor_scalar(kmb[:], kmf[:], 1.0 / 256, None, op0=ALU.mult), reads=R(kmf), writes=R(kmb))
                for h in range(4):
                    for J in range(2):
                        for jj in range(4):
                            j = 4 * J + jj
                            S.op("pe", lambda e, h=h, j=j: e.matmul(pX[:, 0:8], QTs[:, h, j * 128:(j + 1) * 128], kmb[:, h, :], start=True, stop=True), reads=R(QTs, kmb), writes=R(pX))
                            S.op("dve", lambda e, j=j: e.tensor_tensor(gsb[:], pX[:, 0:8], cf[:, O_MG + j * 8:O_MG + j * 8 + 8], op=ALU.add), reads=R(pX, cf), writes=R(gsb))
                            S.op("dve", lambda e: e.max(out=top8[:], in_=gsb[:]), reads=R(gsb), writes=R(top8))
                            S.op("dve", lambda e: e.tensor_scalar(thr[:], top8[:, 2:3], -1e29, None, op0=ALU.max), reads=R(top8), writes=R(thr))
                            S.op("dve", lambda e: e.tensor_scalar(gsb[:], gsb[:], thr[:, 0:1], None, op0=ALU.is_ge), reads=R(gsb, thr), writes=R(gsb))
                            S.op("dve", lambda e, j=j: e.tensor_tensor(gsb[:], gsb[:], cf[:, O_OWN + j * 8:O_OWN + j * 8 + 8], op=ALU.add), reads=R(gsb, cf), writes=R(gsb))
                            S.op("dve", lambda e: e.tensor_scalar(gsb[:], gsb[:], -1.0, 30000.0, op0=ALU.add, op1=ALU.mult), reads=R(gsb), writes=R(gsb))
                            S.op("pe", lambda e: e.transpose(pX[0:8, 128:256], gsb[:], idf), reads=R(gsb, cf), writes=R(pX))
                            S.op("act", lambda e, jj=jj: e.copy(nsT[:, jj * 128:(jj + 1) * 128], pX[0:8, 128:256]), reads=R(pX), writes=R(nsT))
                        qap = (lambda J, h=h: QTs[:, h, J * 512:(J + 1) * 512])
                        softmax_attn(J, qap, lambda a, h=h: KTs[:, h, a * 128:(a + 1) * 128], lambda a, h=h: Vs[:, a, h * 128:(h + 1) * 128], S_C[h],
                                     extra=lambda a: (cb[0:8, O_EALL + (a // 2) * 128:O_EALL + (a // 2 + 1) * 128], nsT[:], R(cb, nsT)))
                        S.op("dve", lambda e: e.reciprocal(rden[:], pD[:]), reads=R(pD), writes=R(rden))
                        out_branch(2, h, J, lambda o: S.op("dve", lambda e, o=o: e.tensor_tensor(o[:], pO[:], rden[:], op=ALU.mult), reads=R(pO, rden), writes=R(o)))

            if "D" in mixers:
                if dgen[0] is not None:
                    for _ in dgen[0]:
                        pass
                    dgen[0] = None
                load_qkv(3, kheads=1, vcols=128, v0=1536)
                for h in range(4):
                    for J in range(2):
                        qap = (lambda J, h=h: QTs[:, h, J * 512:(J + 1) * 512])
                        softmax_attn(J, qap, lambda a: KTs[:, 0, a * 128:(a + 1) * 128], lambda a: Vs[:, a, 0:128], S_D[h],
                                     extra=lambda a, J=J: (idb, nmT[:, a, J * 512:(J + 1) * 512], R(cb, nmT)))
                        S.op("dve", lambda e: e.reciprocal(rden[:], pD[:]), reads=R(pD), writes=R(rden))
                        out_branch(3, h, J, lambda o: S.op("dve", lambda e, o=o: e.tensor_tensor(o[:], pO[:], rden[:], op=ALU.mult), reads=R(pO, rden), writes=R(o)))

        if 3 in phases:
          with Scope() as P3:
            hTo = P3.sb("hTo3", [128, 16, 1024], BF16)
            brT = P3.sb("brT", [128, 16, 1024], BF16)
            yT = P3.sb("yT", [128, 16, 1024], BF16)
            wsl = [P3.sb("wsl3", [128, 16, 512], BF16) for _ in range(2)]
            wbs = [P3.sb("wbs", [128, 4, 512], BF16) for _ in range(2)]
            yacc = P3.sb("yacc", [128, 4, 1024], F32)
            gt = [P3.sb("gt", [128, 512], F32) for _ in range(2)]
            xo = [P3.sb("xo", [128, 1024], F32) for _ in range(2)]
            pg = [P3.ps("pg3", [128, 512]) for _ in range(2)]
            pu = [P3.ps("pu3", [128, 512]) for _ in range(2)]
            pz = [P3.ps("pz3", [128, 512]) for _ in range(2)]
            S.dma("sp", lambda e: e.dma_start(out=hTo[:], in_=hTo_d.rearrange("(k p) n -> p k n", p=128)), reads=["hTo_d"], writes=R(hTo))
            S.dma("sp", lambda e: e.dma_start(out=brT[:], in_=BRT.rearrange("(k p) n -> p k n", p=128)), reads=["BRT"], writes=R(brT))
            wi = 0; gi = 0
            for ds in range(4):
                for n in range(4):
                    ws = wsl[wi % 2]; wb = wbs[wi % 2]; wi += 1
                    c0 = C_GL + n * 2048 + ds * 512
                    S.dma("pool", lambda e, ws=ws, c0=c0: e.dma_start(out=ws[:], in_=w_in[:, c0:c0 + 512].rearrange("(k p) n -> p k n", p=128)), writes=R(ws))
                    S.dma("pool", lambda e, wb=wb, n=n, ds=ds: e.dma_start(out=wb[:], in_=w_br[n, :, ds * 512:(ds + 1) * 512].rearrange("(k p) n -> p k n", p=128)), writes=R(wb))
                    for sub in range(4):
                        dg = ds * 4 + sub
                        for tt in range(2):
                            b = gi % 2; gi += 1
                            for k in range(16):
                                S.op("pe", lambda e, b=b, ws=ws, k=k, sub=sub, tt=tt: e.matmul(pg[b][:], ws[:, k, sub * 128:(sub + 1) * 128], hTo[:, k, tt * 512:(tt + 1) * 512], start=(k == 0), stop=(k == 15)),
                                     reads=R(ws, hTo), writes=R(pg[b]))
                            for k in range(4):
                                S.op("pe", lambda e, b=b, wb=wb, k=k, sub=sub, tt=tt, n=n: e.matmul(pu[b][:], wb[:, k, sub * 128:(sub + 1) * 128], brT[:, n * 4 + k, tt * 512:(tt + 1) * 512], start=(k == 0), stop=(k == 3)),
                                     reads=R(wb, brT), writes=R(pu[b]))
                            S.op("act", lambda e, b=b, n=n, dg=dg: e.activation(out=gt[b][:], in_=pg[b][:], func=AF.Sigmoid, bias=vecs[:, V_GB + n * 16 + dg:V_GB + n * 16 + dg + 1]), reads=R(pg[b], vecs), writes=R(gt[b]))
                            ya = yacc[:, sub, tt * 512:(tt + 1) * 512]
                            if n == 0:
                                S.op("dve", lambda e, b=b, ya=ya: e.tensor_tensor(ya, gt[b][:], pu[b][:], op=ALU.mult), reads=R(gt[b], pu[b]), writes=R(yacc))
                            else:
                                S.op("dve", lambda e, b=b: e.tensor_tensor(gt[b][:], gt[b][:], pu[b][:], op=ALU.mult), reads=R(gt[b], pu[b]), writes=R(gt[b]))
                                S.op("dve", lambda e, b=b, ya=ya: e.tensor_tensor(ya, ya, gt[b][:], op=ALU.add), reads=R(gt[b], yacc), writes=R(yacc))
                S.op("act", lambda e, ds=ds: e.copy(yT[:, ds * 4:(ds + 1) * 4, :], yacc[:]), reads=R(yacc), writes=R(yT))
            zi = 0
            for os_ in range(4):
                ws = wsl[wi % 2]; wi += 1
                S.dma("pool", lambda e, ws=ws, os_=os_: e.dma_start(out=ws[:], in_=w_out[:, os_ * 512:(os_ + 1) * 512].rearrange("(k p) n -> p k n", p=128)), writes=R(ws))
                for sub in range(4):
                    og = os_ * 4 + sub
                    x_ = xo[og % 2]
                    S.dma("sp", lambda e, x_=x_, og=og: e.dma_start(out=x_[:], in_=xTo[og * 128:(og + 1) * 128, :]), writes=R(x_))
                    for tt in range(2):
                        b = zi % 2; zi += 1
                        for k in range(16):
                            S.op("pe", lambda e, b=b, ws=ws, k=k, sub=sub, tt=tt: e.matmul(pz[b][:], ws[:, k, sub * 128:(sub + 1) * 128], yT[:, k, tt * 512:(tt + 1) * 512], start=(k == 0), stop=(k == 15)),
                                 reads=R(ws, yT), writes=R(pz[b]))
                        S.op("dve", lambda e, b=b, x_=x_, og=og, tt=tt: e.scalar_tensor_tensor(out=x_[:, tt * 512:(tt + 1) * 512], in0=pz[b][:], scalar=vecs[:, V_MOD + 32 + og:V_MOD + 32 + og + 1], in1=x_[:, tt * 512:(tt + 1) * 512], op0=ALU.mult, op1=ALU.add),
                             reads=R(pz[b], vecs, x_), writes=R(x_))
                    S.dma("sp", lambda e, x_=x_, og=og: e.dma_start(out=xmT[og * 128:(og + 1) * 128, :], in_=x_[:]), reads=R(x_), writes=["xmT"])

        if 4 in phases:
          with Scope() as P4:
            rw = P4.sb("rw", [128, 16, 32], F32); rb = P4.sb("rb", [1, 32], F32); onesf = P4.sb("onesf", [1, 128], F32)
            hb = [P4.sb("hb", [128, 16, TW], BF16) for _ in range(2)]
            lg = P4.sb("lg", [128, 32], F32); t8 = P4.sb("t8", [128, 8], F32); nm1 = P4.sb("nm1", [128, 1], F32)
            sel = P4.sb("sel", [128, 32], F32); ex = P4.sb("ex", [128, 32], F32); ssum = P4.sb("ssum", [128, 1], F32)
            pl = P4.ps("plg", [128, 32])
            S.dma("sp", lambda e: e.dma_start(out=rw[:], in_=rw_d), writes=R(rw))
            S.dma("sp", lambda e: e.dma_start(out=rb[:], in_=rb_d), writes=R(rb))
            S.op("dve", lambda e: e.memset(onesf[:], 1.0), writes=R(onesf))
            src = xmT if 3 in phases else xTo

            def c_h2(tt, x):
                h_ = hb[tt % 2]
                S.op("dve", lambda e, h_=h_, x=x: e.tensor_copy(h_[:], x[:]), reads=R(x), writes=R(h_))
                S.dma("sp", lambda e, h_=h_, tt=tt: e.dma_start(out=h2T[:, tt * TW:(tt + 1) * TW].rearrange("(k p) n -> p k n", p=128), in_=h_[:]), reads=R(h_), writes=["h2T"])
                for s4 in range(TW // 128):
                    for k in range(16):
                        S.op("pe", lambda e, x=x, k=k, s4=s4: e.matmul(pl[:], x[:, k, s4 * 128:(s4 + 1) * 128], rw[:, k, :], start=(k == 0), stop=False), reads=R(x, rw), writes=R(pl))
                    S.op("pe", lambda e: e.matmul(pl[:], onesf[:], rb[:], start=False, stop=True), reads=R(onesf, rb), writes=R(pl))
                    S.op("dve", lambda e: e.tensor_copy(lg[:], pl[:]), reads=R(pl), writes=R(lg))
                    S.op("dve", lambda e: e.max(out=t8[:], in_=lg[:]), reads=R(lg), writes=R(t8))
                    S.op("dve", lambda e: e.tensor_scalar(nm1[:], t8[:, 0:1], -1.0, None, op0=ALU.mult), reads=R(t8), writes=R(nm1))
                    S.op("dve", lambda e: e.tensor_scalar(sel[:], lg[:], t8[:, 3:4], None, op0=ALU.is_ge), reads=R(lg, t8), writes=R(sel))
                    S.op("act", lambda e: e.activation(out=ex[:], in_=lg[:], func=AF.Exp, bias=nm1[:, 0:1]), reads=R(lg, nm1), writes=R(ex))
                    S.op("dve", lambda e: e.tensor_tensor(ex[:], ex[:], sel[:], op=ALU.mult), reads=R(ex, sel), writes=R(ex))
                    S.op("dve", lambda e: e.reduce_sum(out=ssum[:], in_=ex[:], axis=AX.X), reads=R(ex), writes=R(ssum))
                    S.op("dve", lambda e: e.reciprocal(ssum[:], ssum[:]), reads=R(ssum), writes=R(ssum))
                    S.op("dve", lambda e: e.tensor_scalar(ex[:], ex[:], ssum[:, 0:1], None, op0=ALU.mult), reads=R(ex, ssum), writes=R(ex))
                    S.dma("sp", lambda e, tt=tt, s4=s4: e.dma_start(out=G_o[tt * TW + s4 * 128:tt * TW + (s4 + 1) * 128, :], in_=ex[:]), reads=R(ex), writes=["G"])
            def src_read_hook():
                pass
            norm_tiles_src = src
            _orig_dma = S.dma
            def dma_with_dep(q, fn, reads=(), writes=()):
                return _orig_dma(q, fn, reads=list(reads) + ["xmT"], writes=writes)
            S.dma = dma_with_dep
            norm_tiles(P4, norm_tiles_src, 1024, AB[:, 2, :], mod(3), c_h2)
            S.dma = _orig_dma
        S.finish(["xmT", "h2T", "G", "BRT"])
        S.emit()
    print("M ops", S.n_ops, "waits", S.n_waits)
    return nc


def col16(v):
    return np.ascontiguousarray(v.reshape(16, 128).T)


def prep_vecs(l, mod_b, n1g, n2g, gate_b, a_qn_g, a_kn_g, c_qn_g, c_kn_g, d_qn_g, d_kn_g, a_subln_g, lq1, lk1, lq2, lk2):
    v = np.zeros((128, NV), np.float32)
    for i in range(6):
        v[:, V_MOD + 16 * i:V_MOD + 16 * (i + 1)] = col16(mod_b[i * 2048:(i + 1) * 2048])
    v[:, V_N1G:V_N1G + 16] = col16(n1g); v[:, V_N2G:V_N2G + 16] = col16(n2g)
    v[:, V_GB:V_GB + 64] = gate_b.reshape(64, 128).T
    v[:, V_AQG] = np.tile(a_qn_g, 2); v[:, V_AKG] = np.tile(a_kn_g, 2)
    v[:, V_CQG] = c_qn_g; v[:, V_CKG] = c_kn_g; v[:, V_DQG] = d_qn_g; v[:, V_DKG] = d_kn_g
    v[:, V_SUBLN] = a_subln_g
    v[0:64, V_LAM] = lq1; v[0:64, V_LAM + 1] = lk1; v[0:64, V_LAM + 2] = lq2; v[0:64, V_LAM + 3] = lk2
    li = 0.8 - 0.6 * math.exp(-0.3 * l)
    v[:, V_LI] = li; v[:, V_OML] = 1.0 - li
    return v


def own_cols(p):
    return np.concatenate([np.arange(128 * (2 * j + p), 128 * (2 * j + p + 1)) for j in range(8)])


def prep_m(l, p, x_b, mod_b, P):
    cf, cb = m_consts(p)
    xT = np.ascontiguousarray(x_b.T)
    return {
        "xTa": xT, "xTo": np.ascontiguousarray(xT[:, own_cols(p)]),
        "vecs": prep_vecs(l, mod_b, P["norm1_g"], P["norm2_g"], P["gate_b"], P["a_qn_g"], P["a_kn_g"], P["c_qn_g"], P["c_kn_g"],
                          P["d_qn_g"], P["d_kn_g"], P["a_subln_g"], P["a_lam_q1"], P["a_lam_k1"], P["a_lam_q2"], P["a_lam_k2"]),
        "cf": cf, "cb": cb, "w_in": P["w_in"], "w_br": P["w_branch"], "w_out": P["w_out"],
        "rw": np.ascontiguousarray(P["router_w"].reshape(16, 128, 32).transpose(1, 0, 2)), "rb": np.ascontiguousarray(P["router_b"].reshape(1, 32)),
    }


_PROGS = {}


def _prog(name, fn):
    if name not in _PROGS:
        _PROGS[name] = fn()
    return _PROGS[name]


def _run(nc, in_maps):
    res = run_bass_kernel_spmd(nc, in_maps, core_ids=list(range(len(in_maps))))
    return res.results


def kernel(x, c, ada_w, ada_b, norm1_g, norm2_g, w_in, gate_b, a_qn_g, a_kn_g, a_lam_q1, a_lam_k1,
           a_lam_q2, a_lam_k2, a_subln_g, c_qn_g, c_kn_g, d_qn_g, d_kn_g, w_branch, w_out,
           router_w, router_b, w1, b1, w2, b2):
    f32 = np.float32
    x = np.asarray(x, f32); c = np.asarray(c, f32)
    L = 2
    nc0 = _prog("l0", build_l0)
    cT = np.ascontiguousarray(c.T)
    ada_w = np.asarray(ada_w, f32); ada_b = np.asarray(ada_b, f32)
    r0 = _run(nc0, [{"cT": cT, "w": np.ascontiguousarray(ada_w[:, :, i * 1536:(i + 1) * 1536]),
                     "b": np.ascontiguousarray(ada_b[:, i * 1536:(i + 1) * 1536])} for i in range(8)])
    mod = np.concatenate([r["mod"] for r in r0], axis=2)
    ncm = _prog("m", build_m); nce = _prog("e2", build_e2); ncc = _prog("c2", build_c2)
    for l in range(L):
        P = dict(norm1_g=norm1_g[l], norm2_g=norm2_g[l], w_in=np.asarray(w_in[l], f32), gate_b=gate_b[l], a_qn_g=a_qn_g[l], a_kn_g=a_kn_g[l],
                 a_lam_q1=a_lam_q1[l], a_lam_k1=a_lam_k1[l], a_lam_q2=a_lam_q2[l], a_lam_k2=a_lam_k2[l], a_subln_g=a_subln_g[l],
                 c_qn_g=c_qn_g[l], c_kn_g=c_kn_g[l], d_qn_g=d_qn_g[l], d_kn_g=d_kn_g[l], w_branch=np.asarray(w_branch[l], f32),
                 w_out=np.asarray(w_out[l], f32), router_w=np.asarray(router_w[l], f32), router_b=np.asarray(router_b[l], f32))
        P = {k: np.asarray(v, f32) for k, v in P.items()}
        ims = [prep_m(l, core % 2, x[core // 2], mod[l, core // 2], P) for core in range(8)]
        rm = _run(ncm, ims)
        del ims
        h2_all = np.ascontiguousarray(np.concatenate([r["h2T"].T for r in rm], axis=0))
        G_all = np.concatenate([r["G"] for r in rm], axis=0)
        w1l = np.asarray(w1[l], f32); w2l = np.asarray(w2[l], f32); b1l = np.asarray(b1[l], f32); b2l = np.asarray(b2[l], f32)
        cst = e2_consts()
        ime = []
        for ec in range(8):
            es = slice(4 * ec, 4 * ec + 4)
            ime.append({"h2": h2_all, "Gtok": np.ascontiguousarray(G_all[:, es]), "w1": np.ascontiguousarray(w1l[es]),
                        "b1t_in": np.ascontiguousarray(b1l[es].reshape(4, 32, 128).transpose(2, 0, 1)), "w2": np.ascontiguousarray(w2l[es]),
                        "b2": np.ascontiguousarray(b2l[es]), "cst": cst})
        re_ = _run(nce, ime)
        del ime
        imc = []
        for core in range(8):
            P8 = np.ascontiguousarray(np.stack([re_[ec]["y"][core * 1024:(core + 1) * 1024] for ec in range(8)], axis=0))
            imc.append({"P8": P8, "xm_in": np.ascontiguousarray(rm[core]["xmT"].T),
                        "g2row": np.ascontiguousarray(mod[l, core // 2, 5 * 2048:6 * 2048].reshape(1, 2048))})
        rc = _run(ncc, imc)
        xn = np.empty_like(x)
        for core in range(8):
            xn[core // 2][own_cols(core % 2)] = rc[core]["xn"]
        x = xn
    return x
```

```python
import math
import numpy as np
import ml_dtypes
from contextlib import ExitStack
import concourse.bass as bass
import concourse.mybir as mybir
from concourse.bass_utils import run_bass_kernel_spmd


ENGS = ("pe", "act", "dve", "pool", "sp")


class _Rec:
    def __init__(self):
        self.call = None

    def __getattr__(self, name):
        def f(*a, **k):
            self.call = (name, a, k)
            return self
        return f


class Sched:
    def __init__(self, nc, n_dma_sems=16):
        self.nc = nc
        self.streams = {e: [] for e in ENGS}
        self.cnt = {e: 0 for e in ENGS}
        self.last_w = {}
        self.readers = {}
        self.waited = {e: {} for e in ENGS}
        self.n_dma = n_dma_sems
        self.dma_next = 0
        self.dma_val = [0] * n_dma_sems
        self.n_ops = 0
        self.n_waits = 0

    def _need(self, eng, ev):
        key, val = ev
        if self.waited[eng].get(key, 0) >= val:
            return
        self.waited[eng][key] = val
        self.streams[eng].append(("wait", key, val))
        self.n_waits += 1

    def _deps(self, eng, reads, writes):
        for r in reads:
            ev = self.last_w.get(r)
            if ev is not None:
                self._dep1(eng, ev)
        for w in writes:
            ev = self.last_w.get(w)
            if ev is not None:
                self._dep1(eng, ev)
            for k, v in self.readers.get(w, {}).items():
                self._dep1(eng, (k, v))

    def _dep1(self, eng, ev):
        if eng == "pe" and ev[0] == "pe":
            return
        self._need(eng, ev)

    def _commit(self, ev, reads, writes):
        for r in reads:
            d = self.readers.setdefault(r, {})
            if d.get(ev[0], 0) < ev[1]:
                d[ev[0]] = ev[1]
        for w in writes:
            self.last_w[w] = ev
            self.readers[w] = {}

    def op(self, eng, fn, reads=(), writes=()):
        self._deps(eng, reads, writes)
        self.cnt[eng] += 1
        ev = (eng, self.cnt[eng])
        rec = _Rec(); fn(rec)
        self.streams[eng].append(("op", rec.call, eng, 1))
        self._commit(ev, reads, writes)
        self.n_ops += 1
        return ev

    def dma(self, q, fn, reads=(), writes=()):
        self._deps(q, reads, writes)
        j = self.dma_next
        self.dma_next = (j + 1) % self.n_dma
        key = ("dma", j)
        if self.dma_val[j] > 0:
            self._need(q, (key, self.dma_val[j]))
        self.dma_val[j] += 16
        ev = (key, self.dma_val[j])
        rec = _Rec(); fn(rec)
        self.streams[q].append(("op", rec.call, key, 16))
        self._commit(ev, reads, writes)
        self.n_ops += 1
        return ev

    def finish(self, out_res):
        for r in out_res:
            ev = self.last_w.get(r)
            if ev is not None:
                self._need("sp", ev)

    def emit(self):
        nc = self.nc
        with ExitStack() as st:
            sems = {}
            for e in ENGS:
                sems[e] = st.enter_context(nc.semaphore("s_" + e))
            for j in range(self.n_dma):
                sems[("dma", j)] = st.enter_context(nc.semaphore("s_dma%d" % j))
            block = st.enter_context(nc.Block())

            regcache = {}

            def run(eng_obj, stream):
                for it in stream:
                    if it[0] == "wait":
                        eng_obj.wait_ge(sems[it[1]], it[2])
                    else:
                        name, a, k = it[1]
                        if name == "indirect_dma_start" and isinstance(k.get("bounds_check"), int):
                            v = k["bounds_check"]
                            if v not in regcache:
                                regcache[v] = eng_obj.to_reg(v)
                            k = dict(k); k["bounds_check"] = regcache[v]
                        try:
                            ins = getattr(eng_obj, name)(*a, **k)
                        except Exception as ex_:
                            import traceback
                            traceback.print_exception(ex_)
                            print("CAUSE", repr(ex_.__cause__), repr(ex_.__context__))
                            print("FAILED OP:", name, [str(x)[:200] for x in a], {kk: str(v)[:200] for kk, v in k.items()})
                            raise
                        ins.then_inc(sems[it[2]], it[3])

            @block.tensor
            def _(e):
                run(e, self.streams["pe"])

            @block.scalar
            def _(e):
                run(e, self.streams["act"])

            @block.vector
            def _(e):
                run(e, self.streams["dve"])

            @block.gpsimd
            def _(e):
                run(e, self.streams["pool"])

            @block.sync
            def _(e):
                run(e, self.streams["sp"])


F32 = mybir.dt.float32; BF16 = mybir.dt.bfloat16
AF = mybir.ActivationFunctionType; ALU = mybir.AluOpType
D = 2048


def build_l0(L=2, NCOL=1536):
    nc = bass.Bass("TRN2", target_bir_lowering=False)
    cT = nc.dram_tensor("cT", [D, 4], F32, kind="ExternalInput").ap()
    w = nc.dram_tensor("w", [L, D, NCOL], F32, kind="ExternalInput").ap()
    bia = nc.dram_tensor("b", [L, NCOL], F32, kind="ExternalInput").ap()
    out = nc.dram_tensor("mod", [L, 4, NCOL], F32, kind="ExternalOutput").ap()
    S = Sched(nc)
    with ExitStack() as st:
        def sb(name, shape, dt): return st.enter_context(nc.sbuf_tensor(name, shape, dt))
        def ps(name, shape, dt): return st.enter_context(nc.psum_tensor(name, shape, dt))
        cond = sb("cond", [128, 16, 4], F32)
        ones = sb("ones", [1, 4], F32)
        wt = [sb("wt%d" % i, [128, 16, 512], F32) for i in range(2)]
        bt = [sb("bt%d" % i, [1, 512], F32) for i in range(2)]
        ot = [sb("ot%d" % i, [4, 512], F32) for i in range(2)]
        pt = [ps("pt%d" % i, [4, 512], F32) for i in range(2)]
        S.dma("sp", lambda e: e.dma_start(out=cond[:], in_=cT.rearrange("(k p) b -> p k b", p=128)), writes=["cond"])
        S.op("act", lambda e: e.activation(out=cond[:], in_=cond[:], func=AF.Silu), reads=["cond"], writes=["cond"])
        S.op("dve", lambda e: e.memset(ones[:], 1.0), writes=["ones"])
        it = 0
        for l in range(L):
            for cch in range(NCOL // 512):
                s = it % 2; it += 1
                S.dma("sp", lambda e, s=s, l=l, cch=cch: e.dma_start(out=wt[s][:], in_=w[l, :, cch * 512:(cch + 1) * 512].rearrange("(k p) n -> p k n", p=128)), writes=["wt%d" % s])
                S.dma("sp", lambda e, s=s, l=l, cch=cch: e.dma_start(out=bt[s][:], in_=bia[l:l + 1, cch * 512:(cch + 1) * 512]), writes=["bt%d" % s])
                for k in range(16):
                    S.op("pe", lambda e, s=s, k=k: e.matmul(pt[s][:], cond[:, k, :], wt[s][:, k, :], start=(k == 0), stop=False),
                         reads=["cond", "wt%d" % s], writes=["pt%d" % s])
                S.op("pe", lambda e, s=s: e.matmul(pt[s][:], ones[:], bt[s][:], start=False, stop=True),
                     reads=["ones", "bt%d" % s], writes=["pt%d" % s])
                S.op("dve", lambda e, s=s: e.tensor_copy(ot[s][:], pt[s][:]), reads=["pt%d" % s], writes=["ot%d" % s])
                S.dma("sp", lambda e, s=s, l=l, cch=cch: e.dma_start(out=out[l, :, cch * 512:(cch + 1) * 512], in_=ot[s][:]), reads=["ot%d" % s], writes=["OUT"])
        S.finish(["OUT"])
        S.emit()
    return nc


F32 = mybir.dt.float32; BF16 = mybir.dt.bfloat16; I32 = mybir.dt.int32
AF = mybir.ActivationFunctionType; ALU = mybir.AluOpType
D = 2048; FF = 2048


def e2_consts():
    c = np.zeros((128, 384), np.float32)
    pp, p = np.arange(128)[:, None], np.arange(128)[None, :]
    c[:, 0:128] = (pp < p)
    c[:, 128:256] = 1.0
    c[:, 256:384] = np.eye(128)
    return c.astype(ml_dtypes.bfloat16)


def build_e2(ntok=8192, nexp=4, CAP=2560):
    nc = bass.Bass("TRN2", target_bir_lowering=False)
    NT = ntok // 128; NS = (CAP + 1023) // 1024
    def urows(s_): return min(1024, CAP - s_ * 1024)
    h2 = nc.dram_tensor("h2", [ntok, D], BF16, kind="ExternalInput").ap()
    Gtok = nc.dram_tensor("Gtok", [ntok, nexp], F32, kind="ExternalInput").ap()
    w1 = nc.dram_tensor("w1", [nexp, D, 2 * FF], F32, kind="ExternalInput").ap()
    b1 = nc.dram_tensor("b1t_in", [128, nexp, 32], F32, kind="ExternalInput").ap()
    w2 = nc.dram_tensor("w2", [nexp, FF, D], F32, kind="ExternalInput").ap()
    b2 = nc.dram_tensor("b2", [nexp, D], F32, kind="ExternalInput").ap()
    cst = nc.dram_tensor("cst", [128, 384], BF16, kind="ExternalInput").ap()
    y = nc.dram_tensor("y", [ntok, D], BF16, kind="ExternalOutput").ap()
    Xc = [nc.dram_tensor("Xc%d" % i, [CAP, D], BF16, kind="Internal").ap() for i in range(nexp)]
    Yc = [nc.dram_tensor("Yc%d" % i, [CAP, D], BF16, kind="Internal").ap() for i in range(nexp)]
    S = Sched(nc)
    uid = [0]; RN = {}; KEEP = []

    def barrier():
        for e in ("pe", "act", "dve", "pool", "sp"):
            for e2 in ("pe", "act", "dve", "pool", "sp"):
                if e != e2 and S.cnt[e2] > 0:
                    S._need(e, (e2, S.cnt[e2]))
            for j in range(S.n_dma):
                if S.dma_val[j] > 0:
                    S._need(e, (("dma", j), S.dma_val[j]))

    class Scope:
        def __init__(self): self.st = ExitStack()
        def __enter__(self): self.st.__enter__(); return self
        def __exit__(self, *a):
            barrier(); return self.st.__exit__(*a)
        def sb(self, name, shape, dt):
            uid[0] += 1
            t = self.st.enter_context(nc.sbuf_tensor("%s_%d" % (name, uid[0]), shape, dt)); RN[id(t)] = "%s_%d" % (name, uid[0]); KEEP.append(t); return t
        def ps(self, name, shape, dt=F32):
            uid[0] += 1
            t = self.st.enter_context(nc.psum_tensor("%s_%d" % (name, uid[0]), shape, dt)); RN[id(t)] = "%s_%d" % (name, uid[0]); KEEP.append(t); return t

    def R(*ts): return [t if isinstance(t, str) else RN[id(t)] for t in ts]
    NC4 = NT * nexp
    with Scope() as G0:
        cb = G0.sb("cb", [128, 384], BF16)
        Gt = G0.sb("Gt", [128, NT, nexp], F32)
        slot = G0.sb("slot", [128, NC4], I32)
        b1t = G0.sb("b1t", [128, nexp, 32], F32); b1p = G0.sb("b1p", [128, nexp, 32], F32)
        S.dma("sp", lambda e: e.dma_start(out=cb[:], in_=cst), writes=R(cb))
        S.dma("sp", lambda e: e.dma_start(out=Gt[:], in_=Gtok.rearrange("(t p) e -> p t e", p=128)), writes=R(Gt))
        S.dma("sp", lambda e: e.dma_start(out=b1t[:], in_=b1), writes=R(b1t))
        S.op("dve", lambda e: e.tensor_scalar(b1p[:], b1t[:], 1.0, None, op0=ALU.add), reads=R(b1t), writes=R(b1p))
        triu = cb[:, 0:128]; onesb = cb[:, 128:256]; idb = cb[:, 256:384]
        Gf = Gt[:].rearrange("p t e -> p (t e)")
        with Scope() as PA:
            mask = PA.sb("mask", [128, NC4], BF16); maskf = PA.sb("maskf", [128, NC4], F32)
            sa = PA.sb("sa", [128, NC4], F32); sb_ = PA.sb("sb", [128, NC4], F32); posf = PA.sb("posf", [128, NC4], F32)
            pp1 = PA.ps("pp1", [128, NC4]); ptot = PA.ps("ptot", [128, NC4])
            S.op("dve", lambda e: e.tensor_scalar(maskf[:], Gf, 0.0, None, op0=ALU.is_gt), reads=R(Gt), writes=R(maskf))
            S.op("dve", lambda e: e.tensor_copy(mask[:], maskf[:]), reads=R(maskf), writes=R(mask))
            S.op("pe", lambda e: e.matmul(pp1[:], triu, mask[:], start=True, stop=True), reads=R(cb, mask), writes=R(pp1))
            S.op("pe", lambda e: e.matmul(ptot[:], onesb, mask[:], start=True, stop=True), reads=R(cb, mask), writes=R(ptot))
            S.op("dve", lambda e: e.tensor_copy(sa[:], ptot[:]), reads=R(ptot), writes=R(sa))
            cur, oth = sa, sb_
            s = 1
            while s < NT:
                w = s * nexp
                S.op("dve", lambda e, cur=cur, oth=oth, w=w: e.tensor_tensor(oth[:, w:NC4], cur[:, w:NC4], cur[:, 0:NC4 - w], op=ALU.add), reads=R(cur), writes=R(oth))
                S.op("dve", lambda e, cur=cur, oth=oth, w=w: e.tensor_copy(oth[:, 0:w], cur[:, 0:w]), reads=R(cur), writes=R(oth))
                cur, oth = oth, cur
                s *= 2
            S.op("dve", lambda e, cur=cur: e.tensor_tensor(posf[:], cur[:], ptot[:], op=ALU.subtract), reads=R(cur, ptot), writes=R(posf))
            S.op("dve", lambda e: e.tensor_tensor(posf[:], posf[:], pp1[:], op=ALU.add), reads=R(posf, pp1), writes=R(posf))
            S.op("dve", lambda e: e.tensor_scalar(maskf[:], maskf[:], -1.0e6, 1.0e6, op0=ALU.mult, op1=ALU.add), reads=R(maskf), writes=R(maskf))
            S.op("dve", lambda e: e.tensor_tensor(posf[:], posf[:], maskf[:], op=ALU.add), reads=R(posf, maskf), writes=R(posf))
            S.op("dve", lambda e: e.tensor_copy(slot[:], posf[:]), reads=R(posf), writes=R(slot))
        with Scope() as PB:
            xt = [PB.sb("xt", [128, D], BF16) for _ in range(4)]
            for t in range(NT):
                x_ = xt[t % 4]
                S.dma("sp", lambda e, x_=x_, t=t: e.dma_start(out=x_[:], in_=h2[t * 128:(t + 1) * 128, :]), writes=R(x_))
                for ex in range(nexp):
                    S.dma("pool", lambda e, x_=x_, t=t, ex=ex: e.indirect_dma_start(out=Xc[ex], out_offset=bass.IndirectOffsetOnAxis(ap=slot[:, t * nexp + ex:t * nexp + ex + 1], axis=0),
                                                                                   in_=x_[:], in_offset=None, bounds_check=CAP - 1, oob_is_err=False),
                          reads=R(x_, slot), writes=["Xc%d" % ex])
        with Scope() as PD:
            Xrs = [PD.sb("Xr", [128, 8, D], BF16) for _ in range(2)]
            xri = [0]
            XT = PD.sb("XT", [128, 16, 1024], BF16)
            actT = PD.sb("actT", [128, 16, 1024], BF16)
            Yrs = [PD.sb("Yr", [128, 8, 512], BF16) for _ in range(2)]
            wsl = [PD.sb("wsl", [128, 16, 512], BF16) for _ in range(2)]
            b2bc = PD.sb("b2bc", [128, D], F32)
            tmp = {n: [PD.sb(n, [128, 512], F32) for _ in range(2)] for n in ("xg", "sg", "xl")}
            pg = [PD.ps("pg", [128, 512]) for _ in range(2)]
            pl = [PD.ps("pl", [128, 512]) for _ in range(2)]
            py = [PD.ps("py", [128, 512]) for _ in range(2)]
            ptr = [PD.ps("ptr", [128, 512], BF16) for _ in range(2)]
            wi = 0; ti = 0; yi = 0; tri = 0
            units = [(ex, s_) for ex in range(nexp) for s_ in range(NS)]

            def load_x(u):
                ex, s_ = units[u]
                r0 = s_ * 1024
                Xr = Xrs[u % 2]
                nr = urows(s_)
                S.dma("sp", lambda e: e.dma_start(out=Xr[:, 0:nr // 128, :], in_=Xc[ex][r0:r0 + nr, :].rearrange("(r p) f -> p r f", p=128)), reads=["Xc%d" % ex], writes=R(Xr))

            def prep(u):
                Xr = Xrs[u % 2]
                for fc in range(16):
                    for half in range(urows(units[u][1]) // 512):
                        pt_ = ptr[(fc * 2 + half) % 2]
                        for r4 in range(4):
                            rt = half * 4 + r4
                            S.op("pe", lambda e: e.transpose(pt_[:, r4 * 128:(r4 + 1) * 128], Xr[:, rt, fc * 128:(fc + 1) * 128], idb), reads=R(Xr, cb), writes=R(pt_))
                        if (fc + half) % 2 == 0:
                            S.op("act", lambda e: e.copy(XT[:, fc, half * 512:(half + 1) * 512], pt_[:]), reads=R(pt_), writes=R(XT))
                        else:
                            S.op("dve", lambda e: e.tensor_copy(XT[:, fc, half * 512:(half + 1) * 512], pt_[:]), reads=R(pt_), writes=R(XT))
            load_x(0)
            prep(0)
            if len(units) > 1:
                load_x(1)
            for u, (ex, s_) in enumerate(units):
                r0 = s_ * 1024
                if s_ == 0:
                    S.dma("sp", lambda e, ex=ex: e.dma_start(out=b2bc[:], in_=b2[ex:ex + 1, :].partition_broadcast(128)), writes=R(b2bc))
                for sl in range(8):
                    ws = wsl[wi % 2]; wi += 1
                    c0 = sl * 256
                    S.dma("pool", lambda e, ws=ws, ex=ex, c0=c0: e.dma_start(out=ws[:, :, 0:256], in_=w1[ex, :, c0:c0 + 256].rearrange("(k p) n -> p k n", p=128)), writes=R(ws))
                    S.dma("pool", lambda e, ws=ws, ex=ex, c0=c0: e.dma_start(out=ws[:, :, 256:512], in_=w1[ex, :, FF + c0:FF + c0 + 256].rearrange("(k p) n -> p k n", p=128)), writes=R(ws))
                    for sub in range(2):
                        j = sl * 2 + sub
                        for tt in range(urows(s_) // 512):
                            b = ti % 2; ti += 1
                            for k in range(16):
                                S.op("pe", lambda e, b=b, ws=ws, k=k, sub=sub, tt=tt: e.matmul(pg[b][:], ws[:, k, sub * 128:(sub + 1) * 128], XT[:, k, tt * 512:(tt + 1) * 512], start=(k == 0), stop=(k == 15)),
                                     reads=R(ws, XT), writes=R(pg[b]))
                            for k in range(16):
                                S.op("pe", lambda e, b=b, ws=ws, k=k, sub=sub, tt=tt: e.matmul(pl[b][:], ws[:, k, 256 + sub * 128:256 + (sub + 1) * 128], XT[:, k, tt * 512:(tt + 1) * 512], start=(k == 0), stop=(k == 15)),
                                     reads=R(ws, XT), writes=R(pl[b]))
                            xg, sg, xl = tmp["xg"][b], tmp["sg"][b], tmp["xl"][b]
                            S.op("dve", lambda e, b=b, xg=xg, ex=ex, j=j: e.tensor_scalar(xg[:], pg[b][:], b1t[:, ex, j:j + 1], 7.0, op0=ALU.add, op1=ALU.min), reads=R(pg[b], b1t), writes=R(xg))
                            S.op("act", lambda e, xg=xg, sg=sg: e.activation(out=sg[:], in_=xg[:], func=AF.Sigmoid, scale=1.702), reads=R(xg), writes=R(sg))
                            S.op("act", lambda e, b=b, xl=xl, ex=ex, j=j: e.activation(out=xl[:], in_=pl[b][:], func=AF.Identity, bias=b1p[:, ex, 16 + j:17 + j]), reads=R(pl[b], b1p), writes=R(xl))
                            S.op("dve", lambda e, xl=xl: e.tensor_scalar(xl[:], xl[:], 8.0, -6.0, op0=ALU.min, op1=ALU.max), reads=R(xl), writes=R(xl))
                            S.op("dve", lambda e, xl=xl, xg=xg: e.tensor_tensor(xg[:], xg[:], xl[:], op=ALU.mult), reads=R(xl, xg), writes=R(xg))
                            S.op("dve", lambda e, sg=sg, xg=xg, j=j, tt=tt: e.tensor_tensor(actT[:, j, tt * 512:(tt + 1) * 512], sg[:], xg[:], op=ALU.mult), reads=R(sg, xg), writes=R(actT))
                if u + 1 < len(units):
                    prep(u + 1)
                    if u + 2 < len(units):
                        load_x(u + 2)
                for sl in range(4):
                    ws = wsl[wi % 2]; wi += 1
                    Yr = Yrs[sl % 2]
                    c0 = sl * 512
                    S.dma("pool", lambda e, ws=ws, ex=ex, c0=c0: e.dma_start(out=ws[:], in_=w2[ex, :, c0:c0 + 512].rearrange("(k p) n -> p k n", p=128)), writes=R(ws))
                    for jt in range(urows(s_) // 128):
                        b = yi % 2; yi += 1
                        for k in range(16):
                            S.op("pe", lambda e, b=b, ws=ws, k=k, jt=jt: e.matmul(py[b][:], actT[:, k, jt * 128:(jt + 1) * 128], ws[:, k, :], start=(k == 0), stop=(k == 15)),
                                 reads=R(ws, actT), writes=R(py[b]))
                        S.op("dve", lambda e, b=b, jt=jt, c0=c0, Yr=Yr: e.tensor_tensor(Yr[:, jt, :], py[b][:], b2bc[:, c0:c0 + 512], op=ALU.add), reads=R(py[b], b2bc), writes=R(Yr))
                    S.dma("sp", lambda e, ex=ex, r0=r0, c0=c0, Yr=Yr: e.dma_start(out=Yc[ex][r0:r0 + urows(s_), c0:c0 + 512].rearrange("(r p) f -> p r f", p=128), in_=Yr[:, 0:urows(s_) // 128, :]), reads=R(Yr), writes=["Yc%d" % ex])
        with Scope() as PE_:
            ygt = PE_.sb("ygt", [128, 2 * nexp, D], BF16)
            RNX = {}
            class _V:
                def __init__(self, i, ex): self.i = i; self.ex = ex
                def __getitem__(self, k): return ygt[:, self.i * nexp + self.ex, :]
            yg = [[_V(i, ex) for ex in range(nexp)] for i in range(2)]
            for i in range(2):
                for ex in range(nexp):
                    RN[id(yg[i][ex])] = "yg_%d_%d" % (i, ex)
            acc = [PE_.sb("acc", [128, D], F32) for _ in range(2)]
            ob = [PE_.sb("ob", [128, D], BF16) for _ in range(2)]
            for i in range(2):
                for ex in range(nexp):
                    S.op("dve", lambda e, i=i, ex=ex: e.memset(yg[i][ex][:], 0.0), writes=R(yg[i][ex]))
            for t in range(NT):
                i = t % 2
                for ex in range(nexp):
                    S.dma("pool", lambda e, i=i, ex=ex, t=t: e.indirect_dma_start(out=yg[i][ex][:], out_offset=None, in_=Yc[ex],
                                                                                  in_offset=bass.IndirectOffsetOnAxis(ap=slot[:, t * nexp + ex:t * nexp + ex + 1], axis=0),
                                                                                  bounds_check=CAP - 1, oob_is_err=False),
                          reads=["Yc%d" % ex] + R(slot, yg[i][ex]), writes=R(yg[i][ex]))
                for ex in range(nexp):
                    if ex == 0:
                        S.op("act", lambda e, i=i, t=t: e.activation(out=acc[i][:], in_=yg[i][0][:], func=AF.Identity, scale=Gt[:, t, 0:1]), reads=R(yg[i][0], Gt), writes=R(acc[i]))
                    else:
                        last = (ex == nexp - 1)
                        dst = ob[i] if last else acc[i]
                        S.op("dve", lambda e, i=i, t=t, ex=ex, dst=dst: e.scalar_tensor_tensor(out=dst[:], in0=yg[i][ex][:], scalar=Gt[:, t, ex:ex + 1], in1=acc[i][:], op0=ALU.mult, op1=ALU.add),
                             reads=R(yg[i][ex], Gt, acc[i]), writes=R(dst))
                S.dma("sp", lambda e, i=i, t=t: e.dma_start(out=y[t * 128:(t + 1) * 128, :], in_=ob[i][:]), reads=R(ob[i]), writes=["OUT"])
        S.finish(["OUT"])
        S.emit()
    print("E2 ops", S.n_ops, "waits", S.n_waits)
    return nc


F32 = mybir.dt.float32; BF16 = mybir.dt.bfloat16
AF = mybir.ActivationFunctionType; ALU = mybir.AluOpType
D = 2048


def build_c2():
    nc = bass.Bass("TRN2", target_bir_lowering=False)
    P8 = nc.dram_tensor("P8", [8, 1024, D], BF16, kind="ExternalInput").ap()
    xm = nc.dram_tensor("xm_in", [1024, D], F32, kind="ExternalInput").ap()
    g2 = nc.dram_tensor("g2row", [1, D], F32, kind="ExternalInput").ap()
    xn = nc.dram_tensor("xn", [1024, D], F32, kind="ExternalOutput").ap()
    S = Sched(nc)
    with ExitStack() as st:
        def sb(name, shape, dt): return st.enter_context(nc.sbuf_tensor(name, shape, dt))
        g2t = sb("g2t", [128, D], F32)
        pt = [sb("pt%d" % i, [128, 8, D], BF16) for i in range(2)]
        xt = [sb("xt%d" % i, [128, D], F32) for i in range(2)]
        acc = [sb("acc%d" % i, [128, D], F32) for i in range(2)]
        acb = [sb("acb%d" % i, [128, D], F32) for i in range(2)]
        S.dma("sp", lambda e: e.dma_start(out=g2t[:], in_=g2.partition_broadcast(128)), writes=["g2t"])
        for k in range(8):
            s = k % 2
            S.dma("sp", lambda e, s=s, k=k: e.dma_start(out=pt[s][:], in_=P8[:, k * 128:(k + 1) * 128, :].rearrange("c p n -> p c n")), writes=["pt%d" % s])
            S.dma("sp", lambda e, s=s, k=k: e.dma_start(out=xt[s][:], in_=xm[k * 128:(k + 1) * 128, :]), writes=["xt%d" % s])
            S.op("dve", lambda e, s=s: e.tensor_tensor(acc[s][:], pt[s][:, 0, :], pt[s][:, 1, :], op=ALU.add), reads=["pt%d" % s], writes=["acc%d" % s])
            S.op("pool", lambda e, s=s: e.tensor_tensor(acb[s][:], pt[s][:, 4, :], pt[s][:, 5, :], op=ALU.add), reads=["pt%d" % s], writes=["acb%d" % s])
            for c in (2, 3):
                S.op("dve", lambda e, s=s, c=c: e.tensor_tensor(acc[s][:], acc[s][:], pt[s][:, c, :], op=ALU.add), reads=["pt%d" % s, "acc%d" % s], writes=["acc%d" % s])
            for c in (6, 7):
                S.op("pool", lambda e, s=s, c=c: e.tensor_tensor(acb[s][:], acb[s][:], pt[s][:, c, :], op=ALU.add), reads=["pt%d" % s, "acb%d" % s], writes=["acb%d" % s])
            S.op("dve", lambda e, s=s: e.tensor_tensor(acc[s][:], acc[s][:], acb[s][:], op=ALU.add), reads=["acb%d" % s, "acc%d" % s], writes=["acc%d" % s])
            S.op("dve", lambda e, s=s: e.tensor_tensor(acc[s][:], acc[s][:], g2t[:], op=ALU.mult), reads=["acc%d" % s, "g2t"], writes=["acc%d" % s])
            S.op("dve", lambda e, s=s: e.tensor_tensor(xt[s][:], xt[s][:], acc[s][:], op=ALU.add), reads=["acc%d" % s, "xt%d" % s], writes=["xt%d" % s])
            S.dma("sp", lambda e, s=s, k=k: e.dma_start(out=xn[k * 128:(k + 1) * 128, :], in_=xt[s][:]), reads=["xt%d" % s], writes=["OUT"])
        S.finish(["OUT"])
        S.emit()
    return nc


F32 = mybir.dt.float32; BF16 = mybir.dt.bfloat16
AF = mybir.ActivationFunctionType; ALU = mybir.AluOpType; AX = mybir.AxisListType
D = 2048
NIN = 14152
EPS = 1e-6
TW = 256
SLOPES = [2.0 ** (-8.0 * (i + 1) / 12) for i in range(12)]
S_A, S_C, S_D = SLOPES[0::3], SLOPES[1::3], SLOPES[2::3]
C_AQ, C_AK, C_AV, C_BQ, C_BK, C_BV, C_CQ, C_CK, C_CV = 0, 512, 1024, 1536, 2048, 2560, 3072, 3584, 4096
C_DQ, C_DK, C_DV, C_IQ, C_IK, C_IW, C_GL = 4608, 5120, 5248, 5376, 5888, 5952, 5960
O_BF, O_BD, O_BM, O_TMA, O_MG, O_OWN, O_IDF, O_UNEG, O_ONEG, NCF = 0, 512, 4608, 8704, 8960, 9024, 9088, 9216, 9344, 9472
O_IDB, O_ONESB, O_BLK64, O_EALL, NCB = 0, 128, 256, 384, 1408
V_MOD, V_N1G, V_N2G, V_GB, V_AQG, V_AKG, V_CQG, V_CKG, V_DQG, V_DKG, V_SUBLN, V_LAM, V_LI, V_OML, NV = 0, 96, 112, 128, 192, 193, 194, 195, 196, 197, 198, 199, 203, 204, 208
NEG = -1.0e9


def m_consts(p):
    k = np.arange(128)[:, None].astype(np.float32); q = np.arange(128)[None, :].astype(np.float32)
    kq = k - q
    one = np.ones((128, 128), np.float32)
    cf = np.zeros((128, NCF), np.float32)
    cf[:, O_BF:O_BF + 512] = np.concatenate([kq - 128 * p - 256 * jj for jj in range(4)], axis=1)
    for r in range(8):
        for jj in range(4):
            d = r - 2 * jj - p
            if d < 0:
                bd = kq + 128 * d; bm = one
            elif d == 0:
                bd = np.where(k <= q, kq, NEG); bm = (k < q).astype(np.float32)
            else:
                bd = NEG * one; bm = 0 * one
            cf[:, O_BD + r * 512 + jj * 128:O_BD + r * 512 + (jj + 1) * 128] = bd
            cf[:, O_BM + r * 512 + jj * 128:O_BM + r * 512 + (jj + 1) * 128] = bm
    qq = np.arange(128)[:, None]; ss = np.arange(128)[None, :]
    tri = np.where(ss <= qq, 0.0, -1e30).astype(np.float32)
    blkA = np.zeros((128, 128), np.float32) if p == 1 else tri
    blkB = tri if p == 1 else np.full((128, 128), -1e30, np.float32)
    cf[:, O_TMA:O_TMA + 128] = blkA; cf[:, O_TMA + 128:O_TMA + 256] = blkB
    for j in range(8):
        for n in range(8):
            cf[:, O_MG + j * 8 + n] = 0.0 if n < j else -1e30
            cf[:, O_OWN + j * 8 + n] = 1.0 if n == j else 0.0
    cf[:, O_IDF:O_IDF + 128] = np.eye(128)
    jj_, s_ = np.arange(128)[:, None], np.arange(128)[None, :]
    cf[:, O_UNEG:O_UNEG + 128] = np.where(jj_ >= s_, -1.0, 0.0)
    cf[:, O_ONEG:O_ONEG + 128] = -1.0
    cb = np.zeros((128, NCB), np.float32)
    cb[:, O_IDB:O_IDB + 128] = np.eye(128)
    cb[:, O_ONESB:O_ONESB + 128] = 1.0
    cb[0:64, O_BLK64:O_BLK64 + 64] = 1.0; cb[64:128, O_BLK64 + 64:O_BLK64 + 128] = 1.0
    for n in range(8):
        cb[n, O_EALL + n * 128:O_EALL + (n + 1) * 128] = 1.0
    return cf, cb.astype(ml_dtypes.bfloat16)


def build_m(dbg=False, phases=(1, 2, 3, 4), mixers="ABCD"):
    nc = bass.Bass("TRN2", target_bir_lowering=False)
    def din(name, shape, dt=F32): return nc.dram_tensor(name, shape, dt, kind="ExternalInput").ap()
    def dout(name, shape, dt=F32): return nc.dram_tensor(name, shape, dt, kind="ExternalOutput").ap()
    def dscr(name, shape, dt=BF16): return nc.dram_tensor(name, shape, dt, kind="Internal").ap()
    xTa = din("xTa", [D, 2048]); xTo = din("xTo", [D, 1024])
    vecs_d = din("vecs", [128, NV]); cf_d = din("cf", [128, NCF]); cb_d = din("cb", [128, NCB], BF16)
    w_in = din("w_in", [D, NIN]); w_br = din("w_br", [4, 512, D]); w_out = din("w_out", [D, D])
    rw_d = din("rw", [128, 16, 32]); rb_d = din("rb", [1, 32])
    xmT = dout("xmT", [D, 1024]); h2T = dout("h2T", [D, 1024], BF16); G_o = dout("G", [1024, 32])
    QT = dscr("QT", [5, 512, 1024]); KT = dscr("KT", [4, 512, 2048]); Vd = dscr("Vd", [2048, 1664])
    IW = dscr("IW", [1024, 8], F32)
    hTo_d = dscr("hTo_d", [D, 1024])
    if dbg:
        BRT = dout("BRT", [D, 1024], BF16)
    else:
        BRT = dscr("BRT", [D, 1024])
    S = Sched(nc)
    uid = [0]; RN = {}; KEEP = []

    def barrier():
        for e in ("pe", "act", "dve", "pool", "sp"):
            for e2 in ("pe", "act", "dve", "pool", "sp"):
                if e != e2 and S.cnt[e2] > 0:
                    S._need(e, (e2, S.cnt[e2]))
            for j in range(S.n_dma):
                if S.dma_val[j] > 0:
                    S._need(e, (("dma", j), S.dma_val[j]))

    class Scope:
        def __init__(self): self.st = ExitStack()
        def __enter__(self): self.st.__enter__(); return self
        def __exit__(self, *a):
            barrier(); return self.st.__exit__(*a)
        def sb(self, name, shape, dt):
            uid[0] += 1
            t = self.st.enter_context(nc.sbuf_tensor("%s_%d" % (name, uid[0]), shape, dt)); RN[id(t)] = "%s_%d" % (name, uid[0]); KEEP.append(t); return t
        def ps(self, name, shape, dt=F32):
            uid[0] += 1
            t = self.st.enter_context(nc.psum_tensor("%s_%d" % (name, uid[0]), shape, dt)); RN[id(t)] = "%s_%d" % (name, uid[0]); KEEP.append(t); return t

    def R(*ts): return [t if isinstance(t, str) else RN[id(t)] for t in ts]

    with Scope() as G0:
        vecs = G0.sb("vecs", [128, NV], F32); cf = G0.sb("cf", [128, NCF], F32); cb = G0.sb("cb", [128, NCB], BF16)
        AB = G0.sb("AB", [128, 4, 16], F32)
        epsc = G0.sb("epsc", [128, 1], F32)
        S.op("dve", lambda e: e.memset(epsc[:], EPS), writes=R(epsc))
        S.dma("sp", lambda e: e.dma_start(out=vecs[:], in_=vecs_d), writes=R(vecs))
        S.dma("sp", lambda e: e.dma_start(out=cf[:], in_=cf_d), writes=R(cf))
        S.dma("sp", lambda e: e.dma_start(out=cb[:], in_=cb_d), writes=R(cb))
        mod = lambda i: vecs[:, V_MOD + 16 * i:V_MOD + 16 * (i + 1)]
        for (ai, sci, gi) in ((0, 1, V_N1G), (2, 4, V_N2G)):
            S.op("dve", lambda e, ai=ai, sci=sci: e.tensor_scalar(AB[:, ai, :], mod(sci), 1.0, None, op0=ALU.add), reads=R(vecs), writes=R(AB))
            S.op("dve", lambda e, ai=ai, gi=gi: e.tensor_tensor(AB[:, ai, :], AB[:, ai, :], vecs[:, gi:gi + 16], op=ALU.mult), reads=R(vecs, AB), writes=R(AB))
        onesb = cb[:, O_ONESB:O_ONESB + 128]; idb = cb[:, O_IDB:O_IDB + 128]; blk64 = cb[:, O_BLK64:O_BLK64 + 128]
        idf = cf[:, O_IDF:O_IDF + 128]

        def norm_tiles(sc, src, ntok, Acol, Bcol, consume):
            xs = [sc.sb("xs", [128, 16, TW], F32) for _ in range(2)]
            sq = sc.sb("sq", [128, 16, TW], BF16)
            rstd = sc.sb("rstd", [128, TW], F32)
            pss = sc.ps("pss", [128, TW])
            for tt in range(ntok // TW):
                x = xs[tt % 2]
                S.dma("sp", lambda e, x=x, tt=tt: e.dma_start(out=x[:], in_=src[:, tt * TW:(tt + 1) * TW].rearrange("(k p) n -> p k n", p=128)), writes=R(x))
                S.op("act", lambda e, x=x: e.activation(out=sq[:], in_=x[:], func=AF.Square), reads=R(x), writes=R(sq))
                for k in range(16):
                    S.op("pe", lambda e, k=k: e.matmul(pss[:], onesb, sq[:, k, :], start=(k == 0), stop=(k == 15)), reads=R(sq, cb), writes=R(pss))
                S.op("act", lambda e: e.activation(out=rstd[:], in_=pss[:], func=AF.Sqrt, bias=epsc[:, 0:1], scale=1.0 / D), reads=R(pss, epsc), writes=R(rstd))
                S.op("dve", lambda e: e.reciprocal(rstd[:], rstd[:]), reads=R(rstd), writes=R(rstd))
                for k in range(16):
                    S.op("dve", lambda e, x=x, k=k: e.tensor_tensor(x[:, k, :], x[:, k, :], rstd[:], op=ALU.mult), reads=R(x, rstd), writes=R(x))
                    S.op("act", lambda e, x=x, k=k: e.activation(out=x[:, k, :], in_=x[:, k, :], func=AF.Identity, bias=Bcol[:, k:k + 1], scale=Acol[:, k:k + 1]),
                         reads=R(x, AB, vecs), writes=R(x))
                consume(tt, x)

        if 1 in phases:
          with Scope() as P1:
            hTa = P1.sb("hTa", [128, 16, 2048], BF16)
            hTo = P1.sb("hTo", [128, 16, 1024], BF16)
            gq = P1.sb("gq", [128, 8], F32)
            S.op("dve", lambda e: e.tensor_scalar(gq[:, 0:1], vecs[:, V_AQG:V_AQG + 1], 64 ** -0.5, None, op0=ALU.mult), reads=R(vecs), writes=R(gq))
            S.op("dve", lambda e: e.tensor_copy(gq[:, 1:2], vecs[:, V_AKG:V_AKG + 1]), reads=R(vecs), writes=R(gq))
            S.op("dve", lambda e: e.tensor_scalar(gq[:, 2:3], vecs[:, V_CQG:V_CQG + 1], 128 ** -0.5, None, op0=ALU.mult), reads=R(vecs), writes=R(gq))
            S.op("dve", lambda e: e.tensor_copy(gq[:, 3:4], vecs[:, V_CKG:V_CKG + 1]), reads=R(vecs), writes=R(gq))
            S.op("dve", lambda e: e.tensor_scalar(gq[:, 4:5], vecs[:, V_DQG:V_DQG + 1], 128 ** -0.5, None, op0=ALU.mult), reads=R(vecs), writes=R(gq))
            S.op("dve", lambda e: e.tensor_copy(gq[:, 5:6], vecs[:, V_DKG:V_DKG + 1]), reads=R(vecs), writes=R(gq))
            with Scope() as N1:
                def c_all(tt, x):
                    S.op("dve", lambda e, tt=tt, x=x: e.tensor_copy(hTa[:, :, tt * TW:(tt + 1) * TW], x[:]), reads=R(x), writes=R(hTa))
                norm_tiles(N1, xTa, 2048, AB[:, 0, :], mod(0), c_all)
            with Scope() as N1:
                def c_own(tt, x):
                    S.op("dve", lambda e, tt=tt, x=x: e.tensor_copy(hTo[:, :, tt * TW:(tt + 1) * TW], x[:]), reads=R(x), writes=R(hTo))
                norm_tiles(N1, xTo, 1024, AB[:, 0, :], mod(0), c_own)
            S.dma("sp", lambda e: e.dma_start(out=hTo_d.rearrange("(k p) n -> p k n", p=128), in_=hTo[:]), reads=R(hTo), writes=["hTo_d"])
            with Scope() as PJ:
                wsl = [PJ.sb("wsl", [128, 16, 512], BF16) for _ in range(2)]
                stg = [PJ.sb("stg", [128, 512], BF16) for _ in range(3)]
                stgf = PJ.sb("stgf", [128, 8], F32)
                sqn = [PJ.sb("sqn", [128, 512], BF16) for _ in range(2)]
                rs = [PJ.sb("rs", [128, 512], F32) for _ in range(2)]
                tq = [PJ.sb("tq", [128, 512], F32) for _ in range(2)]
                pp = [PJ.ps("pp", [128, 512]) for _ in range(3)]
                pn = [PJ.ps("pn", [128, 512]) for _ in range(2)]
                cnt = {"w": 0, "p": 0, "s": 0, "n": 0}

                def load_slab(c0, ncols):
                    ws = wsl[cnt["w"] % 2]; cnt["w"] += 1
                    S.dma("pool", lambda e, ws=ws: e.dma_start(out=ws[:, :, 0:ncols], in_=w_in[:, c0:c0 + ncols].rearrange("(k p) n -> p k n", p=128)), writes=R(ws))
                    return ws

                def proj_fm(c0, ncols, src, ntok, dst_fn, normed, gcol, blkmat, dh, cscale=1.0, rows=128):
                    ws = load_slab(c0, ncols)
                    for sub in range(max(1, ncols // 128)):
                        for tt in range(ntok // 512):
                            p_ = pp[cnt["p"] % 3]; cnt["p"] += 1
                            sg_ = stg[cnt["s"] % 3]; cnt["s"] += 1
                            for k in range(16):
                                S.op("pe", lambda e, p_=p_, ws=ws, k=k, sub=sub, tt=tt: e.matmul(p_[0:rows, :], ws[:, k, sub * 128:sub * 128 + rows], src[:, k, tt * 512:(tt + 1) * 512], start=(k == 0), stop=(k == 15)),
                                     reads=R(ws, src), writes=R(p_))
                            if normed:
                                i = cnt["n"] % 2; cnt["n"] += 1
                                S.op("act", lambda e, p_=p_, i=i: e.activation(out=sqn[i][:], in_=p_[:], func=AF.Square), reads=R(p_), writes=R(sqn[i]))
                                S.op("pe", lambda e, i=i: e.matmul(pn[i][:], blkmat, sqn[i][:], start=True, stop=True), reads=R(sqn[i], cb), writes=R(pn[i]))
                                S.op("act", lambda e, i=i: e.activation(out=rs[i][:], in_=pn[i][:], func=AF.Sqrt, bias=epsc[:, 0:1], scale=1.0 / dh), reads=R(pn[i], epsc), writes=R(rs[i]))
                                S.op("dve", lambda e, i=i: e.reciprocal(rs[i][:], rs[i][:]), reads=R(rs[i]), writes=R(rs[i]))
                                S.op("dve", lambda e, i=i, p_=p_: e.tensor_tensor(tq[i][:], p_[:], rs[i][:], op=ALU.mult), reads=R(p_, rs[i]), writes=R(tq[i]))
                                S.op("act", lambda e, i=i, sg_=sg_: e.activation(out=sg_[:], in_=tq[i][:], func=AF.Identity, scale=gcol), reads=R(tq[i], gq), writes=R(sg_))
                            else:
                                S.op("act", lambda e, p_=p_, sg_=sg_: e.activation(out=sg_[0:rows, :], in_=p_[0:rows, :], func=AF.Identity, scale=cscale), reads=R(p_), writes=R(sg_))
                            dst, dres = dst_fn(sub, tt)
                            S.dma("sp", lambda e, dst=dst, sg_=sg_: e.dma_start(out=dst, in_=sg_[0:rows, :]), reads=R(sg_), writes=[dres])

                def qdst(m):
                    return lambda sub, tt: (QT[m, sub * 128:(sub + 1) * 128, tt * 512:(tt + 1) * 512], "QT%d" % m)

                def kdst(m, r0=0, rows=128):
                    return lambda sub, tt: (KT[m, r0 + sub * 128:r0 + sub * 128 + rows, tt * 512:(tt + 1) * 512], "KT%d" % m)
                proj_fm(C_AQ, 512, hTo, 1024, qdst(0), True, gq[:, 0:1], blk64, 64)
                proj_fm(C_BQ, 512, hTo, 1024, qdst(1), False, None, None, 0, cscale=128 ** -0.5)
                proj_fm(C_CQ, 512, hTo, 1024, qdst(2), True, gq[:, 2:3], onesb, 128)
                proj_fm(C_DQ, 512, hTo, 1024, qdst(3), True, gq[:, 4:5], onesb, 128)
                proj_fm(C_IQ, 512, hTo, 1024, qdst(4), False, None, None, 0)
                proj_fm(C_AK, 512, hTa, 2048, kdst(0), True, gq[:, 1:2], blk64, 64)
                proj_fm(C_BK, 512, hTa, 2048, kdst(1), False, None, None, 0)
                proj_fm(C_CK, 512, hTa, 2048, kdst(2), True, gq[:, 3:4], onesb, 128)
                proj_fm(C_DK, 128, hTa, 2048, kdst(3), True, gq[:, 5:6], onesb, 128)
                proj_fm(C_IK, 64, hTa, 2048, kdst(3, r0=128, rows=64), False, None, None, 0, rows=64)
                for (c0, ncols, v0) in ((C_AV, 512, 0), (C_BV, 512, 512), (C_CV, 512, 1024), (C_DV, 128, 1536)):
                    ws = load_slab(c0, ncols)
                    for t in range(16):
                        p_ = pp[cnt["p"] % 3]; cnt["p"] += 1
                        sg_ = stg[cnt["s"] % 3]; cnt["s"] += 1
                        for k in range(16):
                            S.op("pe", lambda e, p_=p_, ws=ws, k=k, t=t, ncols=ncols: e.matmul(p_[:, 0:ncols], hTa[:, k, t * 128:(t + 1) * 128], ws[:, k, 0:ncols], start=(k == 0), stop=(k == 15)),
                                 reads=R(ws, hTa), writes=R(p_))
                        S.op("act", lambda e, p_=p_, sg_=sg_, ncols=ncols: e.copy(sg_[:, 0:ncols], p_[:, 0:ncols]), reads=R(p_), writes=R(sg_))
                        S.dma("sp", lambda e, sg_=sg_, t=t, v0=v0, ncols=ncols: e.dma_start(out=Vd[t * 128:(t + 1) * 128, v0:v0 + ncols], in_=sg_[:, 0:ncols]), reads=R(sg_), writes=["Vd"])
                ws = load_slab(C_IW, 8)
                for t in range(8):
                    p_ = pp[cnt["p"] % 3]; cnt["p"] += 1
                    for k in range(16):
                        S.op("pe", lambda e, p_=p_, ws=ws, k=k, t=t: e.matmul(p_[:, 0:8], hTo[:, k, t * 128:(t + 1) * 128], ws[:, k, 0:8], start=(k == 0), stop=(k == 15)),
                             reads=R(ws, hTo), writes=R(p_))
                    S.op("act", lambda e, p_=p_: e.copy(stgf[:], p_[:, 0:8]), reads=R(p_), writes=R(stgf))
                    S.dma("sp", lambda e, t=t: e.dma_start(out=IW[t * 128:(t + 1) * 128, :], in_=stgf[:]), reads=R(stgf), writes=["IW"])

        if 2 in phases:
          with Scope() as P2:
            QTs = P2.sb("QTs", [128, 4, 1024], BF16)
            KTs = P2.sb("KTs", [128, 4, 2048], BF16)
            Vs = P2.sb("Vs", [128, 16, 512], BF16)
            tS = [P2.sb("tS", [128, 512], F32) for _ in range(2)]
            pT = [P2.sb("pT", [128, 512], BF16) for _ in range(2)]
            ostg = [P2.sb("ostg", [128, 512], BF16) for _ in range(2)]
            rden = P2.sb("rden", [128, 512], F32)
            pS3 = [P2.ps("pS", [128, 512]) for _ in range(3)]
            pS = pS3[0:2]
            pI = P2.ps("pI", [128, 512])
            dgen = [None]

            def tick(n=1):
                if dgen[0] is not None:
                    for _ in range(n):
                        try:
                            next(dgen[0])
                        except StopIteration:
                            dgen[0] = None
                            break
            pO = P2.ps("pO", [128, 512]); pD = P2.ps("pD", [128, 512])
            pX = P2.ps("pX", [128, 512])
            pXb = P2.ps("pXb", [128, 128], BF16)
            cn = {"s": 0, "o": 0}

            def load_qkv(m, kheads=4, vcols=512, v0=0):
                S.dma("sp", lambda e: e.dma_start(out=QTs[:], in_=QT[m].rearrange("(h p) n -> p h n", p=128)), reads=["QT%d" % m], writes=R(QTs))
                if kheads == 4:
                    S.dma("sp", lambda e: e.dma_start(out=KTs[:], in_=KT[m].rearrange("(h p) n -> p h n", p=128)), reads=["KT%d" % m], writes=R(KTs))
                else:
                    S.dma("sp", lambda e: e.dma_start(out=KTs[:, 0, :], in_=KT[m, 0:128, :]), reads=["KT%d" % m], writes=R(KTs))
                S.dma("sp", lambda e: e.dma_start(out=Vs[:, :, 0:vcols], in_=Vd[:, v0:v0 + vcols].rearrange("(t p) c -> p t c", p=128)), reads=["Vd"], writes=R(Vs))

            def softmax_attn(J, qap, kap, vsl, slope, extra=None):
                na = 8 * J + 8

                def emit_qk(a):
                    i3 = a % 3
                    ex = extra(a) if extra is not None else None
                    S.op("pe", lambda e: e.matmul(pS3[i3][:], kap(a), qap(J), start=True, stop=(ex is None)), reads=R(KTs, QTs), writes=R(pS3[i3]))
                    if ex is not None:
                        S.op("pe", lambda e: e.matmul(pS3[i3][:], ex[0], ex[1], start=False, stop=True), reads=ex[2], writes=R(pS3[i3]))
                emit_qk(0)
                if na > 1:
                    emit_qk(1)
                for a in range(na):
                    i = a % 2; i3 = a % 3
                    if a < 8 * J:
                        tab = cf[:, O_BF:O_BF + 512]; cbias = slope * 128.0 * (a - 8 * J)
                    else:
                        r = a - 8 * J
                        tab = cf[:, O_BD + r * 512:O_BD + (r + 1) * 512]; cbias = 0.0
                    S.op("dve", lambda e: e.scalar_tensor_tensor(out=tS[i][:], in0=tab, scalar=float(slope), in1=pS3[i3][:], op0=ALU.mult, op1=ALU.add),
                         reads=R(pS3[i3], cf), writes=R(tS[i]))
                    S.op("act", lambda e: e.activation(out=pT[i][:], in_=tS[i][:], func=AF.Exp, bias=float(cbias)), reads=R(tS[i]), writes=R(pT[i]))
                    if a + 2 < na:
                        emit_qk(a + 2)
                    S.op("pe", lambda e: e.matmul(pO[:], vsl(a), pT[i][:], start=(a == 0), stop=(a == na - 1)), reads=R(Vs, pT[i]), writes=R(pO))
                    S.op("pe", lambda e: e.matmul(pD[:], onesb, pT[i][:], start=(a == 0), stop=(a == na - 1)), reads=R(cb, pT[i]), writes=R(pD))
                    tick()

            qap = None
            QTs_cur = [QTs]

            def out_branch(n, h, J, src_fn):
                o = ostg[cn["o"] % 2]; cn["o"] += 1
                src_fn(o)
                S.dma("sp", lambda e, o=o: e.dma_start(out=BRT[(n * 4 + h) * 128:(n * 4 + h + 1) * 128, J * 512:(J + 1) * 512], in_=o[:]), reads=R(o), writes=["BRT"])

            if "D" in mixers:
                iqT = P2.sb("iqT", [128, 4, 1024], BF16); ikT = P2.sb("ikT", [128, 2048], BF16)
                iw = P2.sb("iw", [128, 8, 8], F32); absw = P2.sb("absw", [128, 8, 8], F32); sgn = P2.sb("sgn", [128, 8, 8], F32)
                acc = P2.sb("acc", [128, 2048], F32); work = P2.sb("work", [128, 2048], F32)
                rl = [P2.sb("rl", [128, 512], F32) for _ in range(2)]
                nm = P2.sb("nm", [128, 2048], BF16)
                nmT = P2.sb("nmT", [128, 16, 1024], BF16)
                m8 = P2.sb("m8", [128, 8], F32)
                blo = P2.sb("blo", [128, 1], F32); bmid = P2.sb("bmid", [128, 1], F32); bcnt = P2.sb("bcnt", [128, 1], F32)
                BW = 65536.0; NIT = 26
                S.dma("sp", lambda e: e.dma_start(out=iqT[:], in_=QT[4].rearrange("(h p) n -> p h n", p=128)), reads=["QT4"], writes=R(iqT))
                S.dma("sp", lambda e: e.dma_start(out=ikT[0:64, :], in_=KT[3, 128:192, :]), reads=["KT3"], writes=R(ikT))
                S.dma("sp", lambda e: e.dma_start(out=ikT[64:128, :], in_=KT[3, 128:192, :]), reads=["KT3"], writes=R(ikT))
                S.dma("sp", lambda e: e.dma_start(out=iw[:], in_=IW.rearrange("(t p) c -> p t c", p=128)), reads=["IW"], writes=R(iw))
                S.op("act", lambda e: e.activation(out=absw[:], in_=iw[:], func=AF.Abs), reads=R(iw), writes=R(absw))
                S.op("dve", lambda e: e.tensor_scalar(sgn[:], iw[:], 0.0, 2.0, op0=ALU.is_ge, op1=ALU.mult), reads=R(iw), writes=R(sgn))
                S.op("dve", lambda e: e.tensor_scalar(sgn[:], sgn[:], -1.0, None, op0=ALU.add), reads=R(sgn), writes=R(sgn))
                S.op("pool", lambda e: e.memset(nmT[:], 0.0), writes=R(nmT))

                def d_indexer():
                    ri = 0
                    for j in range(1, 8):
                        Lk = 256 * (j + 1)
                        for ih in range(8):
                            lo = (ih % 2) * 64
                            for c in range((Lk + 511) // 512):
                                w_ = min(512, Lk - c * 512)
                                i = ri % 2; ri += 1
                                S.op("pe", lambda e: e.matmul(pI[:, 0:w_], iqT[lo:lo + 64, ih // 2, j * 128:(j + 1) * 128], ikT[lo:lo + 64, c * 512:c * 512 + w_], start=True, stop=True),
                                     reads=R(iqT, ikT), writes=R(pI))
                                S.op("act", lambda e: e.activation(out=rl[i][:, 0:w_], in_=pI[:, 0:w_], func=AF.Relu, scale=absw[:, j, ih:ih + 1]), reads=R(pI, absw), writes=R(rl[i]))
                                if ih == 0:
                                    S.op("dve", lambda e: e.tensor_scalar(acc[:, c * 512:c * 512 + w_], rl[i][:, 0:w_], sgn[:, j, 0:1], None, op0=ALU.mult), reads=R(rl[i], sgn), writes=R(acc))
                                else:
                                    S.op("dve", lambda e: e.scalar_tensor_tensor(out=acc[:, c * 512:c * 512 + w_], in0=rl[i][:, 0:w_], scalar=sgn[:, j, ih:ih + 1], in1=acc[:, c * 512:c * 512 + w_], op0=ALU.mult, op1=ALU.add),
                                         reads=R(rl[i], sgn, acc), writes=R(acc))
                                yield
                        S.op("pool", lambda e: e.tensor_tensor(acc[:, Lk - 256:Lk], acc[:, Lk - 256:Lk], cf[:, O_TMA:O_TMA + 256], op=ALU.add), reads=R(acc, cf), writes=R(acc))
                        S.op("dve", lambda e: e.reduce_max(out=blo[:], in_=acc[:, 0:Lk], axis=AX.X), reads=R(acc), writes=R(blo))
                        S.op("dve", lambda e: e.tensor_scalar(blo[:], blo[:], -BW, None, op0=ALU.add), reads=R(blo), writes=R(blo))
                        yield
                        for it in range(NIT):
                            half = BW / (2.0 ** (it + 1))
                            S.op("dve", lambda e: e.tensor_scalar(bmid[:], blo[:], half, None, op0=ALU.add), reads=R(blo), writes=R(bmid))
                            S.op("dve", lambda e: e.tensor_scalar(work[:, 0:Lk], acc[:, 0:Lk], bmid[:, 0:1], 0.0, op0=ALU.is_ge, op1=ALU.add, accum_out=bcnt[:]), reads=R(acc, bmid), writes=R(work, bcnt))
                            S.op("dve", lambda e: e.tensor_scalar(bcnt[:], bcnt[:], 255.5, half, op0=ALU.is_ge, op1=ALU.mult), reads=R(bcnt), writes=R(bcnt))
                            S.op("dve", lambda e: e.tensor_tensor(blo[:], blo[:], bcnt[:], op=ALU.add), reads=R(blo, bcnt), writes=R(blo))
                            yield
                        S.op("dve", lambda e: e.tensor_scalar(nm[:, 0:Lk], acc[:, 0:Lk], blo[:, 0:1], -30000.0, op0=ALU.is_lt, op1=ALU.mult), reads=R(acc, blo), writes=R(nm))
                        for a in range(2 * j + 2):
                            S.op("pe", lambda e: e.transpose(pXb[:], nm[:, a * 128:(a + 1) * 128], idb), reads=R(nm, cb), writes=R(pXb))
                            S.op("act", lambda e: e.copy(nmT[:, a, j * 128:(j + 1) * 128], pXb[:]), reads=R(pXb), writes=R(nmT))
                            yield
                dgen[0] = d_indexer()

            if "A" in mixers:
                load_qkv(0, vcols=512, v0=0)
                lam = P2.sb("lam", [128, 4], F32)
                o0 = P2.sb("o0", [128, 512], F32); o1 = P2.sb("o1", [128, 512], F32); osq = P2.sb("osq", [128, 512], BF16)
                gsub = P2.sb("gsub", [128, 1], F32)
                S.op("dve", lambda e: e.tensor_tensor(lam[:, 0:1], vecs[:, V_LAM:V_LAM + 1], vecs[:, V_LAM + 1:V_LAM + 2], op=ALU.mult), reads=R(vecs), writes=R(lam))
                S.op("dve", lambda e: e.tensor_tensor(lam[:, 1:2], vecs[:, V_LAM + 2:V_LAM + 3], vecs[:, V_LAM + 3:V_LAM + 4], op=ALU.mult), reads=R(vecs), writes=R(lam))
                S.op("pe", lambda e: e.matmul(pX[:, 0:2], cf[:, O_ONEG:O_ONEG + 128], lam[:, 0:2], start=True, stop=True), reads=R(cf, lam), writes=R(pX))
                S.op("act", lambda e: e.activation(out=lam[:, 2:4], in_=pX[:, 0:2], func=AF.Exp, scale=-1.0), reads=R(pX), writes=R(lam))
                S.op("dve", lambda e: e.tensor_tensor(lam[:, 0:1], lam[:, 3:4], lam[:, 2:3], op=ALU.subtract), reads=R(lam), writes=R(lam))
                S.op("dve", lambda e: e.tensor_tensor(lam[:, 0:1], lam[:, 0:1], vecs[:, V_LI:V_LI + 1], op=ALU.subtract), reads=R(lam, vecs), writes=R(lam))
                S.op("dve", lambda e: e.tensor_tensor(gsub[:], vecs[:, V_SUBLN:V_SUBLN + 1], vecs[:, V_OML:V_OML + 1], op=ALU.mult), reads=R(vecs), writes=R(gsub))
                for h in range(4):
                    for J in range(2):
                        for comp in range(2):
                            lo, hi = comp * 64, comp * 64 + 64
                            qap = (lambda J, lo=lo, hi=hi, h=h: QTs[lo:hi, h, J * 512:(J + 1) * 512])
                            softmax_attn(J, qap, lambda a, lo=lo, hi=hi, h=h: KTs[lo:hi, h, a * 128:(a + 1) * 128],
                                         lambda a, h=h: Vs[:, a, h * 128:(h + 1) * 128], S_A[h])
                            dst = o0 if comp == 0 else o1
                            S.op("dve", lambda e: e.reciprocal(rden[:], pD[:]), reads=R(pD), writes=R(rden))
                            S.op("dve", lambda e, dst=dst: e.tensor_tensor(dst[:], pO[:], rden[:], op=ALU.mult), reads=R(pO, rden), writes=R(dst))
                        S.op("dve", lambda e: e.scalar_tensor_tensor(out=o0[:], in0=o1[:], scalar=lam[:, 0:1], in1=o0[:], op0=ALU.mult, op1=ALU.add), reads=R(o0, o1, lam), writes=R(o0))
                        S.op("act", lambda e: e.activation(out=osq[:], in_=o0[:], func=AF.Square), reads=R(o0), writes=R(osq))
                        S.op("pe", lambda e: e.matmul(pX[:], onesb, osq[:], start=True, stop=True), reads=R(osq, cb), writes=R(pX))
                        S.op("act", lambda e: e.activation(out=rden[:], in_=pX[:], func=AF.Sqrt, bias=epsc[:, 0:1], scale=1.0 / 128), reads=R(pX, epsc), writes=R(rden))
                        S.op("dve", lambda e: e.reciprocal(rden[:], rden[:]), reads=R(rden), writes=R(rden))
                        S.op("dve", lambda e: e.tensor_tensor(o0[:], o0[:], rden[:], op=ALU.mult), reads=R(o0, rden), writes=R(o0))
                        out_branch(0, h, J, lambda o: S.op("act", lambda e, o=o: e.activation(out=o[:], in_=o0[:], func=AF.Identity, scale=gsub[:, 0:1]), reads=R(o0, gsub), writes=R(o)))

            if "B" in mixers:
                load_qkv(1, vcols=512, v0=512)
                Ef = [P2.sb("Ef", [128, 512], F32) for _ in range(3)]
                SPf = [P2.sb("SPf", [128, 512], F32) for _ in range(3)]
                Racc = P2.sb("Racc", [128, 512], F32)
                pWs = [pD, pX]
                uneg = cf[:, O_UNEG:O_UNEG + 128]; oneg = cf[:, O_ONEG:O_ONEG + 128]
                for h in range(4):
                    for J in range(2):
                        na = 8 * J + 8
                        order = list(range(na - 1, -1, -1))

                        def ktqt(a):
                            return KTs[:, h, a * 128:(a + 1) * 128], QTs[:, h, J * 512:(J + 1) * 512]

                        def stage1(ai):
                            a = order[ai]; i3 = ai % 3
                            kt, qt = ktqt(a)
                            S.op("pe", lambda e: e.matmul(pS3[i3][:], kt, qt, start=True, stop=True), reads=R(KTs, QTs), writes=R(pS3[i3]))
                            S.op("act", lambda e: e.activation(out=Ef[i3][:], in_=pS3[i3][:], func=AF.Exp), reads=R(pS3[i3]), writes=R(Ef[i3]))
                            S.op("act", lambda e: e.activation(out=SPf[i3][:], in_=Ef[i3][:], func=AF.Ln, bias=1.0), reads=R(Ef[i3]), writes=R(SPf[i3]))
                            if a >= 8 * J:
                                bm = cf[:, O_BM + (a - 8 * J) * 512:O_BM + (a - 8 * J + 1) * 512]
                                S.op("dve", lambda e: e.tensor_tensor(SPf[i3][:], SPf[i3][:], bm, op=ALU.mult), reads=R(SPf[i3], cf), writes=R(SPf[i3]))
                        stage1(0)
                        if na > 1:
                            stage1(1)
                        for ai in range(na):
                            a = order[ai]; i = ai % 2; i3 = ai % 3
                            pW = pWs[ai % 2]
                            kt, qt = ktqt(a)
                            S.op("pe", lambda e: e.matmul(pW[:], kt, qt, start=True, stop=False), reads=R(KTs, QTs), writes=R(pW))
                            S.op("pe", lambda e: e.matmul(pW[:], uneg, SPf[i3][:], start=False, stop=(ai == 0)), reads=R(cf, SPf[i3]), writes=R(pW))
                            if ai > 0:
                                S.op("pe", lambda e: e.matmul(pW[:], oneg, Racc[:], start=False, stop=True), reads=R(cf, Racc), writes=R(pW))
                            S.op("act", lambda e: e.activation(out=pT[i][:], in_=pW[:], func=AF.Exp), reads=R(pW), writes=R(pT[i]))
                            if a >= 8 * J:
                                bm = cf[:, O_BM + (a - 8 * J) * 512:O_BM + (a - 8 * J + 1) * 512]
                                S.op("dve", lambda e: e.tensor_tensor(pT[i][:], pT[i][:], bm, op=ALU.mult), reads=R(pT[i], cf), writes=R(pT[i]))
                            if ai == 0:
                                S.op("dve", lambda e: e.tensor_copy(Racc[:], SPf[i3][:]), reads=R(SPf[i3]), writes=R(Racc))
                            else:
                                S.op("dve", lambda e: e.tensor_tensor(Racc[:], Racc[:], SPf[i3][:], op=ALU.add), reads=R(SPf[i3], Racc), writes=R(Racc))
                            if ai + 2 < na:
                                stage1(ai + 2)
                            S.op("pe", lambda e: e.matmul(pO[:], Vs[:, a, h * 128:(h + 1) * 128], pT[i][:], start=(ai == 0), stop=(ai == na - 1)), reads=R(Vs, pT[i]), writes=R(pO))
                            tick()
                        out_branch(1, h, J, lambda o: S.op("act", lambda e, o=o: e.copy(o[:], pO[:]), reads=R(pO), writes=R(o)))

            if "C" in mixers:
                load_qkv(2, vcols=512, v0=1024)
                kmf = P2.sb("kmf", [128, 4, 8], F32); kmb = P2.sb("kmb", [128, 4, 8], BF16)
                gsb = P2.sb("gsb", [128, 8], F32); top8 = P2.sb("top8", [128, 8], F32); thr = P2.sb("thr", [128, 1], F32)
                nsT = P2.sb("nsT", [8, 512], BF16)
                for h in range(4):
                    S.op("dve", lambda e, h=h: e.tensor_reduce(out=kmf[:, h, :], in_=KTs[:, h, :].rearrange("p (n k) -> p n k", k=256), axis=AX.X, op=ALU.add), reads=R(KTs), writes=R(kmf))
                S.op("dve", lambda e: e.tensor_scalar(kmb[:], kmf[:], 1.0 / 256, None, op0=ALU.mult), reads=R(kmf), writes=R(kmb))
                for h in range(4):
                    for J in range(2):
                        for jj in range(4):
                            j = 4 * J + jj
                            S.op("pe", lambda e, h=h, j=j: e.matmul(pX[:, 0:8], QTs[:, h, j * 128:(j + 1) * 128], kmb[:, h, :], start=True, stop=True), reads=R(QTs, kmb), writes=R(pX))
                            S.op("dve", lambda e, j=j: e.tensor_tensor(gsb[:], pX[:, 0:8], cf[:, O_MG + j * 8:O_MG + j * 8 + 8], op=ALU.add), reads=R(pX, cf), writes=R(gsb))
                            S.op("dve", lambda e: e.max(out=top8[:], in_=gsb[:]), reads=R(gsb), writes=R(top8))
                            S.op("dve", lambda e: e.tensor_scalar(thr[:], top8[:, 2:3], -1e29, None, op0=ALU.max), reads=R(top8), writes=R(thr))
                            S.op("dve", lambda e: e.tensor_scalar(gsb[:], gsb[:], thr[:, 0:1], None, op0=ALU.is_ge), reads=R(gsb, thr), writes=R(gsb))
                            S.op("dve", lambda e, j=j: e.tensor_tensor(gsb[:], gsb[:], cf[:, O_OWN + j * 8:O_OWN + j * 8 + 8], op=ALU.add), reads=R(gsb, cf), writes=R(gsb))
                            S.op("dve", lambda e: e.tensor_scalar(gsb[:], gsb[:], -1.0, 30000.0, op0=ALU.add, op1=ALU.mult), reads=R(gsb), writes=R(gsb))
                            S.op("pe", lambda e: e.transpose(pX[0:8, 128:256], gsb[:], idf), reads=R(gsb, cf), writes=R(pX))
                            S.op("act", lambda e, jj=jj: e.copy(nsT[:, jj * 128:(jj + 1) * 128], pX[0:8, 128:256]), reads=R(pX), writes=R(nsT))
                        qap = (lambda J, h=h: QTs[:, h, J * 512:(J + 1) * 512])
                        softmax_attn(J, qap, lambda a, h=h: KTs[:, h, a * 128:(a + 1) * 128], lambda a, h=h: Vs[:, a, h * 128:(h + 1) * 128], S_C[h],
                                     extra=lambda a: (cb[0:8, O_EALL + (a // 2) * 128:O_EALL + (a // 2 + 1) * 128], nsT[:], R(cb, nsT)))
                        S.op("dve", lambda e: e.reciprocal(rden[:], pD[:]), reads=R(pD), writes=R(rden))
                        out_branch(2, h, J, lambda o: S.op("dve", lambda e, o=o: e.tensor_tensor(o[:], pO[:], rden[:], op=ALU.mult), reads=R(pO, rden), writes=R(o)))

            if "D" in mixers:
                if dgen[0] is not None:
                    for _ in dgen[0]:
                        pass
                    dgen[0] = None
                load_qkv(3, kheads=1, vcols=128, v0=1536)
                for h in range(4):
                    for J in range(2):
                        qap = (lambda J, h=h: QTs[:, h, J * 512:(J + 1) * 512])
                        softmax_attn(J, qap, lambda a: KTs[:, 0, a * 128:(a + 1) * 128], lambda a: Vs[:, a, 0:128], S_D[h],
                                     extra=lambda a, J=J: (idb, nmT[:, a, J * 512:(J + 1) * 512], R(cb, nmT)))
                        S.op("dve", lambda e: e.reciprocal(rden[:], pD[:]), reads=R(pD), writes=R(rden))
                        out_branch(3, h, J, lambda o: S.op("dve", lambda e, o=o: e.tensor_tensor(o[:], pO[:], rden[:], op=ALU.mult), reads=R(pO, rden), writes=R(o)))

        if 3 in phases:
          with Scope() as P3:
            hTo = P3.sb("hTo3", [128, 16, 1024], BF16)
            brT = P3.sb("brT", [128, 16, 1024], BF16)
            yT = P3.sb("yT", [128, 16, 1024], BF16)
            wsl = [P3.sb("wsl3", [128, 16, 512], BF16) for _ in range(2)]
            wbs = [P3.sb("wbs", [128, 4, 512], BF16) for _ in range(2)]
            yacc = P3.sb("yacc", [128, 4, 1024], F32)
            gt = [P3.sb("gt", [128, 512], F32) for _ in range(2)]
            xo = [P3.sb("xo", [128, 1024], F32) for _ in range(2)]
            pg = [P3.ps("pg3", [128, 512]) for _ in range(2)]
            pu = [P3.ps("pu3", [128, 512]) for _ in range(2)]
            pz = [P3.ps("pz3", [128, 512]) for _ in range(2)]
            S.dma("sp", lambda e: e.dma_start(out=hTo[:], in_=hTo_d.rearrange("(k p) n -> p k n", p=128)), reads=["hTo_d"], writes=R(hTo))
            S.dma("sp", lambda e: e.dma_start(out=brT[:], in_=BRT.rearrange("(k p) n -> p k n", p=128)), reads=["BRT"], writes=R(brT))
            wi = 0; gi = 0
            for ds in range(4):
                for n in range(4):
                    ws = wsl[wi % 2]; wb = wbs[wi % 2]; wi += 1
                    c0 = C_GL + n * 2048 + ds * 512
                    S.dma("pool", lambda e, ws=ws, c0=c0: e.dma_start(out=ws[:], in_=w_in[:, c0:c0 + 512].rearrange("(k p) n -> p k n", p=128)), writes=R(ws))
                    S.dma("pool", lambda e, wb=wb, n=n, ds=ds: e.dma_start(out=wb[:], in_=w_br[n, :, ds * 512:(ds + 1) * 512].rearrange("(k p) n -> p k n", p=128)), writes=R(wb))
                    for sub in range(4):
                        dg = ds * 4 + sub
                        for tt in range(2):
                            b = gi % 2; gi += 1
                            for k in range(16):
                                S.op("pe", lambda e, b=b, ws=ws, k=k, sub=sub, tt=tt: e.matmul(pg[b][:], ws[:, k, sub * 128:(sub + 1) * 128], hTo[:, k, tt * 512:(tt + 1) * 512], start=(k == 0), stop=(k == 15)),
                                     reads=R(ws, hTo), writes=R(pg[b]))
                            for k in range(4):
                                S.op("pe", lambda e, b=b, wb=wb, k=k, sub=sub, tt=tt, n=n: e.matmul(pu[b][:], wb[:, k, sub * 128:(sub + 1) * 128], brT[:, n * 4 + k, tt * 512:(tt + 1) * 512], start=(k == 0), stop=(k == 3)),
                                     reads=R(wb, brT), writes=R(pu[b]))
                            S.op("act", lambda e, b=b, n=n, dg=dg: e.activation(out=gt[b][:], in_=pg[b][:], func=AF.Sigmoid, bias=vecs[:, V_GB + n * 16 + dg:V_GB + n * 16 + dg + 1]), reads=R(pg[b], vecs), writes=R(gt[b]))
                            ya = yacc[:, sub, tt * 512:(tt + 1) * 512]
                            if n == 0:
                                S.op("dve", lambda e, b=b, ya=ya: e.tensor_tensor(ya, gt[b][:], pu[b][:], op=ALU.mult), reads=R(gt[b], pu[b]), writes=R(yacc))
                            else:
                                S.op("dve", lambda e, b=b: e.tensor_tensor(gt[b][:], gt[b][:], pu[b][:], op=ALU.mult), reads=R(gt[b], pu[b]), writes=R(gt[b]))
                                S.op("dve", lambda e, b=b, ya=ya: e.tensor_tensor(ya, ya, gt[b][:], op=ALU.add), reads=R(gt[b], yacc), writes=R(yacc))
                S.op("act", lambda e, ds=ds: e.copy(yT[:, ds * 4:(ds + 1) * 4, :], yacc[:]), reads=R(yacc), writes=R(yT))
            zi = 0
            for os_ in range(4):
                ws = wsl[wi % 2]; wi += 1
                S.dma("pool", lambda e, ws=ws, os_=os_: e.dma_start(out=ws[:], in_=w_out[:, os_ * 512:(os_ + 1) * 512].rearrange("(k p) n -> p k n", p=128)), writes=R(ws))
                for sub in range(4):
                    og = os_ * 4 + sub
                    x_ = xo[og % 2]
                    S.dma("sp", lambda e, x_=x_, og=og: e.dma_start(out=x_[:], in_=xTo[og * 128:(og + 1) * 128, :]), writes=R(x_))
                    for tt in range(2):
                        b = zi % 2; zi += 1
                        for k in range(16):
                            S.op("pe", lambda e, b=b, ws=ws, k=k, sub=sub, tt=tt: e.matmul(pz[b][:], ws[:, k, sub * 128:(sub + 1) * 128], yT[:, k, tt * 512:(tt + 1) * 512], start=(k == 0), stop=(k == 15)),
                                 reads=R(ws, yT), writes=R(pz[b]))
                        S.op("dve", lambda e, b=b, x_=x_, og=og, tt=tt: e.scalar_tensor_tensor(out=x_[:, tt * 512:(tt + 1) * 512], in0=pz[b][:], scalar=vecs[:, V_MOD + 32 + og:V_MOD + 32 + og + 1], in1=x_[:, tt * 512:(tt + 1) * 512], op0=ALU.mult, op1=ALU.add),
                             reads=R(pz[b], vecs, x_), writes=R(x_))
                    S.dma("sp", lambda e, x_=x_, og=og: e.dma_start(out=xmT[og * 128:(og + 1) * 128, :], in_=x_[:]), reads=R(x_), writes=["xmT"])

        if 4 in phases:
          with Scope() as P4:
            rw = P4.sb("rw", [128, 16, 32], F32); rb = P4.sb("rb", [1, 32], F32); onesf = P4.sb("onesf", [1, 128], F32)
            hb = [P4.sb("hb", [128, 16, TW], BF16) for _ in range(2)]
            lg = P4.sb("lg", [128, 32], F32); t8 = P4.sb("t8", [128, 8], F32); nm1 = P4.sb("nm1", [128, 1], F32)
            sel = P4.sb("sel", [128, 32], F32); ex = P4.sb("ex", [128, 32], F32); ssum = P4.sb("ssum", [128, 1], F32)
            pl = P4.ps("plg", [128, 32])
            S.dma("sp", lambda e: e.dma_start(out=rw[:], in_=rw_d), writes=R(rw))
            S.dma("sp", lambda e: e.dma_start(out=rb[:], in_=rb_d), writes=R(rb))
            S.op("dve", lambda e: e.memset(onesf[:], 1.0), writes=R(onesf))
            src = xmT if 3 in phases else xTo

            def c_h2(tt, x):
                h_ = hb[tt % 2]
                S.op("dve", lambda e, h_=h_, x=x: e.tensor_copy(h_[:], x[:]), reads=R(x), writes=R(h_))
                S.dma("sp", lambda e, h_=h_, tt=tt: e.dma_start(out=h2T[:, tt * TW:(tt + 1) * TW].rearrange("(k p) n -> p k n", p=128), in_=h_[:]), reads=R(h_), writes=["h2T"])
                for s4 in range(TW // 128):
                    for k in range(16):
                        S.op("pe", lambda e, x=x, k=k, s4=s4: e.matmul(pl[:], x[:, k, s4 * 128:(s4 + 1) * 128], rw[:, k, :], start=(k == 0), stop=False), reads=R(x, rw), writes=R(pl))
                    S.op("pe", lambda e: e.matmul(pl[:], onesf[:], rb[:], start=False, stop=True), reads=R(onesf, rb), writes=R(pl))
                    S.op("dve", lambda e: e.tensor_copy(lg[:], pl[:]), reads=R(pl), writes=R(lg))
                    S.op("dve", lambda e: e.max(out=t8[:], in_=lg[:]), reads=R(lg), writes=R(t8))
                    S.op("dve", lambda e: e.tensor_scalar(nm1[:], t8[:, 0:1], -1.0, None, op0=ALU.mult), reads=R(t8), writes=R(nm1))
                    S.op("dve", lambda e: e.tensor_scalar(sel[:], lg[:], t8[:, 3:4], None, op0=ALU.is_ge), reads=R(lg, t8), writes=R(sel))
                    S.op("act", lambda e: e.activation(out=ex[:], in_=lg[:], func=AF.Exp, bias=nm1[:, 0:1]), reads=R(lg, nm1), writes=R(ex))
                    S.op("dve", lambda e: e.tensor_tensor(ex[:], ex[:], sel[:], op=ALU.mult), reads=R(ex, sel), writes=R(ex))
                    S.op("dve", lambda e: e.reduce_sum(out=ssum[:], in_=ex[:], axis=AX.X), reads=R(ex), writes=R(ssum))
                    S.op("dve", lambda e: e.reciprocal(ssum[:], ssum[:]), reads=R(ssum), writes=R(ssum))
                    S.op("dve", lambda e: e.tensor_scalar(ex[:], ex[:], ssum[:, 0:1], None, op0=ALU.mult), reads=R(ex, ssum), writes=R(ex))
                    S.dma("sp", lambda e, tt=tt, s4=s4: e.dma_start(out=G_o[tt * TW + s4 * 128:tt * TW + (s4 + 1) * 128, :], in_=ex[:]), reads=R(ex), writes=["G"])
            def src_read_hook():
                pass
            norm_tiles_src = src
            _orig_dma = S.dma
            def dma_with_dep(q, fn, reads=(), writes=()):
                return _orig_dma(q, fn, reads=list(reads) + ["xmT"], writes=writes)
            S.dma = dma_with_dep
            norm_tiles(P4, norm_tiles_src, 1024, AB[:, 2, :], mod(3), c_h2)
            S.dma = _orig_dma
        S.finish(["xmT", "h2T", "G", "BRT"])
        S.emit()
    print("M ops", S.n_ops, "waits", S.n_waits)
    return nc


def col16(v):
    return np.ascontiguousarray(v.reshape(16, 128).T)


def prep_vecs(l, mod_b, n1g, n2g, gate_b, a_qn_g, a_kn_g, c_qn_g, c_kn_g, d_qn_g, d_kn_g, a_subln_g, lq1, lk1, lq2, lk2):
    v = np.zeros((128, NV), np.float32)
    for i in range(6):
        v[:, V_MOD + 16 * i:V_MOD + 16 * (i + 1)] = col16(mod_b[i * 2048:(i + 1) * 2048])
    v[:, V_N1G:V_N1G + 16] = col16(n1g); v[:, V_N2G:V_N2G + 16] = col16(n2g)
    v[:, V_GB:V_GB + 64] = gate_b.reshape(64, 128).T
    v[:, V_AQG] = np.tile(a_qn_g, 2); v[:, V_AKG] = np.tile(a_kn_g, 2)
    v[:, V_CQG] = c_qn_g; v[:, V_CKG] = c_kn_g; v[:, V_DQG] = d_qn_g; v[:, V_DKG] = d_kn_g
    v[:, V_SUBLN] = a_subln_g
    v[0:64, V_LAM] = lq1; v[0:64, V_LAM + 1] = lk1; v[0:64, V_LAM + 2] = lq2; v[0:64, V_LAM + 3] = lk2
    li = 0.8 - 0.6 * math.exp(-0.3 * l)
    v[:, V_LI] = li; v[:, V_OML] = 1.0 - li
    return v


def own_cols(p):
    return np.concatenate([np.arange(128 * (2 * j + p), 128 * (2 * j + p + 1)) for j in range(8)])


def prep_m(l, p, x_b, mod_b, P):
    cf, cb = m_consts(p)
    xT = np.ascontiguousarray(x_b.T)
    return {
        "xTa": xT, "xTo": np.ascontiguousarray(xT[:, own_cols(p)]),
        "vecs": prep_vecs(l, mod_b, P["norm1_g"], P["norm2_g"], P["gate_b"], P["a_qn_g"], P["a_kn_g"], P["c_qn_g"], P["c_kn_g"],
                          P["d_qn_g"], P["d_kn_g"], P["a_subln_g"], P["a_lam_q1"], P["a_lam_k1"], P["a_lam_q2"], P["a_lam_k2"]),
        "cf": cf, "cb": cb, "w_in": P["w_in"], "w_br": P["w_branch"], "w_out": P["w_out"],
        "rw": np.ascontiguousarray(P["router_w"].reshape(16, 128, 32).transpose(1, 0, 2)), "rb": np.ascontiguousarray(P["router_b"].reshape(1, 32)),
    }


_PROGS = {}


def _prog(name, fn):
    if name not in _PROGS:
        _PROGS[name] = fn()
    return _PROGS[name]


def _run(nc, in_maps):
    res = run_bass_kernel_spmd(nc, in_maps, core_ids=list(range(len(in_maps))))
    return res.results


def kernel(x, c, ada_w, ada_b, norm1_g, norm2_g, w_in, gate_b, a_qn_g, a_kn_g, a_lam_q1, a_lam_k1,
           a_lam_q2, a_lam_k2, a_subln_g, c_qn_g, c_kn_g, d_qn_g, d_kn_g, w_branch, w_out,
           router_w, router_b, w1, b1, w2, b2):
    f32 = np.float32
    x = np.asarray(x, f32); c = np.asarray(c, f32)
    L = 2
    nc0 = _prog("l0", build_l0)
    cT = np.ascontiguousarray(c.T)
    ada_w = np.asarray(ada_w, f32); ada_b = np.asarray(ada_b, f32)
    r0 = _run(nc0, [{"cT": cT, "w": np.ascontiguousarray(ada_w[:, :, i * 1536:(i + 1) * 1536]),
                     "b": np.ascontiguousarray(ada_b[:, i * 1536:(i + 1) * 1536])} for i in range(8)])
    mod = np.concatenate([r["mod"] for r in r0], axis=2)
    ncm = _prog("m", build_m); nce = _prog("e2", build_e2); ncc = _prog("c2", build_c2)
    for l in range(L):
        P = dict(norm1_g=norm1_g[l], norm2_g=norm2_g[l], w_in=np.asarray(w_in[l], f32), gate_b=gate_b[l], a_qn_g=a_qn_g[l], a_kn_g=a_kn_g[l],
                 a_lam_q1=a_lam_q1[l], a_lam_k1=a_lam_k1[l], a_lam_q2=a_lam_q2[l], a_lam_k2=a_lam_k2[l], a_subln_g=a_subln_g[l],
                 c_qn_g=c_qn_g[l], c_kn_g=c_kn_g[l], d_qn_g=d_qn_g[l], d_kn_g=d_kn_g[l], w_branch=np.asarray(w_branch[l], f32),
                 w_out=np.asarray(w_out[l], f32), router_w=np.asarray(router_w[l], f32), router_b=np.asarray(router_b[l], f32))
        P = {k: np.asarray(v, f32) for k, v in P.items()}
        ims = [prep_m(l, core % 2, x[core // 2], mod[l, core // 2], P) for core in range(8)]
        rm = _run(ncm, ims)
        del ims
        h2_all = np.ascontiguousarray(np.concatenate([r["h2T"].T for r in rm], axis=0))
        G_all = np.concatenate([r["G"] for r in rm], axis=0)
        w1l = np.asarray(w1[l], f32); w2l = np.asarray(w2[l], f32); b1l = np.asarray(b1[l], f32); b2l = np.asarray(b2[l], f32)
        cst = e2_consts()
        ime = []
        for ec in range(8):
            es = slice(4 * ec, 4 * ec + 4)
            ime.append({"h2": h2_all, "Gtok": np.ascontiguousarray(G_all[:, es]), "w1": np.ascontiguousarray(w1l[es]),
                        "b1t_in": np.ascontiguousarray(b1l[es].reshape(4, 32, 128).transpose(2, 0, 1)), "w2": np.ascontiguousarray(w2l[es]),
                        "b2": np.ascontiguousarray(b2l[es]), "cst": cst})
        re_ = _run(nce, ime)
        del ime
        imc = []
        for core in range(8):
            P8 = np.ascontiguousarray(np.stack([re_[ec]["y"][core * 1024:(core + 1) * 1024] for ec in range(8)], axis=0))
            imc.append({"P8": P8, "xm_in": np.ascontiguousarray(rm[core]["xmT"].T),
                        "g2row": np.ascontiguousarray(mod[l, core // 2, 5 * 2048:6 * 2048].reshape(1, 2048))})
        rc = _run(ncc, imc)
        xn = np.empty_like(x)
        for core in range(8):
            xn[core // 2][own_cols(core % 2)] = rc[core]["xn"]
        x = xn
    return x
```

```python
import math
import numpy as np
import ml_dtypes
from contextlib import ExitStack
import concourse.bass as bass
import concourse.mybir as mybir
from concourse.bass_utils import run_bass_kernel_spmd


ENGS = ("pe", "act", "dve", "pool", "sp")


class _Rec:
    def __init__(self):
        self.call = None

    def __getattr__(self, name):
        def f(*a, **k):
            self.call = (name, a, k)
            return self
        return f


class Sched:
    def __init__(self, nc, n_dma_sems=16):
        self.nc = nc
        self.streams = {e: [] for e in ENGS}
        self.cnt = {e: 0 for e in ENGS}
        self.last_w = {}
        self.readers = {}
        self.waited = {e: {} for e in ENGS}
        self.n_dma = n_dma_sems
        self.dma_next = 0
        self.dma_val = [0] * n_dma_sems
        self.n_ops = 0
        self.n_waits = 0

    def _need(self, eng, ev):
        key, val = ev
        if self.waited[eng].get(key, 0) >= val:
            return
        self.waited[eng][key] = val
        self.streams[eng].append(("wait", key, val))
        self.n_waits += 1

    def _deps(self, eng, reads, writes):
        for r in reads:
            ev = self.last_w.get(r)
            if ev is not None:
                self._dep1(eng, ev)
        for w in writes:
            ev = self.last_w.get(w)
            if ev is not None:
                self._dep1(eng, ev)
            for k, v in self.readers.get(w, {}).items():
                self._dep1(eng, (k, v))

    def _dep1(self, eng, ev):
        if eng == "pe" and ev[0] == "pe":
            return
        self._need(eng, ev)

    def _commit(self, ev, reads, writes):
        for r in reads:
            d = self.readers.setdefault(r, {})
            if d.get(ev[0], 0) < ev[1]:
                d[ev[0]] = ev[1]
        for w in writes:
            self.last_w[w] = ev
            self.readers[w] = {}

    def op(self, eng, fn, reads=(), writes=()):
        self._deps(eng, reads, writes)
        self.cnt[eng] += 1
        ev = (eng, self.cnt[eng])
        rec = _Rec(); fn(rec)
        self.streams[eng].append(("op", rec.call, eng, 1))
        self._commit(ev, reads, writes)
        self.n_ops += 1
        return ev

    def dma(self, q, fn, reads=(), writes=()):
        self._deps(q, reads, writes)
        j = self.dma_next
        self.dma_next = (j + 1) % self.n_dma
        key = ("dma", j)
        if self.dma_val[j] > 0:
            self._need(q, (key, self.dma_val[j]))
        self.dma_val[j] += 16
        ev = (key, self.dma_val[j])
        rec = _Rec(); fn(rec)
        self.streams[q].append(("op", rec.call, key, 16))
        self._commit(ev, reads, writes)
        self.n_ops += 1
        return ev

    def finish(self, out_res):
        for r in out_res:
            ev = self.last_w.get(r)
            if ev is not None:
                self._need("sp", ev)

    def emit(self):
        nc = self.nc
        with ExitStack() as st:
            sems = {}
            for e in ENGS:
                sems[e] = st.enter_context(nc.semaphore("s_" + e))
            for j in range(self.n_dma):
                sems[("dma", j)] = st.enter_context(nc.semaphore("s_dma%d" % j))
            block = st.enter_context(nc.Block())

            regcache = {}

            def run(eng_obj, stream):
                for it in stream:
                    if it[0] == "wait":
                        eng_obj.wait_ge(sems[it[1]], it[2])
                    else:
                        name, a, k = it[1]
                        if name == "indirect_dma_start" and isinstance(k.get("bounds_check"), int):
                            v = k["bounds_check"]
                            if v not in regcache:
                                regcache[v] = eng_obj.to_reg(v)
                            k = dict(k); k["bounds_check"] = regcache[v]
                        try:
                            ins = getattr(eng_obj, name)(*a, **k)
                        except Exception as ex_:
                            import traceback
                            traceback.print_exception(ex_)
                            print("CAUSE", repr(ex_.__cause__), repr(ex_.__context__))
                            print("FAILED OP:", name, [str(x)[:200] for x in a], {kk: str(v)[:200] for kk, v in k.items()})
                            raise
                        ins.then_inc(sems[it[2]], it[3])

            @block.tensor
            def _(e):
                run(e, self.streams["pe"])

            @block.scalar
            def _(e):
                run(e, self.streams["act"])

            @block.vector
            def _(e):
                run(e, self.streams["dve"])

            @block.gpsimd
            def _(e):
                run(e, self.streams["pool"])

            @block.sync
            def _(e):
                run(e, self.streams["sp"])


F32 = mybir.dt.float32; BF16 = mybir.dt.bfloat16
AF = mybir.ActivationFunctionType; ALU = mybir.AluOpType
D = 2048


def build_l0(L=2, NCOL=1536):
    nc = bass.Bass("TRN2", target_bir_lowering=False)
    cT = nc.dram_tensor("cT", [D, 4], F32, kind="ExternalInput").ap()
    w = nc.dram_tensor("w", [L, D, NCOL], F32, kind="ExternalInput").ap()
    bia = nc.dram_tensor("b", [L, NCOL], F32, kind="ExternalInput").ap()
    out = nc.dram_tensor("mod", [L, 4, NCOL], F32, kind="ExternalOutput").ap()
    S = Sched(nc)
    with ExitStack() as st:
        def sb(name, shape, dt): return st.enter_context(nc.sbuf_tensor(name, shape, dt))
        def ps(name, shape, dt): return st.enter_context(nc.psum_tensor(name, shape, dt))
        cond = sb("cond", [128, 16, 4], F32)
        ones = sb("ones", [1, 4], F32)
        wt = [sb("wt%d" % i, [128, 16, 512], F32) for i in range(2)]
        bt = [sb("bt%d" % i, [1, 512], F32) for i in range(2)]
        ot = [sb("ot%d" % i, [4, 512], F32) for i in range(2)]
        pt = [ps("pt%d" % i, [4, 512], F32) for i in range(2)]
        S.dma("sp", lambda e: e.dma_start(out=cond[:], in_=cT.rearrange("(k p) b -> p k b", p=128)), writes=["cond"])
        S.op("act", lambda e: e.activation(out=cond[:], in_=cond[:], func=AF.Silu), reads=["cond"], writes=["cond"])
        S.op("dve", lambda e: e.memset(ones[:], 1.0), writes=["ones"])
        it = 0
        for l in range(L):
            for cch in range(NCOL // 512):
                s = it % 2; it += 1
                S.dma("sp", lambda e, s=s, l=l, cch=cch: e.dma_start(out=wt[s][:], in_=w[l, :, cch * 512:(cch + 1) * 512].rearrange("(k p) n -> p k n", p=128)), writes=["wt%d" % s])
                S.dma("sp", lambda e, s=s, l=l, cch=cch: e.dma_start(out=bt[s][:], in_=bia[l:l + 1, cch * 512:(cch + 1) * 512]), writes=["bt%d" % s])
                for k in range(16):
                    S.op("pe", lambda e, s=s, k=k: e.matmul(pt[s][:], cond[:, k, :], wt[s][:, k, :], start=(k == 0), stop=False),
                         reads=["cond", "wt%d" % s], writes=["pt%d" % s])
                S.op("pe", lambda e, s=s: e.matmul(pt[s][:], ones[:], bt[s][:], start=False, stop=True),
                     reads=["ones", "bt%d" % s], writes=["pt%d" % s])
                S.op("dve", lambda e, s=s: e.tensor_copy(ot[s][:], pt[s][:]), reads=["pt%d" % s], writes=["ot%d" % s])
                S.dma("sp", lambda e, s=s, l=l, cch=cch: e.dma_start(out=out[l, :, cch * 512:(cch + 1) * 512], in_=ot[s][:]), reads=["ot%d" % s], writes=["OUT"])
        S.finish(["OUT"])
        S.emit()
    return nc


F32 = mybir.dt.float32; BF16 = mybir.dt.bfloat16; I32 = mybir.dt.int32
AF = mybir.ActivationFunctionType; ALU = mybir.AluOpType
D = 2048; FF = 2048


def e2_consts():
    c = np.zeros((128, 384), np.float32)
    pp, p = np.arange(128)[:, None], np.arange(128)[None, :]
    c[:, 0:128] = (pp < p)
    c[:, 128:256] = 1.0
    c[:, 256:384] = np.eye(128)
    return c.astype(ml_dtypes.bfloat16)


def build_e2(ntok=8192, nexp=4, CAP=2560):
    nc = bass.Bass("TRN2", target_bir_lowering=False)
    NT = ntok // 128; NS = (CAP + 1023) // 1024
    def urows(s_): return min(1024, CAP - s_ * 1024)
    h2 = nc.dram_tensor("h2", [ntok, D], BF16, kind="ExternalInput").ap()
    Gtok = nc.dram_tensor("Gtok", [ntok, nexp], F32, kind="ExternalInput").ap()
    w1 = nc.dram_tensor("w1", [nexp, D, 2 * FF], F32, kind="ExternalInput").ap()
    b1 = nc.dram_tensor("b1t_in", [128, nexp, 32], F32, kind="ExternalInput").ap()
    w2 = nc.dram_tensor("w2", [nexp, FF, D], F32, kind="ExternalInput").ap()
    b2 = nc.dram_tensor("b2", [nexp, D], F32, kind="ExternalInput").ap()
    cst = nc.dram_tensor("cst", [128, 384], BF16, kind="ExternalInput").ap()
    y = nc.dram_tensor("y", [ntok, D], BF16, kind="ExternalOutput").ap()
    Xc = [nc.dram_tensor("Xc%d" % i, [CAP, D], BF16, kind="Internal").ap() for i in range(nexp)]
    Yc = [nc.dram_tensor("Yc%d" % i, [CAP, D], BF16, kind="Internal").ap() for i in range(nexp)]
    S = Sched(nc)
    uid = [0]; RN = {}; KEEP = []

    def barrier():
        for e in ("pe", "act", "dve", "pool", "sp"):
            for e2 in ("pe", "act", "dve", "pool", "sp"):
                if e != e2 and S.cnt[e2] > 0:
                    S._need(e, (e2, S.cnt[e2]))
            for j in range(S.n_dma):
                if S.dma_val[j] > 0:
                    S._need(e, (("dma", j), S.dma_val[j]))

    class Scope:
        def __init__(self): self.st = ExitStack()
        def __enter__(self): self.st.__enter__(); return self
        def __exit__(self, *a):
            barrier(); return self.st.__exit__(*a)
        def sb(self, name, shape, dt):
            uid[0] += 1
            t = self.st.enter_context(nc.sbuf_tensor("%s_%d" % (name, uid[0]), shape, dt)); RN[id(t)] = "%s_%d" % (name, uid[0]); KEEP.append(t); return t
        def ps(self, name, shape, dt=F32):
            uid[0] += 1
            t = self.st.enter_context(nc.psum_tensor("%s_%d" % (name, uid[0]), shape, dt)); RN[id(t)] = "%s_%d" % (name, uid[0]); KEEP.append(t); return t

    def R(*ts): return [t if isinstance(t, str) else RN[id(t)] for t in ts]
    NC4 = NT * nexp
    with Scope() as G0:
        cb = G0.sb("cb", [128, 384], BF16)
        Gt = G0.sb("Gt", [128, NT, nexp], F32)
        slot = G0.sb("slot", [128, NC4], I32)
        b1t = G0.sb("b1t", [128, nexp, 32], F32); b1p = G0.sb("b1p", [128, nexp, 32], F32)
        S.dma("sp", lambda e: e.dma_start(out=cb[:], in_=cst), writes=R(cb))
        S.dma("sp", lambda e: e.dma_start(out=Gt[:], in_=Gtok.rearrange("(t p) e -> p t e", p=128)), writes=R(Gt))
        S.dma("sp", lambda e: e.dma_start(out=b1t[:], in_=b1), writes=R(b1t))
        S.op("dve", lambda e: e.tensor_scalar(b1p[:], b1t[:], 1.0, None, op0=ALU.add), reads=R(b1t), writes=R(b1p))
        triu = cb[:, 0:128]; onesb = cb[:, 128:256]; idb = cb[:, 256:384]
        Gf = Gt[:].rearrange("p t e -> p (t e)")
        with Scope() as PA:
            mask = PA.sb("mask", [128, NC4], BF16); maskf = PA.sb("maskf", [128, NC4], F32)
            sa = PA.sb("sa", [128, NC4], F32); sb_ = PA.sb("sb", [128, NC4], F32); posf = PA.sb("posf", [128, NC4], F32)
            pp1 = PA.ps("pp1", [128, NC4]); ptot = PA.ps("ptot", [128, NC4])
            S.op("dve", lambda e: e.tensor_scalar(maskf[:], Gf, 0.0, None, op0=ALU.is_gt), reads=R(Gt), writes=R(maskf))
            S.op("dve", lambda e: e.tensor_copy(mask[:], maskf[:]), reads=R(maskf), writes=R(mask))
            S.op("pe", lambda e: e.matmul(pp1[:], triu, mask[:], start=True, stop=True), reads=R(cb, mask), writes=R(pp1))
            S.op("pe", lambda e: e.matmul(ptot[:], onesb, mask[:], start=True, stop=True), reads=R(cb, mask), writes=R(ptot))
            S.op("dve", lambda e: e.tensor_copy(sa[:], ptot[:]), reads=R(ptot), writes=R(sa))
            cur, oth = sa, sb_
            s = 1
            while s < NT:
                w = s * nexp
                S.op("dve", lambda e, cur=cur, oth=oth, w=w: e.tensor_tensor(oth[:, w:NC4], cur[:, w:NC4], cur[:, 0:NC4 - w], op=ALU.add), reads=R(cur), writes=R(oth))
                S.op("dve", lambda e, cur=cur, oth=oth, w=w: e.tensor_copy(oth[:, 0:w], cur[:, 0:w]), reads=R(cur), writes=R(oth))
                cur, oth = oth, cur
                s *= 2
            S.op("dve", lambda e, cur=cur: e.tensor_tensor(posf[:], cur[:], ptot[:], op=ALU.subtract), reads=R(cur, ptot), writes=R(posf))
            S.op("dve", lambda e: e.tensor_tensor(posf[:], posf[:], pp1[:], op=ALU.add), reads=R(posf, pp1), writes=R(posf))
            S.op("dve", lambda e: e.tensor_scalar(maskf[:], maskf[:], -1.0e6, 1.0e6, op0=ALU.mult, op1=ALU.add), reads=R(maskf), writes=R(maskf))
            S.op("dve", lambda e: e.tensor_tensor(posf[:], posf[:], maskf[:], op=ALU.add), reads=R(posf, maskf), writes=R(posf))
            S.op("dve", lambda e: e.tensor_copy(slot[:], posf[:]), reads=R(posf), writes=R(slot))
        with Scope() as PB:
            xt = [PB.sb("xt", [128, D], BF16) for _ in range(4)]
            for t in range(NT):
                x_ = xt[t % 4]
                S.dma("sp", lambda e, x_=x_, t=t: e.dma_start(out=x_[:], in_=h2[t * 128:(t + 1) * 128, :]), writes=R(x_))
                for ex in range(nexp):
                    S.dma("pool", lambda e, x_=x_, t=t, ex=ex: e.indirect_dma_start(out=Xc[ex], out_offset=bass.IndirectOffsetOnAxis(ap=slot[:, t * nexp + ex:t * nexp + ex + 1], axis=0),
                                                                                   in_=x_[:], in_offset=None, bounds_check=CAP - 1, oob_is_err=False),
                          reads=R(x_, slot), writes=["Xc%d" % ex])
        with Scope() as PD:
            Xrs = [PD.sb("Xr", [128, 8, D], BF16) for _ in range(2)]
            xri = [0]
            XT = PD.sb("XT", [128, 16, 1024], BF16)
            actT = PD.sb("actT", [128, 16, 1024], BF16)
            Yrs = [PD.sb("Yr", [128, 8, 512], BF16) for _ in range(2)]
            wsl = [PD.sb("wsl", [128, 16, 512], BF16) for _ in range(2)]
            b2bc = PD.sb("b2bc", [128, D], F32)
            tmp = {n: [PD.sb(n, [128, 512], F32) for _ in range(2)] for n in ("xg", "sg", "xl")}
            pg = [PD.ps("pg", [128, 512]) for _ in range(2)]
            pl = [PD.ps("pl", [128, 512]) for _ in range(2)]
            py = [PD.ps("py", [128, 512]) for _ in range(2)]
            ptr = [PD.ps("ptr", [128, 512], BF16) for _ in range(2)]
            wi = 0; ti = 0; yi = 0; tri = 0
            units = [(ex, s_) for ex in range(nexp) for s_ in range(NS)]

            def load_x(u):
                ex, s_ = units[u]
                r0 = s_ * 1024
                Xr = Xrs[u % 2]
                nr = urows(s_)
                S.dma("sp", lambda e: e.dma_start(out=Xr[:, 0:nr // 128, :], in_=Xc[ex][r0:r0 + nr, :].rearrange("(r p) f -> p r f", p=128)), reads=["Xc%d" % ex], writes=R(Xr))

            def prep(u):
                Xr = Xrs[u % 2]
                for fc in range(16):
                    for half in range(urows(units[u][1]) // 512):
                        pt_ = ptr[(fc * 2 + half) % 2]
                        for r4 in range(4):
                            rt = half * 4 + r4
                            S.op("pe", lambda e: e.transpose(pt_[:, r4 * 128:(r4 + 1) * 128], Xr[:, rt, fc * 128:(fc + 1) * 128], idb), reads=R(Xr, cb), writes=R(pt_))
                        if (fc + half) % 2 == 0:
                            S.op("act", lambda e: e.copy(XT[:, fc, half * 512:(half + 1) * 512], pt_[:]), reads=R(pt_), writes=R(XT))
                        else:
                            S.op("dve", lambda e: e.tensor_copy(XT[:, fc, half * 512:(half + 1) * 512], pt_[:]), reads=R(pt_), writes=R(XT))
            load_x(0)
            prep(0)
            if len(units) > 1:
                load_x(1)
            for u, (ex, s_) in enumerate(units):
                r0 = s_ * 1024
                if s_ == 0:
                    S.dma("sp", lambda e, ex=ex: e.dma_start(out=b2bc[:], in_=b2[ex:ex + 1, :].partition_broadcast(128)), writes=R(b2bc))
                for sl in range(8):
                    ws = wsl[wi % 2]; wi += 1
                    c0 = sl * 256
                    S.dma("pool", lambda e, ws=ws, ex=ex, c0=c0: e.dma_start(out=ws[:, :, 0:256], in_=w1[ex, :, c0:c0 + 256].rearrange("(k p) n -> p k n", p=128)), writes=R(ws))
                    S.dma("pool", lambda e, ws=ws, ex=ex, c0=c0: e.dma_start(out=ws[:, :, 256:512], in_=w1[ex, :, FF + c0:FF + c0 + 256].rearrange("(k p) n -> p k n", p=128)), writes=R(ws))
                    for sub in range(2):
                        j = sl * 2 + sub
                        for tt in range(urows(s_) // 512):
                            b = ti % 2; ti += 1
                            for k in range(16):
                                S.op("pe", lambda e, b=b, ws=ws, k=k, sub=sub, tt=tt: e.matmul(pg[b][:], ws[:, k, sub * 128:(sub + 1) * 128], XT[:, k, tt * 512:(tt + 1) * 512], start=(k == 0), stop=(k == 15)),
                                     reads=R(ws, XT), writes=R(pg[b]))
                            for k in range(16):
                                S.op("pe", lambda e, b=b, ws=ws, k=k, sub=sub, tt=tt: e.matmul(pl[b][:], ws[:, k, 256 + sub * 128:256 + (sub + 1) * 128], XT[:, k, tt * 512:(tt + 1) * 512], start=(k == 0), stop=(k == 15)),
                                     reads=R(ws, XT), writes=R(pl[b]))
                            xg, sg, xl = tmp["xg"][b], tmp["sg"][b], tmp["xl"][b]
                            S.op("dve", lambda e, b=b, xg=xg, ex=ex, j=j: e.tensor_scalar(xg[:], pg[b][:], b1t[:, ex, j:j + 1], 7.0, op0=ALU.add, op1=ALU.min), reads=R(pg[b], b1t), writes=R(xg))
                            S.op("act", lambda e, xg=xg, sg=sg: e.activation(out=sg[:], in_=xg[:], func=AF.Sigmoid, scale=1.702), reads=R(xg), writes=R(sg))
                            S.op("act", lambda e, b=b, xl=xl, ex=ex, j=j: e.activation(out=xl[:], in_=pl[b][:], func=AF.Identity, bias=b1p[:, ex, 16 + j:17 + j]), reads=R(pl[b], b1p), writes=R(xl))
                            S.op("dve", lambda e, xl=xl: e.tensor_scalar(xl[:], xl[:], 8.0, -6.0, op0=ALU.min, op1=ALU.max), reads=R(xl), writes=R(xl))
                            S.op("dve", lambda e, xl=xl, xg=xg: e.tensor_tensor(xg[:], xg[:], xl[:], op=ALU.mult), reads=R(xl, xg), writes=R(xg))
                            S.op("dve", lambda e, sg=sg, xg=xg, j=j, tt=tt: e.tensor_tensor(actT[:, j, tt * 512:(tt + 1) * 512], sg[:], xg[:], op=ALU.mult), reads=R(sg, xg), writes=R(actT))
                if u + 1 < len(units):
                    prep(u + 1)
                    if u + 2 < len(units):
                        load_x(u + 2)
                for sl in range(4):
                    ws = wsl[wi % 2]; wi += 1
                    Yr = Yrs[sl % 2]
                    c0 = sl * 512
                    S.dma("pool", lambda e, ws=ws, ex=ex, c0=c0: e.dma_start(out=ws[:], in_=w2[ex, :, c0:c0 + 512].rearrange("(k p) n -> p k n", p=128)), writes=R(ws))
                    for jt in range(urows(s_) // 128):
                        b = yi % 2; yi += 1
                        for k in range(16):
                            S.op("pe", lambda e, b=b, ws=ws, k=k, jt=jt: e.matmul(py[b][:], actT[:, k, jt * 128:(jt + 1) * 128], ws[:, k, :], start=(k == 0), stop=(k == 15)),
                                 reads=R(ws, actT), writes=R(py[b]))
                        S.op("dve", lambda e, b=b, jt=jt, c0=c0, Yr=Yr: e.tensor_tensor(Yr[:, jt, :], py[b][:], b2bc[:, c0:c0 + 512], op=ALU.add), reads=R(py[b], b2bc), writes=R(Yr))
                    S.dma("sp", lambda e, ex=ex, r0=r0, c0=c0, Yr=Yr: e.dma_start(out=Yc[ex][r0:r0 + urows(s_), c0:c0 + 512].rearrange("(r p) f -> p r f", p=128), in_=Yr[:, 0:urows(s_) // 128, :]), reads=R(Yr), writes=["Yc%d" % ex])
        with Scope() as PE_:
            ygt = PE_.sb("ygt", [128, 2 * nexp, D], BF16)
            RNX = {}
            class _V:
                def __init__(self, i, ex): self.i = i; self.ex = ex
                def __getitem__(self, k): return ygt[:, self.i * nexp + self.ex, :]
            yg = [[_V(i, ex) for ex in range(nexp)] for i in range(2)]
            for i in range(2):
                for ex in range(nexp):
                    RN[id(yg[i][ex])] = "yg_%d_%d" % (i, ex)
            acc = [PE_.sb("acc", [128, D], F32) for _ in range(2)]
            ob = [PE_.sb("ob", [128, D], BF16) for _ in range(2)]
            for i in range(2):
                for ex in range(nexp):
                    S.op("dve", lambda e, i=i, ex=ex: e.memset(yg[i][ex][:], 0.0), writes=R(yg[i][ex]))
            for t in range(NT):
                i = t % 2
                for ex in range(nexp):
                    S.dma("pool", lambda e, i=i, ex=ex, t=t: e.indirect_dma_start(out=yg[i][ex][:], out_offset=None, in_=Yc[ex],
                                                                                  in_offset=bass.IndirectOffsetOnAxis(ap=slot[:, t * nexp + ex:t * nexp + ex + 1], axis=0),
                                                                                  bounds_check=CAP - 1, oob_is_err=False),
                          reads=["Yc%d" % ex] + R(slot, yg[i][ex]), writes=R(yg[i][ex]))
                for ex in range(nexp):
                    if ex == 0:
                        S.op("act", lambda e, i=i, t=t: e.activation(out=acc[i][:], in_=yg[i][0][:], func=AF.Identity, scale=Gt[:, t, 0:1]), reads=R(yg[i][0], Gt), writes=R(acc[i]))
                    else:
                        last = (ex == nexp - 1)
                        dst = ob[i] if last else acc[i]
                        S.op("dve", lambda e, i=i, t=t, ex=ex, dst=dst: e.scalar_tensor_tensor(out=dst[:], in0=yg[i][ex][:], scalar=Gt[:, t, ex:ex + 1], in1=acc[i][:], op0=ALU.mult, op1=ALU.add),
                             reads=R(yg[i][ex], Gt, acc[i]), writes=R(dst))
                S.dma("sp", lambda e, i=i, t=t: e.dma_start(out=y[t * 128:(t + 1) * 128, :], in_=ob[i][:]), reads=R(ob[i]), writes=["OUT"])
        S.finish(["OUT"])
        S.emit()
    print("E2 ops", S.n_ops, "waits", S.n_waits)
    return nc


F32 = mybir.dt.float32; BF16 = mybir.dt.bfloat16
AF = mybir.ActivationFunctionType; ALU = mybir.AluOpType
D = 2048


def build_c2():
    nc = bass.Bass("TRN2", target_bir_lowering=False)
    P8 = nc.dram_tensor("P8", [8, 1024, D], BF16, kind="ExternalInput").ap()
    xm = nc.dram_tensor("xm_in", [1024, D], F32, kind="ExternalInput").ap()
    g2 = nc.dram_tensor("g2row", [1, D], F32, kind="ExternalInput").ap()
    xn = nc.dram_tensor("xn", [1024, D], F32, kind="ExternalOutput").ap()
    S = Sched(nc)
    with ExitStack() as st:
        def sb(name, shape, dt): return st.enter_context(nc.sbuf_tensor(name, shape, dt))
        g2t = sb("g2t", [128, D], F32)
        pt = [sb("pt%d" % i, [128, 8, D], BF16) for i in range(2)]
        xt = [sb("xt%d" % i, [128, D], F32) for i in range(2)]
        acc = [sb("acc%d" % i, [128, D], F32) for i in range(2)]
        acb = [sb("acb%d" % i, [128, D], F32) for i in range(2)]
        S.dma("sp", lambda e: e.dma_start(out=g2t[:], in_=g2.partition_broadcast(128)), writes=["g2t"])
        for k in range(8):
            s = k % 2
            S.dma("sp", lambda e, s=s, k=k: e.dma_start(out=pt[s][:], in_=P8[:, k * 128:(k + 1) * 128, :].rearrange("c p n -> p c n")), writes=["pt%d" % s])
            S.dma("sp", lambda e, s=s, k=k: e.dma_start(out=xt[s][:], in_=xm[k * 128:(k + 1) * 128, :]), writes=["xt%d" % s])
            S.op("dve", lambda e, s=s: e.tensor_tensor(acc[s][:], pt[s][:, 0, :], pt[s][:, 1, :], op=ALU.add), reads=["pt%d" % s], writes=["acc%d" % s])
            S.op("pool", lambda e, s=s: e.tensor_tensor(acb[s][:], pt[s][:, 4, :], pt[s][:, 5, :], op=ALU.add), reads=["pt%d" % s], writes=["acb%d" % s])
            for c in (2, 3):
                S.op("dve", lambda e, s=s, c=c: e.tensor_tensor(acc[s][:], acc[s][:], pt[s][:, c, :], op=ALU.add), reads=["pt%d" % s, "acc%d" % s], writes=["acc%d" % s])
            for c in (6, 7):
                S.op("pool", lambda e, s=s, c=c: e.tensor_tensor(acb[s][:], acb[s][:], pt[s][:, c, :], op=ALU.add), reads=["pt%d" % s, "acb%d" % s], writes=["acb%d" % s])
            S.op("dve", lambda e, s=s: e.tensor_tensor(acc[s][:], acc[s][:], acb[s][:], op=ALU.add), reads=["acb%d" % s, "acc%d" % s], writes=["acc%d" % s])
            S.op("dve", lambda e, s=s: e.tensor_tensor(acc[s][:], acc[s][:], g2t[:], op=ALU.mult), reads=["acc%d" % s, "g2t"], writes=["acc%d" % s])
            S.op("dve", lambda e, s=s: e.tensor_tensor(xt[s][:], xt[s][:], acc[s][:], op=ALU.add), reads=["acc%d" % s, "xt%d" % s], writes=["xt%d" % s])
            S.dma("sp", lambda e, s=s, k=k: e.dma_start(out=xn[k * 128:(k + 1) * 128, :], in_=xt[s][:]), reads=["xt%d" % s], writes=["OUT"])
        S.finish(["OUT"])
        S.emit()
    return nc


F32 = mybir.dt.float32; BF16 = mybir.dt.bfloat16
AF = mybir.ActivationFunctionType; ALU = mybir.AluOpType; AX = mybir.AxisListType
D = 2048
NIN = 14152
EPS = 1e-6
TW = 256
SLOPES = [2.0 ** (-8.0 * (i + 1) / 12) for i in range(12)]
S_A, S_C, S_D = SLOPES[0::3], SLOPES[1::3], SLOPES[2::3]
C_AQ, C_AK, C_AV, C_BQ, C_BK, C_BV, C_CQ, C_CK, C_CV = 0, 512, 1024, 1536, 2048, 2560, 3072, 3584, 4096
C_DQ, C_DK, C_DV, C_IQ, C_IK, C_IW, C_GL = 4608, 5120, 5248, 5376, 5888, 5952, 5960
O_BF, O_BD, O_BM, O_TMA, O_MG, O_OWN, O_IDF, O_UNEG, O_ONEG, NCF = 0, 512, 4608, 8704, 8960, 9024, 9088, 9216, 9344, 9472
O_IDB, O_ONESB, O_BLK64, O_EALL, NCB = 0, 128, 256, 384, 1408
V_MOD, V_N1G, V_N2G, V_GB, V_AQG, V_AKG, V_CQG, V_CKG, V_DQG, V_DKG, V_SUBLN, V_LAM, V_LI, V_OML, NV = 0, 96, 112, 128, 192, 193, 194, 195, 196, 197, 198, 199, 203, 204, 208
NEG = -1.0e9


def m_consts(p):
    k = np.arange(128)[:, None].astype(np.float32); q = np.arange(128)[None, :].astype(np.float32)
    kq = k - q
    one = np.ones((128, 128), np.float32)
    cf = np.zeros((128, NCF), np.float32)
    cf[:, O_BF:O_BF + 512] = np.concatenate([kq - 128 * p - 256 * jj for jj in range(4)], axis=1)
    for r in range(8):
        for jj in range(4):
            d = r - 2 * jj - p
            if d < 0:
                bd = kq + 128 * d; bm = one
            elif d == 0:
                bd = np.where(k <= q, kq, NEG); bm = (k < q).astype(np.float32)
            else:
                bd = NEG * one; bm = 0 * one
            cf[:, O_BD + r * 512 + jj * 128:O_BD + r * 512 + (jj + 1) * 128] = bd
            cf[:, O_BM + r * 512 + jj * 128:O_BM + r * 512 + (jj + 1) * 128] = bm
    qq = np.arange(128)[:, None]; ss = np.arange(128)[None, :]
    tri = np.where(ss <= qq, 0.0, -1e30).astype(np.float32)
    blkA = np.zeros((128, 128), np.float32) if p == 1 else tri
    blkB = tri if p == 1 else np.full((128, 128), -1e30, np.float32)
    cf[:, O_TMA:O_TMA + 128] = blkA; cf[:, O_TMA + 128:O_TMA + 256] = blkB
    for j in range(8):
        for n in range(8):
            cf[:, O_MG + j * 8 + n] = 0.0 if n < j else -1e30
            cf[:, O_OWN + j * 8 + n] = 1.0 if n == j else 0.0
    cf[:, O_IDF:O_IDF + 128] = np.eye(128)
    jj_, s_ = np.arange(128)[:, None], np.arange(128)[None, :]
    cf[:, O_UNEG:O_UNEG + 128] = np.where(jj_ >= s_, -1.0, 0.0)
    cf[:, O_ONEG:O_ONEG + 128] = -1.0
    cb = np.zeros((128, NCB), np.float32)
    cb[:, O_IDB:O_IDB + 128] = np.eye(128)
    cb[:, O_ONESB:O_ONESB + 128] = 1.0
    cb[0:64, O_BLK64:O_BLK64 + 64] = 1.0; cb[64:128, O_BLK64 + 64:O_BLK64 + 128] = 1.0
    for n in range(8):
        cb[n, O_EALL + n * 128:O_EALL + (n + 1) * 128] = 1.0
    return cf, cb.astype(ml_dtypes.bfloat16)


def build_m(dbg=False, phases=(1, 2, 3, 4), mixers="ABCD"):
    nc = bass.Bass("TRN2", target_bir_lowering=False)
    def din(name, shape, dt=F32): return nc.dram_tensor(name, shape, dt, kind="ExternalInput").ap()
    def dout(name, shape, dt=F32): return nc.dram_tensor(name, shape, dt, kind="ExternalOutput").ap()
    def dscr(name, shape, dt=BF16): return nc.dram_tensor(name, shape, dt, kind="Internal").ap()
    xTa = din("xTa", [D, 2048]); xTo = din("xTo", [D, 1024])
    vecs_d = din("vecs", [128, NV]); cf_d = din("cf", [128, NCF]); cb_d = din("cb", [128, NCB], BF16)
    w_in = din("w_in", [D, NIN]); w_br = din("w_br", [4, 512, D]); w_out = din("w_out", [D, D])
    rw_d = din("rw", [128, 16, 32]); rb_d = din("rb", [1, 32])
    xmT = dout("xmT", [D, 1024]); h2T = dout("h2T", [D, 1024], BF16); G_o = dout("G", [1024, 32])
    QT = dscr("QT", [5, 512, 1024]); KT = dscr("KT", [4, 512, 2048]); Vd = dscr("Vd", [2048, 1664])
    IW = dscr("IW", [1024, 8], F32)
    hTo_d = dscr("hTo_d", [D, 1024])
    if dbg:
        BRT = dout("BRT", [D, 1024], BF16)
    else:
        BRT = dscr("BRT", [D, 1024])
    S = Sched(nc)
    uid = [0]; RN = {}; KEEP = []

    def barrier():
        for e in ("pe", "act", "dve", "pool", "sp"):
            for e2 in ("pe", "act", "dve", "pool", "sp"):
                if e != e2 and S.cnt[e2] > 0:
                    S._need(e, (e2, S.cnt[e2]))
            for j in range(S.n_dma):
                if S.dma_val[j] > 0:
                    S._need(e, (("dma", j), S.dma_val[j]))

    class Scope:
        def __init__(self): self.st = ExitStack()
        def __enter__(self): self.st.__enter__(); return self
        def __exit__(self, *a):
            barrier(); return self.st.__exit__(*a)
        def sb(self, name, shape, dt):
            uid[0] += 1
            t = self.st.enter_context(nc.sbuf_tensor("%s_%d" % (name, uid[0]), shape, dt)); RN[id(t)] = "%s_%d" % (name, uid[0]); KEEP.append(t); return t
        def ps(self, name, shape, dt=F32):
            uid[0] += 1
            t = self.st.enter_context(nc.psum_tensor("%s_%d" % (name, uid[0]), shape, dt)); RN[id(t)] = "%s_%d" % (name, uid[0]); KEEP.append(t); return t

    def R(*ts): return [t if isinstance(t, str) else RN[id(t)] for t in ts]

    with Scope() as G0:
        vecs = G0.sb("vecs", [128, NV], F32); cf = G0.sb("cf", [128, NCF], F32); cb = G0.sb("cb", [128, NCB], BF16)
        AB = G0.sb("AB", [128, 4, 16], F32)
        epsc = G0.sb("epsc", [128, 1], F32)
        S.op("dve", lambda e: e.memset(epsc[:], EPS), writes=R(epsc))
        S.dma("sp", lambda e: e.dma_start(out=vecs[:], in_=vecs_d), writes=R(vecs))
        S.dma("sp", lambda e: e.dma_start(out=cf[:], in_=cf_d), writes=R(cf))
        S.dma("sp", lambda e: e.dma_start(out=cb[:], in_=cb_d), writes=R(cb))
        mod = lambda i: vecs[:, V_MOD + 16 * i:V_MOD + 16 * (i + 1)]
        for (ai, sci, gi) in ((0, 1, V_N1G), (2, 4, V_N2G)):
            S.op("dve", lambda e, ai=ai, sci=sci: e.tensor_scalar(AB[:, ai, :], mod(sci), 1.0, None, op0=ALU.add), reads=R(vecs), writes=R(AB))
            S.op("dve", lambda e, ai=ai, gi=gi: e.tensor_tensor(AB[:, ai, :], AB[:, ai, :], vecs[:, gi:gi + 16], op=ALU.mult), reads=R(vecs, AB), writes=R(AB))
        onesb = cb[:, O_ONESB:O_ONESB + 128]; idb = cb[:, O_IDB:O_IDB + 128]; blk64 = cb[:, O_BLK64:O_BLK64 + 128]
        idf = cf[:, O_IDF:O_IDF + 128]

        def norm_tiles(sc, src, ntok, Acol, Bcol, consume):
            xs = [sc.sb("xs", [128, 16, TW], F32) for _ in range(2)]
            sq = sc.sb("sq", [128, 16, TW], BF16)
            rstd = sc.sb("rstd", [128, TW], F32)
            pss = sc.ps("pss", [128, TW])
            for tt in range(ntok // TW):
                x = xs[tt % 2]
                S.dma("sp", lambda e, x=x, tt=tt: e.dma_start(out=x[:], in_=src[:, tt * TW:(tt + 1) * TW].rearrange("(k p) n -> p k n", p=128)), writes=R(x))
                S.op("act", lambda e, x=x: e.activation(out=sq[:], in_=x[:], func=AF.Square), reads=R(x), writes=R(sq))
                for k in range(16):
                    S.op("pe", lambda e, k=k: e.matmul(pss[:], onesb, sq[:, k, :], start=(k == 0), stop=(k == 15)), reads=R(sq, cb), writes=R(pss))
                S.op("act", lambda e: e.activation(out=rstd[:], in_=pss[:], func=AF.Ln, bias=epsc[:, 0:1], scale=1.0 / D), reads=R(pss, epsc), writes=R(rstd))
                S.op("act", lambda e: e.activation(out=rstd[:], in_=rstd[:], func=AF.Exp, scale=-0.5), reads=R(rstd), writes=R(rstd))
                for k in range(16):
                    S.op("dve", lambda e, x=x, k=k: e.tensor_tensor(x[:, k, :], x[:, k, :], rstd[:], op=ALU.mult), reads=R(x, rstd), writes=R(x))
                    S.op("act", lambda e, x=x, k=k: e.activation(out=x[:, k, :], in_=x[:, k, :], func=AF.Identity, bias=Bcol[:, k:k + 1], scale=Acol[:, k:k + 1]),
                         reads=R(x, AB, vecs), writes=R(x))
                consume(tt, x)

        if 1 in phases:
          with Scope() as P1:
            hTa = P1.sb("hTa", [128, 16, 2048], BF16)
            hTo = P1.sb("hTo", [128, 16, 1024], BF16)
            gq = P1.sb("gq", [128, 8], F32)
            S.op("dve", lambda e: e.tensor_scalar(gq[:, 0:1], vecs[:, V_AQG:V_AQG + 1], 64 ** -0.5, None, op0=ALU.mult), reads=R(vecs), writes=R(gq))
            S.op("dve", lambda e: e.tensor_copy(gq[:, 1:2], vecs[:, V_AKG:V_AKG + 1]), reads=R(vecs), writes=R(gq))
            S.op("dve", lambda e: e.tensor_scalar(gq[:, 2:3], vecs[:, V_CQG:V_CQG + 1], 128 ** -0.5, None, op0=ALU.mult), reads=R(vecs), writes=R(gq))
            S.op("dve", lambda e: e.tensor_copy(gq[:, 3:4], vecs[:, V_CKG:V_CKG + 1]), reads=R(vecs), writes=R(gq))
            S.op("dve", lambda e: e.tensor_scalar(gq[:, 4:5], vecs[:, V_DQG:V_DQG + 1], 128 ** -0.5, None, op0=ALU.mult), reads=R(vecs), writes=R(gq))
            S.op("dve", lambda e: e.tensor_copy(gq[:, 5:6], vecs[:, V_DKG:V_DKG + 1]), reads=R(vecs), writes=R(gq))
            with Scope() as N1:
                def c_all(tt, x):
                    S.op("dve", lambda e, tt=tt, x=x: e.tensor_copy(hTa[:, :, tt * TW:(tt + 1) * TW], x[:]), reads=R(x), writes=R(hTa))
                norm_tiles(N1, xTa, 2048, AB[:, 0, :], mod(0), c_all)
            with Scope() as N1:
                def c_own(tt, x):
                    S.op("dve", lambda e, tt=tt, x=x: e.tensor_copy(hTo[:, :, tt * TW:(tt + 1) * TW], x[:]), reads=R(x), writes=R(hTo))
                norm_tiles(N1, xTo, 1024, AB[:, 0, :], mod(0), c_own)
            S.dma("sp", lambda e: e.dma_start(out=hTo_d.rearrange("(k p) n -> p k n", p=128), in_=hTo[:]), reads=R(hTo), writes=["hTo_d"])
            with Scope() as PJ:
                wsl = [PJ.sb("wsl", [128, 16, 512], BF16) for _ in range(2)]
                stg = [PJ.sb("stg", [128, 512], BF16) for _ in range(3)]
                stgf = PJ.sb("stgf", [128, 8], F32)
                sqn = [PJ.sb("sqn", [128, 512], BF16) for _ in range(2)]
                rs = [PJ.sb("rs", [128, 512], F32) for _ in range(2)]
                tq = [PJ.sb("tq", [128, 512], F32) for _ in range(2)]
                pp = [PJ.ps("pp", [128, 512]) for _ in range(3)]
                pn = [PJ.ps("pn", [128, 512]) for _ in range(2)]
                cnt = {"w": 0, "p": 0, "s": 0, "n": 0}

                def load_slab(c0, ncols):
                    ws = wsl[cnt["w"] % 2]; cnt["w"] += 1
                    S.dma("pool", lambda e, ws=ws: e.dma_start(out=ws[:, :, 0:ncols], in_=w_in[:, c0:c0 + ncols].rearrange("(k p) n -> p k n", p=128)), writes=R(ws))
                    return ws

                def proj_fm(c0, ncols, src, ntok, dst_fn, normed, gcol, blkmat, dh, cscale=1.0, rows=128):
                    ws = load_slab(c0, ncols)
                    for sub in range(max(1, ncols // 128)):
                        for tt in range(ntok // 512):
                            p_ = pp[cnt["p"] % 3]; cnt["p"] += 1
                            sg_ = stg[cnt["s"] % 3]; cnt["s"] += 1
                            for k in range(16):
                                S.op("pe", lambda e, p_=p_, ws=ws, k=k, sub=sub, tt=tt: e.matmul(p_[0:rows, :], ws[:, k, sub * 128:sub * 128 + rows], src[:, k, tt * 512:(tt + 1) * 512], start=(k == 0), stop=(k == 15)),
                                     reads=R(ws, src), writes=R(p_))
                            if normed:
                                i = cnt["n"] % 2; cnt["n"] += 1
                                S.op("act", lambda e, p_=p_, i=i: e.activation(out=sqn[i][:], in_=p_[:], func=AF.Square), reads=R(p_), writes=R(sqn[i]))
                                S.op("pe", lambda e, i=i: e.matmul(pn[i][:], blkmat, sqn[i][:], start=True, stop=True), reads=R(sqn[i], cb), writes=R(pn[i]))
                                S.op("act", lambda e, i=i: e.activation(out=rs[i][:], in_=pn[i][:], func=AF.Ln, bias=epsc[:, 0:1], scale=1.0 / dh), reads=R(pn[i], epsc), writes=R(rs[i]))
                                S.op("act", lambda e, i=i: e.activation(out=rs[i][:], in_=rs[i][:], func=AF.Exp, scale=-0.5), reads=R(rs[i]), writes=R(rs[i]))
                                S.op("dve", lambda e, i=i, p_=p_: e.tensor_tensor(tq[i][:], p_[:], rs[i][:], op=ALU.mult), reads=R(p_, rs[i]), writes=R(tq[i]))
                                S.op("act", lambda e, i=i, sg_=sg_: e.activation(out=sg_[:], in_=tq[i][:], func=AF.Identity, scale=gcol), reads=R(tq[i], gq), writes=R(sg_))
                            else:
                                S.op("act", lambda e, p_=p_, sg_=sg_: e.activation(out=sg_[0:rows, :], in_=p_[0:rows, :], func=AF.Identity, scale=cscale), reads=R(p_), writes=R(sg_))
                            dst, dres = dst_fn(sub, tt)
                            S.dma("sp", lambda e, dst=dst, sg_=sg_: e.dma_start(out=dst, in_=sg_[0:rows, :]), reads=R(sg_), writes=[dres])

                def qdst(m):
                    return lambda sub, tt: (QT[m, sub * 128:(sub + 1) * 128, tt * 512:(tt + 1) * 512], "QT%d" % m)

                def kdst(m, r0=0, rows=128):
                    return lambda sub, tt: (KT[m, r0 + sub * 128:r0 + sub * 128 + rows, tt * 512:(tt + 1) * 512], "KT%d" % m)
                proj_fm(C_AQ, 512, hTo, 1024, qdst(0), True, gq[:, 0:1], blk64, 64)
                proj_fm(C_BQ, 512, hTo, 1024, qdst(1), False, None, None, 0, cscale=128 ** -0.5)
                proj_fm(C_CQ, 512, hTo, 1024, qdst(2), True, gq[:, 2:3], onesb, 128)
                proj_fm(C_DQ, 512, hTo, 1024, qdst(3), True, gq[:, 4:5], onesb, 128)
                proj_fm(C_IQ, 512, hTo, 1024, qdst(4), False, None, None, 0)
                proj_fm(C_AK, 512, hTa, 2048, kdst(0), True, gq[:, 1:2], blk64, 64)
                proj_fm(C_BK, 512, hTa, 2048, kdst(1), False, None, None, 0)
                proj_fm(C_CK, 512, hTa, 2048, kdst(2), True, gq[:, 3:4], onesb, 128)
                proj_fm(C_DK, 128, hTa, 2048, kdst(3), True, gq[:, 5:6], onesb, 128)
                proj_fm(C_IK, 64, hTa, 2048, kdst(3, r0=128, rows=64), False, None, None, 0, rows=64)
                for (c0, ncols, v0) in ((C_AV, 512, 0), (C_BV, 512, 512), (C_CV, 512, 1024), (C_DV, 128, 1536)):
                    ws = load_slab(c0, ncols)
                    for t in range(16):
                        p_ = pp[cnt["p"] % 3]; cnt["p"] += 1
                        sg_ = stg[cnt["s"] % 3]; cnt["s"] += 1
                        for k in range(16):
                            S.op("pe", lambda e, p_=p_, ws=ws, k=k, t=t, ncols=ncols: e.matmul(p_[:, 0:ncols], hTa[:, k, t * 128:(t + 1) * 128], ws[:, k, 0:ncols], start=(k == 0), stop=(k == 15)),
                                 reads=R(ws, hTa), writes=R(p_))
                        S.op("act", lambda e, p_=p_, sg_=sg_, ncols=ncols: e.copy(sg_[:, 0:ncols], p_[:, 0:ncols]), reads=R(p_), writes=R(sg_))
                        S.dma("sp", lambda e, sg_=sg_, t=t, v0=v0, ncols=ncols: e.dma_start(out=Vd[t * 128:(t + 1) * 128, v0:v0 + ncols], in_=sg_[:, 0:ncols]), reads=R(sg_), writes=["Vd"])
                ws = load_slab(C_IW, 8)
                for t in range(8):
                    p_ = pp[cnt["p"] % 3]; cnt["p"] += 1
                    for k in range(16):
                        S.op("pe", lambda e, p_=p_, ws=ws, k=k, t=t: e.matmul(p_[:, 0:8], hTo[:, k, t * 128:(t + 1) * 128], ws[:, k, 0:8], start=(k == 0), stop=(k == 15)),
                             reads=R(ws, hTo), writes=R(p_))
                    S.op("act", lambda e, p_=p_: e.copy(stgf[:], p_[:, 0:8]), reads=R(p_), writes=R(stgf))
                    S.dma("sp", lambda e, t=t: e.dma_start(out=IW[t * 128:(t + 1) * 128, :], in_=stgf[:]), reads=R(stgf), writes=["IW"])

        if 2 in phases:
          with Scope() as P2:
            QTs = P2.sb("QTs", [128, 4, 1024], BF16)
            KTs = P2.sb("KTs", [128, 4, 2048], BF16)
            Vs = P2.sb("Vs", [128, 16, 512], BF16)
            tS = [P2.sb("tS", [128, 512], F32) for _ in range(2)]
            pT = [P2.sb("pT", [128, 512], BF16) for _ in range(2)]
            ostg = [P2.sb("ostg", [128, 512], BF16) for _ in range(2)]
            rden = P2.sb("rden", [128, 512], F32)
            pS3 = [P2.ps("pS", [128, 512]) for _ in range(3)]
            pS = pS3[0:2]
            pI = P2.ps("pI", [128, 512])
            dgen = [None]

            def tick(n=1):
                if dgen[0] is not None:
                    for _ in range(n):
                        try:
                            next(dgen[0])
                        except StopIteration:
                            dgen[0] = None
                            break
            pO = P2.ps("pO", [128, 512]); pD = P2.ps("pD", [128, 512])
            pX = P2.ps("pX", [128, 512])
            pXb = P2.ps("pXb", [128, 128], BF16)
            cn = {"s": 0, "o": 0}

            def load_qkv(m, kheads=4, vcols=512, v0=0):
                S.dma("sp", lambda e: e.dma_start(out=QTs[:], in_=QT[m].rearrange("(h p) n -> p h n", p=128)), reads=["QT%d" % m], writes=R(QTs))
                if kheads == 4:
                    S.dma("sp", lambda e: e.dma_start(out=KTs[:], in_=KT[m].rearrange("(h p) n -> p h n", p=128)), reads=["KT%d" % m], writes=R(KTs))
                else:
                    S.dma("sp", lambda e: e.dma_start(out=KTs[:, 0, :], in_=KT[m, 0:128, :]), reads=["KT%d" % m], writes=R(KTs))
                S.dma("sp", lambda e: e.dma_start(out=Vs[:, :, 0:vcols], in_=Vd[:, v0:v0 + vcols].rearrange("(t p) c -> p t c", p=128)), reads=["Vd"], writes=R(Vs))

            def softmax_attn(J, qap, kap, vsl, slope, extra=None):
                na = 8 * J + 8

                def emit_qk(a):
                    i3 = a % 3
                    ex = extra(a) if extra is not None else None
                    S.op("pe", lambda e: e.matmul(pS3[i3][:], kap(a), qap(J), start=True, stop=(ex is None)), reads=R(KTs, QTs), writes=R(pS3[i3]))
                    if ex is not None:
                        S.op("pe", lambda e: e.matmul(pS3[i3][:], ex[0], ex[1], start=False, stop=True), reads=ex[2], writes=R(pS3[i3]))
                emit_qk(0)
                if na > 1:
                    emit_qk(1)
                for a in range(na):
                    i = a % 2; i3 = a % 3
                    if a < 8 * J:
                        tab = cf[:, O_BF:O_BF + 512]; cbias = slope * 128.0 * (a - 8 * J)
                    else:
                        r = a - 8 * J
                        tab = cf[:, O_BD + r * 512:O_BD + (r + 1) * 512]; cbias = 0.0
                    S.op("dve", lambda e: e.scalar_tensor_tensor(out=tS[i][:], in0=tab, scalar=float(slope), in1=pS3[i3][:], op0=ALU.mult, op1=ALU.add),
                         reads=R(pS3[i3], cf), writes=R(tS[i]))
                    S.op("act", lambda e: e.activation(out=pT[i][:], in_=tS[i][:], func=AF.Exp, bias=float(cbias)), reads=R(tS[i]), writes=R(pT[i]))
                    if a + 2 < na:
                        emit_qk(a + 2)
                    S.op("pe", lambda e: e.matmul(pO[:], vsl(a), pT[i][:], start=(a == 0), stop=(a == na - 1)), reads=R(Vs, pT[i]), writes=R(pO))
                    S.op("pe", lambda e: e.matmul(pD[:], onesb, pT[i][:], start=(a == 0), stop=(a == na - 1)), reads=R(cb, pT[i]), writes=R(pD))
                    tick()

            qap = None
            QTs_cur = [QTs]

            def out_branch(n, h, J, src_fn):
                o = ostg[cn["o"] % 2]; cn["o"] += 1
                src_fn(o)
                S.dma("sp", lambda e, o=o: e.dma_start(out=BRT[(n * 4 + h) * 128:(n * 4 + h + 1) * 128, J * 512:(J + 1) * 512], in_=o[:]), reads=R(o), writes=["BRT"])

            if "D" in mixers:
                iqT = P2.sb("iqT", [128, 4, 1024], BF16); ikT = P2.sb("ikT", [128, 2048], BF16)
                iw = P2.sb("iw", [128, 8, 8], F32); absw = P2.sb("absw", [128, 8, 8], F32); sgn = P2.sb("sgn", [128, 8, 8], F32)
                acc = P2.sb("acc", [128, 2048], F32); work = P2.sb("work", [128, 2048], F32)
                rl = [P2.sb("rl", [128, 512], F32) for _ in range(2)]
                nm = P2.sb("nm", [128, 2048], BF16)
                nmT = P2.sb("nmT", [128, 16, 1024], BF16)
                m8 = P2.sb("m8", [128, 8], F32)
                blo = P2.sb("blo", [128, 1], F32); bmid = P2.sb("bmid", [128, 1], F32); bcnt = P2.sb("bcnt", [128, 1], F32)
                BW = 65536.0; NIT = 26
                S.dma("sp", lambda e: e.dma_start(out=iqT[:], in_=QT[4].rearrange("(h p) n -> p h n", p=128)), reads=["QT4"], writes=R(iqT))
                S.dma("sp", lambda e: e.dma_start(out=ikT[0:64, :], in_=KT[3, 128:192, :]), reads=["KT3"], writes=R(ikT))
                S.dma("sp", lambda e: e.dma_start(out=ikT[64:128, :], in_=KT[3, 128:192, :]), reads=["KT3"], writes=R(ikT))
                S.dma("sp", lambda e: e.dma_start(out=iw[:], in_=IW.rearrange("(t p) c -> p t c", p=128)), reads=["IW"], writes=R(iw))
                S.op("act", lambda e: e.activation(out=absw[:], in_=iw[:], func=AF.Abs), reads=R(iw), writes=R(absw))
                S.op("dve", lambda e: e.tensor_scalar(sgn[:], iw[:], 0.0, 2.0, op0=ALU.is_ge, op1=ALU.mult), reads=R(iw), writes=R(sgn))
                S.op("dve", lambda e: e.tensor_scalar(sgn[:], sgn[:], -1.0, None, op0=ALU.add), reads=R(sgn), writes=R(sgn))
                S.op("pool", lambda e: e.memset(nmT[:], 0.0), writes=R(nmT))

                def d_indexer():
                    ri = 0
                    for j in range(1, 8):
                        Lk = 256 * (j + 1)
                        for ih in range(8):
                            lo = (ih % 2) * 64
                            for c in range((Lk + 511) // 512):
                                w_ = min(512, Lk - c * 512)
                                i = ri % 2; ri += 1
                                S.op("pe", lambda e: e.matmul(pI[:, 0:w_], iqT[lo:lo + 64, ih // 2, j * 128:(j + 1) * 128], ikT[lo:lo + 64, c * 512:c * 512 + w_], start=True, stop=True),
                                     reads=R(iqT, ikT), writes=R(pI))
                                S.op("act", lambda e: e.activation(out=rl[i][:, 0:w_], in_=pI[:, 0:w_], func=AF.Relu, scale=absw[:, j, ih:ih + 1]), reads=R(pI, absw), writes=R(rl[i]))
                                if ih == 0:
                                    S.op("dve", lambda e: e.tensor_scalar(acc[:, c * 512:c * 512 + w_], rl[i][:, 0:w_], sgn[:, j, 0:1], None, op0=ALU.mult), reads=R(rl[i], sgn), writes=R(acc))
                                else:
                                    S.op("dve", lambda e: e.scalar_tensor_tensor(out=acc[:, c * 512:c * 512 + w_], in0=rl[i][:, 0:w_], scalar=sgn[:, j, ih:ih + 1], in1=acc[:, c * 512:c * 512 + w_], op0=ALU.mult, op1=ALU.add),
                                         reads=R(rl[i], sgn, acc), writes=R(acc))
                                yield
                        S.op("pool", lambda e: e.tensor_tensor(acc[:, Lk - 256:Lk], acc[:, Lk - 256:Lk], cf[:, O_TMA:O_TMA + 256], op=ALU.add), reads=R(acc, cf), writes=R(acc))
                        S.op("dve", lambda e: e.reduce_max(out=blo[:], in_=acc[:, 0:Lk], axis=AX.X), reads=R(acc), writes=R(blo))
                        S.op("dve", lambda e: e.tensor_scalar(blo[:], blo[:], -BW, None, op0=ALU.add), reads=R(blo), writes=R(blo))
                        yield
                        for it in range(NIT):
                            half = BW / (2.0 ** (it + 1))
                            S.op("dve", lambda e: e.tensor_scalar(bmid[:], blo[:], half, None, op0=ALU.add), reads=R(blo), writes=R(bmid))
                            S.op("dve", lambda e: e.tensor_scalar(work[:, 0:Lk], acc[:, 0:Lk], bmid[:, 0:1], 0.0, op0=ALU.is_ge, op1=ALU.add, accum_out=bcnt[:]), reads=R(acc, bmid), writes=R(work, bcnt))
                            S.op("dve", lambda e: e.tensor_scalar(bcnt[:], bcnt[:], 255.5, half, op0=ALU.is_ge, op1=ALU.mult), reads=R(bcnt), writes=R(bcnt))
                            S.op("dve", lambda e: e.tensor_tensor(blo[:], blo[:], bcnt[:], op=ALU.add), reads=R(blo, bcnt), writes=R(blo))
                            yield
                        S.op("dve", lambda e: e.tensor_scalar(nm[:, 0:Lk], acc[:, 0:Lk], blo[:, 0:1], -30000.0, op0=ALU.is_lt, op1=ALU.mult), reads=R(acc, blo), writes=R(nm))
                        for a in range(2 * j + 2):
                            S.op("pe", lambda e: e.transpose(pXb[:], nm[:, a * 128:(a + 1) * 128], idb), reads=R(nm, cb), writes=R(pXb))
                            S.op("act", lambda e: e.copy(nmT[:, a, j * 128:(j + 1) * 128], pXb[:]), reads=R(pXb), writes=R(nmT))
                            yield
                dgen[0] = d_indexer()

            if "A" in mixers:
                load_qkv(0, vcols=512, v0=0)
                lam = P2.sb("lam", [128, 4], F32)
                o0 = P2.sb("o0", [128, 512], F32); o1 = P2.sb("o1", [128, 512], F32); osq = P2.sb("osq", [128, 512], BF16)
                gsub = P2.sb("gsub", [128, 1], F32)
                S.op("dve", lambda e: e.tensor_tensor(lam[:, 0:1], vecs[:, V_LAM:V_LAM + 1], vecs[:, V_LAM + 1:V_LAM + 2], op=ALU.mult), reads=R(vecs), writes=R(lam))
                S.op("dve", lambda e: e.tensor_tensor(lam[:, 1:2], vecs[:, V_LAM + 2:V_LAM + 3], vecs[:, V_LAM + 3:V_LAM + 4], op=ALU.mult), reads=R(vecs), writes=R(lam))
                S.op("pe", lambda e: e.matmul(pX[:, 0:2], cf[:, O_ONEG:O_ONEG + 128], lam[:, 0:2], start=True, stop=True), reads=R(cf, lam), writes=R(pX))
                S.op("act", lambda e: e.activation(out=lam[:, 2:4], in_=pX[:, 0:2], func=AF.Exp, scale=-1.0), reads=R(pX), writes=R(lam))
                S.op("dve", lambda e: e.tensor_tensor(lam[:, 0:1], lam[:, 3:4], lam[:, 2:3], op=ALU.subtract), reads=R(lam), writes=R(lam))
                S.op("dve", lambda e: e.tensor_tensor(lam[:, 0:1], lam[:, 0:1], vecs[:, V_LI:V_LI + 1], op=ALU.subtract), reads=R(lam, vecs), writes=R(lam))
                S.op("dve", lambda e: e.tensor_tensor(gsub[:], vecs[:, V_SUBLN:V_SUBLN + 1], vecs[:, V_OML:V_OML + 1], op=ALU.mult), reads=R(vecs), writes=R(gsub))
                for h in range(4):
                    for J in range(2):
                        for comp in range(2):
                            lo, hi = comp * 64, comp * 64 + 64
                            qap = (lambda J, lo=lo, hi=hi, h=h: QTs[lo:hi, h, J * 512:(J + 1) * 512])
                            softmax_attn(J, qap, lambda a, lo=lo, hi=hi, h=h: KTs[lo:hi, h, a * 128:(a + 1) * 128],
                                         lambda a, h=h: Vs[:, a, h * 128:(h + 1) * 128], S_A[h])
                            dst = o0 if comp == 0 else o1
                            S.op("dve", lambda e: e.reciprocal(rden[:], pD[:]), reads=R(pD), writes=R(rden))
                            S.op("dve", lambda e, dst=dst: e.tensor_tensor(dst[:], pO[:], rden[:], op=ALU.mult), reads=R(pO, rden), writes=R(dst))
                        S.op("dve", lambda e: e.scalar_tensor_tensor(out=o0[:], in0=o1[:], scalar=lam[:, 0:1], in1=o0[:], op0=ALU.mult, op1=ALU.add), reads=R(o0, o1, lam), writes=R(o0))
                        S.op("act", lambda e: e.activation(out=osq[:], in_=o0[:], func=AF.Square), reads=R(o0), writes=R(osq))
                        S.op("pe", lambda e: e.matmul(pX[:], onesb, osq[:], start=True, stop=True), reads=R(osq, cb), writes=R(pX))
                        S.op("act", lambda e: e.activation(out=rden[:], in_=pX[:], func=AF.Ln, bias=epsc[:, 0:1], scale=1.0 / 128), reads=R(pX, epsc), writes=R(rden))
                        S.op("act", lambda e: e.activation(out=rden[:], in_=rden[:], func=AF.Exp, scale=-0.5), reads=R(rden), writes=R(rden))
                        S.op("dve", lambda e: e.tensor_tensor(o0[:], o0[:], rden[:], op=ALU.mult), reads=R(o0, rden), writes=R(o0))
                        out_branch(0, h, J, lambda o: S.op("act", lambda e, o=o: e.activation(out=o[:], in_=o0[:], func=AF.Identity, scale=gsub[:, 0:1]), reads=R(o0, gsub), writes=R(o)))

            if "B" in mixers:
                load_qkv(1, vcols=512, v0=512)
                Ef = [P2.sb("Ef", [128, 512], F32) for _ in range(3)]
                SPf = [P2.sb("SPf", [128, 512], F32) for _ in range(3)]
                Racc = P2.sb("Racc", [128, 512], F32)
                pWs = [pD, pX]
                uneg = cf[:, O_UNEG:O_UNEG + 128]; oneg = cf[:, O_ONEG:O_ONEG + 128]
                for h in range(4):
                    for J in range(2):
                        na = 8 * J + 8
                        order = list(range(na - 1, -1, -1))

                        def ktqt(a):
                            return KTs[:, h, a * 128:(a + 1) * 128], QTs[:, h, J * 512:(J + 1) * 512]

                        def stage1(ai):
                            a = order[ai]; i3 = ai % 3
                            kt, qt = ktqt(a)
                            S.op("pe", lambda e: e.matmul(pS3[i3][:], kt, qt, start=True, stop=True), reads=R(KTs, QTs), writes=R(pS3[i3]))
                            S.op("act", lambda e: e.activation(out=Ef[i3][:], in_=pS3[i3][:], func=AF.Exp), reads=R(pS3[i3]), writes=R(Ef[i3]))
                            S.op("act", lambda e: e.activation(out=SPf[i3][:], in_=Ef[i3][:], func=AF.Ln, bias=1.0), reads=R(Ef[i3]), writes=R(SPf[i3]))
                            if a >= 8 * J:
                                bm = cf[:, O_BM + (a - 8 * J) * 512:O_BM + (a - 8 * J + 1) * 512]
                                S.op("dve", lambda e: e.tensor_tensor(SPf[i3][:], SPf[i3][:], bm, op=ALU.mult), reads=R(SPf[i3], cf), writes=R(SPf[i3]))
                        stage1(0)
                        if na > 1:
                            stage1(1)
                        for ai in range(na):
                            a = order[ai]; i = ai % 2; i3 = ai % 3
                            pW = pWs[ai % 2]
                            kt, qt = ktqt(a)
                            S.op("pe", lambda e: e.matmul(pW[:], kt, qt, start=True, stop=False), reads=R(KTs, QTs), writes=R(pW))
                            S.op("pe", lambda e: e.matmul(pW[:], uneg, SPf[i3][:], start=False, stop=(ai == 0)), reads=R(cf, SPf[i3]), writes=R(pW))
                            if ai > 0:
                                S.op("pe", lambda e: e.matmul(pW[:], oneg, Racc[:], start=False, stop=True), reads=R(cf, Racc), writes=R(pW))
                            S.op("act", lambda e: e.activation(out=pT[i][:], in_=pW[:], func=AF.Exp), reads=R(pW), writes=R(pT[i]))
                            if a >= 8 * J:
                                bm = cf[:, O_BM + (a - 8 * J) * 512:O_BM + (a - 8 * J + 1) * 512]
                                S.op("dve", lambda e: e.tensor_tensor(pT[i][:], pT[i][:], bm, op=ALU.mult), reads=R(pT[i], cf), writes=R(pT[i]))
                            if ai == 0:
                                S.op("dve", lambda e: e.tensor_copy(Racc[:], SPf[i3][:]), reads=R(SPf[i3]), writes=R(Racc))
                            else:
                                S.op("dve", lambda e: e.tensor_tensor(Racc[:], Racc[:], SPf[i3][:], op=ALU.add), reads=R(SPf[i3], Racc), writes=R(Racc))
                            if ai + 2 < na:
                                stage1(ai + 2)
                            S.op("pe", lambda e: e.matmul(pO[:], Vs[:, a, h * 128:(h + 1) * 128], pT[i][:], start=(ai == 0), stop=(ai == na - 1)), reads=R(Vs, pT[i]), writes=R(pO))
                            tick()
                        out_branch(1, h, J, lambda o: S.op("act", lambda e, o=o: e.copy(o[:], pO[:]), reads=R(pO), writes=R(o)))

            if "C" in mixers:
                load_qkv(2, vcols=512, v0=1024)
                kmf = P2.sb("kmf", [128, 4, 8], F32); kmb = P2.sb("kmb", [128, 4, 8], BF16)
                gsb = P2.sb("gsb", [128, 8], F32); top8 = P2.sb("top8", [128, 8], F32); thr = P2.sb("thr", [128, 1], F32)
                nsT = P2.sb("nsT", [8, 512], BF16)
                for h in range(4):
                    S.op("dve", lambda e, h=h: e.tensor_reduce(out=kmf[:, h, :], in_=KTs[:, h, :].rearrange("p (n k) -> p n k", k=256), axis=AX.X, op=ALU.add), reads=R(KTs), writes=R(kmf))
                S.op("dve", lambda e: e.tensor_scalar(kmb[:], kmf[:], 1.0 / 256, None, op0=ALU.mult), reads=R(kmf), writes=R(kmb))
                for h in range(4):
                    for J in range(2):
                        for jj in range(4):
                            j = 4 * J + jj
                            S.op("pe", lambda e, h=h, j=j: e.matmul(pX[:, 0:8], QTs[:, h, j * 128:(j + 1) * 128], kmb[:, h, :], start=True, stop=True), reads=R(QTs, kmb), writes=R(pX))
                            S.op("dve", lambda e, j=j: e.tensor_tensor(gsb[:], pX[:, 0:8], cf[:, O_MG + j * 8:O_MG + j * 8 + 8], op=ALU.add), reads=R(pX, cf), writes=R(gsb))
                            S.op("dve", lambda e: e.max(out=top8[:], in_=gsb[:]), reads=R(gsb), writes=R(top8))
                            S.op("dve", lambda e: e.tensor_scalar(thr[:], top8[:, 2:3], -1e29, None, op0=ALU.max), reads=R(top8), writes=R(thr))
                            S.op("dve", lambda e: e.tensor_scalar(gsb[:], gsb[:], thr[:, 0:1], None, op0=ALU.is_ge), reads=R(gsb, thr), writes=R(gsb))
                            S.op("dve", lambda e, j=j: e.tensor_tensor(gsb[:], gsb[:], cf[:, O_OWN + j * 8:O_OWN + j * 8 + 8], op=ALU.add), reads=R(gsb, cf), writes=R(gsb))
                            S.op("dve", lambda e: e.tensor_scalar(gsb[:], gsb[:], -1.0, 30000.0, op0=ALU.add, op1=ALU.mult), reads=R(gsb), writes=R(gsb))
                            S.op("pe", lambda e: e.transpose(pX[0:8, 128:256], gsb[:], idf), reads=R(gsb, cf), writes=R(pX))
                            S.op("act", lambda e, jj=jj: e.copy(nsT[:, jj * 128:(jj + 1) * 128], pX[0:8, 128:256]), reads=R(pX), writes=R(nsT))
                        qap = (lambda J, h=h: QTs[:, h, J * 512:(J + 1) * 512])
                        softmax_attn(J, qap, lambda a, h=h: KTs[:, h, a * 128:(a + 1) * 128], lambda a, h=h: Vs[:, a, h * 128:(h + 1) * 128], S_C[h],
                                     extra=lambda a: (cb[0:8, O_EALL + (a // 2) * 128:O_EALL + (a // 2 + 1) * 128], nsT[:], R(cb, nsT)))
                        S.op("dve", lambda e: e.reciprocal(rden[:], pD[:]), reads=R(pD), writes=R(rden))
                        out_branch(2, h, J, lambda o: S.op("dve", lambda e, o=o: e.tensor_tensor(o[:], pO[:], rden[:], op=ALU.mult), reads=R(pO, rden), writes=R(o)))

            if "D" in mixers:
                if dgen[0] is not None:
                    for _ in dgen[0]:
                        pass
                    dgen[0] = None
                load_qkv(3, kheads=1, vcols=128, v0=1536)
                for h in range(4):
                    for J in range(2):
                        qap = (lambda J, h=h: QTs[:, h, J * 512:(J + 1) * 512])
                        softmax_attn(J, qap, lambda a: KTs[:, 0, a * 128:(a + 1) * 128], lambda a: Vs[:, a, 0:128], S_D[h],
                                     extra=lambda a, J=J: (idb, nmT[:, a, J * 512:(J + 1) * 512], R(cb, nmT)))
                        S.op("dve", lambda e: e.reciprocal(rden[:], pD[:]), reads=R(pD), writes=R(rden))
                        out_branch(3, h, J, lambda o: S.op("dve", lambda e, o=o: e.tensor_tensor(o[:], pO[:], rden[:], op=ALU.mult), reads=R(pO, rden), writes=R(o)))

        if 3 in phases:
          with Scope() as P3:
            hTo = P3.sb("hTo3", [128, 16, 1024], BF16)
            brT = P3.sb("brT", [128, 16, 1024], BF16)
            yT = P3.sb("yT", [128, 16, 1024], BF16)
            wsl = [P3.sb("wsl3", [128, 16, 512], BF16) for _ in range(2)]
            wbs = [P3.sb("wbs", [128, 4, 512], BF16) for _ in range(2)]
            yacc = P3.sb("yacc", [128, 4, 1024], F32)
            gt = [P3.sb("gt", [128, 512], F32) for _ in range(2)]
            xo = [P3.sb("xo", [128, 1024], F32) for _ in range(2)]
            pg = [P3.ps("pg3", [128, 512]) for _ in range(2)]
            pu = [P3.ps("pu3", [128, 512]) for _ in range(2)]
            pz = [P3.ps("pz3", [128, 512]) for _ in range(2)]
            S.dma("sp", lambda e: e.dma_start(out=hTo[:], in_=hTo_d.rearrange("(k p) n -> p k n", p=128)), reads=["hTo_d"], writes=R(hTo))
            S.dma("sp", lambda e: e.dma_start(out=brT[:], in_=BRT.rearrange("(k p) n -> p k n", p=128)), reads=["BRT"], writes=R(brT))
            wi = 0; gi = 0
            for ds in range(4):
                for n in range(4):
                    ws = wsl[wi % 2]; wb = wbs[wi % 2]; wi += 1
                    c0 = C_GL + n * 2048 + ds * 512
                    S.dma("pool", lambda e, ws=ws, c0=c0: e.dma_start(out=ws[:], in_=w_in[:, c0:c0 + 512].rearrange("(k p) n -> p k n", p=128)), writes=R(ws))
                    S.dma("pool", lambda e, wb=wb, n=n, ds=ds: e.dma_start(out=wb[:], in_=w_br[n, :, ds * 512:(ds + 1) * 512].rearrange("(k p) n -> p k n", p=128)), writes=R(wb))
                    for sub in range(4):
                        dg = ds * 4 + sub
                        for tt in range(2):
                            b = gi % 2; gi += 1
                            for k in range(16):
                                S.op("pe", lambda e, b=b, ws=ws, k=k, sub=sub, tt=tt: e.matmul(pg[b][:], ws[:, k, sub * 128:(sub + 1) * 128], hTo[:, k, tt * 512:(tt + 1) * 512], start=(k == 0), stop=(k == 15)),
                                     reads=R(ws, hTo), writes=R(pg[b]))
                            for k in range(4):
                                S.op("pe", lambda e, b=b, wb=wb, k=k, sub=sub, tt=tt, n=n: e.matmul(pu[b][:], wb[:, k, sub * 128:(sub + 1) * 128], brT[:, n * 4 + k, tt * 512:(tt + 1) * 512], start=(k == 0), stop=(k == 3)),
                                     reads=R(wb, brT), writes=R(pu[b]))
                            S.op("act", lambda e, b=b, n=n, dg=dg: e.activation(out=gt[b][:], in_=pg[b][:], func=AF.Sigmoid, bias=vecs[:, V_GB + n * 16 + dg:V_GB + n * 16 + dg + 1]), reads=R(pg[b], vecs), writes=R(gt[b]))
                            ya = yacc[:, sub, tt * 512:(tt + 1) * 512]
                            if n == 0:
                                S.op("dve", lambda e, b=b, ya=ya: e.tensor_tensor(ya, gt[b][:], pu[b][:], op=ALU.mult), reads=R(gt[b], pu[b]), writes=R(yacc))
                            else:
                                S.op("dve", lambda e, b=b: e.tensor_tensor(gt[b][:], gt[b][:], pu[b][:], op=ALU.mult), reads=R(gt[b], pu[b]), writes=R(gt[b]))
                                S.op("dve", lambda e, b=b, ya=ya: e.tensor_tensor(ya, ya, gt[b][:], op=ALU.add), reads=R(gt[b], yacc), writes=R(yacc))
                S.op("act", lambda e, ds=ds: e.copy(yT[:, ds * 4:(ds + 1) * 4, :], yacc[:]), reads=R(yacc), writes=R(yT))
            zi = 0
            for os_ in range(4):
                ws = wsl[wi % 2]; wi += 1
                S.dma("pool", lambda e, ws=ws, os_=os_: e.dma_start(out=ws[:], in_=w_out[:, os_ * 512:(os_ + 1) * 512].rearrange("(k p) n -> p k n", p=128)), writes=R(ws))
                for sub in range(4):
                    og = os_ * 4 + sub
                    x_ = xo[og % 2]
                    S.dma("sp", lambda e, x_=x_, og=og: e.dma_start(out=x_[:], in_=xTo[og * 128:(og + 1) * 128, :]), writes=R(x_))
                    for tt in range(2):
                        b = zi % 2; zi += 1
                        for k in range(16):
                            S.op("pe", lambda e, b=b, ws=ws, k=k, sub=sub, tt=tt: e.matmul(pz[b][:], ws[:, k, sub * 128:(sub + 1) * 128], yT[:, k, tt * 512:(tt + 1) * 512], start=(k == 0), stop=(k == 15)),
                                 reads=R(ws, yT), writes=R(pz[b]))
                        S.op("dve", lambda e, b=b, x_=x_, og=og, tt=tt: e.scalar_tensor_tensor(out=x_[:, tt * 512:(tt + 1) * 512], in0=pz[b][:], scalar=vecs[:, V_MOD + 32 + og:V_MOD + 32 + og + 1], in1=x_[:, tt * 512:(tt + 1) * 512], op0=ALU.mult, op1=ALU.add),
                             reads=R(pz[b], vecs, x_), writes=R(x_))
                    S.dma("sp", lambda e, x_=x_, og=og: e.dma_start(out=xmT[og * 128:(og + 1) * 128, :], in_=x_[:]), reads=R(x_), writes=["xmT"])

        if 4 in phases:
          with Scope() as P4:
            rw = P4.sb("rw", [128, 16, 32], F32); rb = P4.sb("rb", [1, 32], F32); onesf = P4.sb("onesf", [1, 128], F32)
            hb = [P4.sb("hb", [128, 16, TW], BF16) for _ in range(2)]
            lg = P4.sb("lg", [128, 32], F32); t8 = P4.sb("t8", [128, 8], F32); nm1 = P4.sb("nm1", [128, 1], F32)
            sel = P4.sb("sel", [128, 32], F32); ex = P4.sb("ex", [128, 32], F32); ssum = P4.sb("ssum", [128, 1], F32)
            pl = P4.ps("plg", [128, 32])
            S.dma("sp", lambda e: e.dma_start(out=rw[:], in_=rw_d), writes=R(rw))
            S.dma("sp", lambda e: e.dma_start(out=rb[:], in_=rb_d), writes=R(rb))
            S.op("dve", lambda e: e.memset(onesf[:], 1.0), writes=R(onesf))
            src = xmT if 3 in phases else xTo

            def c_h2(tt, x):
                h_ = hb[tt % 2]
                S.op("dve", lambda e, h_=h_, x=x: e.tensor_copy(h_[:], x[:]), reads=R(x), writes=R(h_))
                S.dma("sp", lambda e, h_=h_, tt=tt: e.dma_start(out=h2T[:, tt * TW:(tt + 1) * TW].rearrange("(k p) n -> p k n", p=128), in_=h_[:]), reads=R(h_), writes=["h2T"])
                for s4 in range(TW // 128):
                    for k in range(16):
                        S.op("pe", lambda e, x=x, k=k, s4=s4: e.matmul(pl[:], x[:, k, s4 * 128:(s4 + 1) * 128], rw[:, k, :], start=(k == 0), stop=False), reads=R(x, rw), writes=R(pl))
                    S.op("pe", lambda e: e.matmul(pl[:], onesf[:], rb[:], start=False, stop=True), reads=R(onesf, rb), writes=R(pl))
                    S.op("dve", lambda e: e.tensor_copy(lg[:], pl[:]), reads=R(pl), writes=R(lg))
                    S.op("dve", lambda e: e.max(out=t8[:], in_=lg[:]), reads=R(lg), writes=R(t8))
                    S.op("dve", lambda e: e.tensor_scalar(nm1[:], t8[:, 0:1], -1.0, None, op0=ALU.mult), reads=R(t8), writes=R(nm1))
                    S.op("dve", lambda e: e.tensor_scalar(sel[:], lg[:], t8[:, 3:4], None, op0=ALU.is_ge), reads=R(lg, t8), writes=R(sel))
                    S.op("act", lambda e: e.activation(out=ex[:], in_=lg[:], func=AF.Exp, bias=nm1[:, 0:1]), reads=R(lg, nm1), writes=R(ex))
                    S.op("dve", lambda e: e.tensor_tensor(ex[:], ex[:], sel[:], op=ALU.mult), reads=R(ex, sel), writes=R(ex))
                    S.op("dve", lambda e: e.reduce_sum(out=ssum[:], in_=ex[:], axis=AX.X), reads=R(ex), writes=R(ssum))
                    S.op("dve", lambda e: e.reciprocal(ssum[:], ssum[:]), reads=R(ssum), writes=R(ssum))
                    S.op("dve", lambda e: e.tensor_scalar(ex[:], ex[:], ssum[:, 0:1], None, op0=ALU.mult), reads=R(ex, ssum), writes=R(ex))
                    S.dma("sp", lambda e, tt=tt, s4=s4: e.dma_start(out=G_o[tt * TW + s4 * 128:tt * TW + (s4 + 1) * 128, :], in_=ex[:]), reads=R(ex), writes=["G"])
            def src_read_hook():
                pass
            norm_tiles_src = src
            _orig_dma = S.dma
            def dma_with_dep(q, fn, reads=(), writes=()):
                return _orig_dma(q, fn, reads=list(reads) + ["xmT"], writes=writes)
            S.dma = dma_with_dep
            norm_tiles(P4, norm_tiles_src, 1024, AB[:, 2, :], mod(3), c_h2)
            S.dma = _orig_dma
        S.finish(["xmT", "h2T", "G", "BRT"])
        S.emit()
    print("M ops", S.n_ops, "waits", S.n_waits)
    return nc


def col16(v):
    return np.ascontiguousarray(v.reshape(16, 128).T)


def prep_vecs(l, mod_b, n1g, n2g, gate_b, a_qn_g, a_kn_g, c_qn_g, c_kn_g, d_qn_g, d_kn_g, a_subln_g, lq1, lk1, lq2, lk2):
    v = np.zeros((128, NV), np.float32)
    for i in range(6):
        v[:, V_MOD + 16 * i:V_MOD + 16 * (i + 1)] = col16(mod_b[i * 2048:(i + 1) * 2048])
    v[:, V_N1G:V_N1G + 16] = col16(n1g); v[:, V_N2G:V_N2G + 16] = col16(n2g)
    v[:, V_GB:V_GB + 64] = gate_b.reshape(64, 128).T
    v[:, V_AQG] = np.tile(a_qn_g, 2); v[:, V_AKG] = np.tile(a_kn_g, 2)
    v[:, V_CQG] = c_qn_g; v[:, V_CKG] = c_kn_g; v[:, V_DQG] = d_qn_g; v[:, V_DKG] = d_kn_g
    v[:, V_SUBLN] = a_subln_g
    v[0:64, V_LAM] = lq1; v[0:64, V_LAM + 1] = lk1; v[0:64, V_LAM + 2] = lq2; v[0:64, V_LAM + 3] = lk2
    li = 0.8 - 0.6 * math.exp(-0.3 * l)
    v[:, V_LI] = li; v[:, V_OML] = 1.0 - li
    return v


def own_cols(p):
    return np.concatenate([np.arange(128 * (2 * j + p), 128 * (2 * j + p + 1)) for j in range(8)])


def prep_m(l, p, x_b, mod_b, P):
    cf, cb = m_consts(p)
    xT = np.ascontiguousarray(x_b.T)
    return {
        "xTa": xT, "xTo": np.ascontiguousarray(xT[:, own_cols(p)]),
        "vecs": prep_vecs(l, mod_b, P["norm1_g"], P["norm2_g"], P["gate_b"], P["a_qn_g"], P["a_kn_g"], P["c_qn_g"], P["c_kn_g"],
                          P["d_qn_g"], P["d_kn_g"], P["a_subln_g"], P["a_lam_q1"], P["a_lam_k1"], P["a_lam_q2"], P["a_lam_k2"]),
        "cf": cf, "cb": cb, "w_in": P["w_in"], "w_br": P["w_branch"], "w_out": P["w_out"],
        "rw": np.ascontiguousarray(P["router_w"].reshape(16, 128, 32).transpose(1, 0, 2)), "rb": np.ascontiguousarray(P["router_b"].reshape(1, 32)),
    }


_PROGS = {}


def _prog(name, fn):
    if name not in _PROGS:
        _PROGS[name] = fn()
    return _PROGS[name]


def _run(nc, in_maps):
    res = run_bass_kernel_spmd(nc, in_maps, core_ids=list(range(len(in_maps))))
    return res.results


def kernel(x, c, ada_w, ada_b, norm1_g, norm2_g, w_in, gate_b, a_qn_g, a_kn_g, a_lam_q1, a_lam_k1,
           a_lam_q2, a_lam_k2, a_subln_g, c_qn_g, c_kn_g, d_qn_g, d_kn_g, w_branch, w_out,
           router_w, router_b, w1, b1, w2, b2):
    f32 = np.float32
    x = np.asarray(x, f32); c = np.asarray(c, f32)
    L = 2
    nc0 = _prog("l0", build_l0)
    cT = np.ascontiguousarray(c.T)
    ada_w = np.asarray(ada_w, f32); ada_b = np.asarray(ada_b, f32)
    r0 = _run(nc0, [{"cT": cT, "w": np.ascontiguousarray(ada_w[:, :, i * 1536:(i + 1) * 1536]),
                     "b": np.ascontiguousarray(ada_b[:, i * 1536:(i + 1) * 1536])} for i in range(8)])
    mod = np.concatenate([r["mod"] for r in r0], axis=2)
    ncm = _prog("m", build_m); nce = _prog("e2", build_e2); ncc = _prog("c2", build_c2)
    for l in range(L):
        P = dict(norm1_g=norm1_g[l], norm2_g=norm2_g[l], w_in=np.asarray(w_in[l], f32), gate_b=gate_b[l], a_qn_g=a_qn_g[l], a_kn_g=a_kn_g[l],
                 a_lam_q1=a_lam_q1[l], a_lam_k1=a_lam_k1[l], a_lam_q2=a_lam_q2[l], a_lam_k2=a_lam_k2[l], a_subln_g=a_subln_g[l],
                 c_qn_g=c_qn_g[l], c_kn_g=c_kn_g[l], d_qn_g=d_qn_g[l], d_kn_g=d_kn_g[l], w_branch=np.asarray(w_branch[l], f32),
                 w_out=np.asarray(w_out[l], f32), router_w=np.asarray(router_w[l], f32), router_b=np.asarray(router_b[l], f32))
        P = {k: np.asarray(v, f32) for k, v in P.items()}
        ims = [prep_m(l, core % 2, x[core // 2], mod[l, core // 2], P) for core in range(8)]
        rm = _run(ncm, ims)
        del ims
        h2_all = np.ascontiguousarray(np.concatenate([r["h2T"].T for r in rm], axis=0))
        G_all = np.concatenate([r["G"] for r in rm], axis=0)
        w1l = np.asarray(w1[l], f32); w2l = np.asarray(w2[l], f32); b1l = np.asarray(b1[l], f32); b2l = np.asarray(b2[l], f32)
        cst = e2_consts()
        ime = []
        for ec in range(8):
            es = slice(4 * ec, 4 * ec + 4)
            ime.append({"h2": h2_all, "Gtok": np.ascontiguousarray(G_all[:, es]), "w1": np.ascontiguousarray(w1l[es]),
                        "b1t_in": np.ascontiguousarray(b1l[es].reshape(4, 32, 128).transpose(2, 0, 1)), "w2": np.ascontiguousarray(w2l[es]),
                        "b2": np.ascontiguousarray(b2l[es]), "cst": cst})
        re_ = _run(nce, ime)
        del ime
        imc = []
        for core in range(8):
            P8 = np.ascontiguousarray(np.stack([re_[ec]["y"][core * 1024:(core + 1) * 1024] for ec in range(8)], axis=0))
            imc.append({"P8": P8, "xm_in": np.ascontiguousarray(rm[core]["xmT"].T),
                        "g2row": np.ascontiguousarray(mod[l, core // 2, 5 * 2048:6 * 2048].reshape(1, 2048))})
        rc = _run(ncc, imc)
        xn = np.empty_like(x)
        for core in range(8):
            xn[core // 2][own_cols(core % 2)] = rc[core]["xn"]
        x = xn
    return x
```
